# Optimizing a Trainium2 kernel written in Bass

```python
import math
import jax
import jax.numpy as jnp
from jax import lax
import numpy as np


D_MODEL = 1024
BATCH = 8
SEQ = 8192
DEPTH = 2

CHUNK = 64
Q_BLOCK = 128
MEM_LEN = 256
N_MIXERS = 2
ROPE_THETA = 500000.0
LN_EPS = 1e-5
DEEPNORM_ALPHA = (2.0 * DEPTH) ** 0.25
DEEPNORM_BETA = (8.0 * DEPTH) ** -0.25

DIFF_HEADS = 8
DIFF_HEAD_DIM = 64
DIFF_ROPE_DIM = DIFF_HEAD_DIM // 4

MLA_HEADS = 8
MLA_Q_RANK = 256
MLA_KV_RANK = 256
MLA_ROPE_DIM = 32
MLA_NOPE_DIM = 96
MLA_V_DIM = 128
IDX_HEADS = 16
IDX_DIM = 64
IDX_ROPE_DIM = IDX_DIM // 4
IDX_TOPK_MAX = 256

MEM_HEADS = 4
MEM_HEAD_DIM = D_MODEL // MEM_HEADS

N_GROUPS = 4
EXPERTS_PER_GROUP = 4
N_EXPERTS = N_GROUPS * EXPERTS_PER_GROUP
EXPERT_TOP_K = 2
EXPERT_FF = 512

N_DIFF_LAYERS = (DEPTH + 1) // 2
N_DSA_LAYERS = DEPTH // 2

kernel_name = "hybrid_diff_dsa_hmoe_deepnorm_encoder"


def layer_norm(x, g, b):
    xf = x.astype(jnp.float32)
    mu = jnp.mean(xf, axis=-1, keepdims=True)
    xc = xf - mu
    var = jnp.mean(xc * xc, axis=-1, keepdims=True)
    y = xc * lax.rsqrt(var + LN_EPS) * g.astype(jnp.float32) + b.astype(jnp.float32)
    return y.astype(x.dtype)


def rms_norm(x, g):
    xf = x.astype(jnp.float32)
    y = xf * lax.rsqrt(jnp.mean(xf * xf, axis=-1, keepdims=True) + LN_EPS) * g.astype(jnp.float32)
    return y.astype(x.dtype)


def rope_tables(positions, rot_dim):
    inv_freq = ROPE_THETA ** (-jnp.arange(0, rot_dim, 2, dtype=jnp.float32) / rot_dim)
    ang = positions.astype(jnp.float32)[..., None] * inv_freq
    return jnp.cos(ang), jnp.sin(ang)


def apply_partial_rope(x, cos, sin):
    half = cos.shape[-1]
    c = cos[:, :, None, :].astype(x.dtype)
    s = sin[:, :, None, :].astype(x.dtype)
    x1 = x[..., :half]
    x2 = x[..., half:2 * half]
    return jnp.concatenate([x1 * c - x2 * s, x2 * c + x1 * s, x[..., 2 * half:]], axis=-1)


def to_blocks(a, blk):
    b, s = a.shape[0], a.shape[1]
    return jnp.moveaxis(a.reshape((b, s // blk, blk) + a.shape[2:]), 1, 0)


def from_blocks(a):
    nb, b, blk = a.shape[0], a.shape[1], a.shape[2]
    return jnp.moveaxis(a, 0, 1).reshape((b, nb * blk) + a.shape[3:])


def diff_lambda_init(layer_idx):
    return 0.8 - 0.6 * math.exp(-0.3 * layer_idx)


def differential_attention(x, cos, sin, w_in, lam, subln_g, w_out, lambda_init):
    bsz, seq, _ = x.shape
    h, d = DIFF_HEADS, DIFF_HEAD_DIM
    q, k, v = jnp.split(x @ w_in, 3, axis=-1)
    q = apply_partial_rope(q.reshape(bsz, seq, 2 * h, d), cos, sin) * (d ** -0.5)
    k = apply_partial_rope(k.reshape(bsz, seq, 2 * h, d), cos, sin)
    q = q.reshape(bsz, seq, h, 2, d)
    k = k.reshape(bsz, seq, h, 2, d)
    v = v.reshape(bsz, seq, h, 2 * d)
    lamf = lam.astype(jnp.float32)
    lam_full = jnp.exp(jnp.sum(lamf[0] * lamf[1])) - jnp.exp(jnp.sum(lamf[2] * lamf[3])) + lambda_init
    key_chunk = jnp.arange(seq) // CHUNK

    def block(args):
        q_blk, blk_idx = args
        q_chunk = (blk_idx * Q_BLOCK + jnp.arange(Q_BLOCK)) // CHUNK
        allowed = key_chunk[None, :] <= q_chunk[:, None]
        s = jnp.einsum('bqhcd,bkhcd->bhcqk', q_blk, k).astype(jnp.float32)
        p = jax.nn.softmax(jnp.where(allowed, s, -jnp.inf), axis=-1)
        p_diff = p[:, :, 0] - lam_full * p[:, :, 1]
        return jnp.einsum('bhqk,bkhe->bqhe', p_diff.astype(v.dtype), v)

    o = from_blocks(lax.map(block, (to_blocks(q, Q_BLOCK), jnp.arange(seq // Q_BLOCK))))
    o = rms_norm(o, subln_g) * (1.0 - lambda_init)
    return o.reshape(bsz, seq, h * 2 * d) @ w_out


def dsa_sparse_mla(x, cos_r, sin_r, cos_i, sin_i, w_in, q_norm_g, kv_norm_g, w_uq, w_qidx, w_uk, w_uv, w_out):
    bsz, seq, _ = x.shape
    h = MLA_HEADS
    splits = [MLA_Q_RANK,
              MLA_Q_RANK + MLA_KV_RANK,
              MLA_Q_RANK + MLA_KV_RANK + MLA_ROPE_DIM,
              MLA_Q_RANK + MLA_KV_RANK + MLA_ROPE_DIM + IDX_DIM]
    c_q, c_kv, k_rope, k_idx, w_idx = jnp.split(x @ w_in, splits, axis=-1)
    c_q = rms_norm(c_q, q_norm_g)
    c_kv = rms_norm(c_kv, kv_norm_g)
    q = (c_q @ w_uq).reshape(bsz, seq, h, MLA_ROPE_DIM + MLA_NOPE_DIM)
    q_rope = apply_partial_rope(q[..., :MLA_ROPE_DIM], cos_r, sin_r)
    q_lat = jnp.einsum('bshn,hrn->bshr', q[..., MLA_ROPE_DIM:], w_uk)
    q_full = jnp.concatenate([q_lat, q_rope], axis=-1) * ((MLA_ROPE_DIM + MLA_NOPE_DIM) ** -0.5)
    k_rope = apply_partial_rope(k_rope[:, :, None, :], cos_r, sin_r)[:, :, 0]
    kv_lat = jnp.concatenate([c_kv, k_rope], axis=-1)
    q_idx = apply_partial_rope((c_q @ w_qidx).reshape(bsz, seq, IDX_HEADS, IDX_DIM), cos_i, sin_i)
    k_idx = apply_partial_rope(k_idx[:, :, None, :], cos_i, sin_i)[:, :, 0]
    w_idx = w_idx * ((IDX_HEADS * IDX_DIM) ** -0.5)
    top_k = min(IDX_TOPK_MAX, seq // 4)
    key_chunk = jnp.arange(seq) // CHUNK
    gather = jax.vmap(lambda table, idx: table[idx])

    def block(args):
        qf, qi, wi, chunk_idx = args
        logits = jnp.einsum('bqhd,bkd->bqhk', qi, k_idx)
        score = jnp.einsum('bqh,bqhk->bqk', wi, jax.nn.relu(logits)).astype(jnp.float32)
        score = jnp.where((key_chunk <= chunk_idx)[None, None, :], score, -jnp.inf)
        top_score, top_idx = lax.top_k(score, top_k)
        valid = jnp.isfinite(top_score)
        sel = gather(kv_lat, top_idx)
        s = jnp.einsum('bqhd,bqkd->bhqk', qf, sel).astype(jnp.float32)
        p = jax.nn.softmax(jnp.where(valid[:, None], s, -jnp.inf), axis=-1)
        return jnp.einsum('bhqk,bqkr->bqhr', p.astype(sel.dtype), sel[..., :MLA_KV_RANK])

    o_lat = from_blocks(lax.map(block, (to_blocks(q_full, CHUNK), to_blocks(q_idx, CHUNK),
                                        to_blocks(w_idx, CHUNK), jnp.arange(seq // CHUNK))))
    o = jnp.einsum('bshr,hrv->bshv', o_lat, w_uv).reshape(bsz, seq, h * MLA_V_DIM)
    return o @ w_out


def memory_cross_attention(x, mem_k, mem_v, w_q, w_out):
    bsz, seq, _ = x.shape
    q = (x @ w_q).reshape(bsz, seq, MEM_HEADS, MEM_HEAD_DIM) * (MEM_HEAD_DIM ** -0.5)
    s = jnp.einsum('bshd,bmhd->bhsm', q, mem_k).astype(jnp.float32)
    p = jax.nn.softmax(s, axis=-1)
    o = jnp.einsum('bhsm,bmhd->bshd', p.astype(mem_v.dtype), mem_v).reshape(bsz, seq, MEM_HEADS * MEM_HEAD_DIM)
    return o @ w_out


def hierarchical_moe(x, w_group, b_group, w_expert, b_expert, w_gate, w_up, w_down):
    bsz, seq, dm = x.shape
    n_tok = bsz * seq
    t = x.reshape(n_tok, dm)
    g_logits = (t @ w_group + b_group).astype(jnp.float32)
    g_sel = jnp.argmax(g_logits, axis=-1)
    g_gate = jnp.max(jax.nn.softmax(g_logits, axis=-1), axis=-1, keepdims=True)
    e_logits = (t @ w_expert + b_expert).astype(jnp.float32).reshape(n_tok, N_GROUPS, EXPERTS_PER_GROUP)
    e_logits = e_logits[jnp.arange(n_tok), g_sel]
    top_val, top_idx = lax.top_k(e_logits, EXPERT_TOP_K)
    top_w = jax.nn.softmax(top_val, axis=-1) * g_gate
    expert_id = g_sel[:, None] * EXPERTS_PER_GROUP + top_idx
    combine = jnp.sum(jax.nn.one_hot(expert_id, N_EXPERTS, dtype=jnp.float32) * top_w[..., None], axis=1).astype(t.dtype)
    y = jnp.zeros_like(t)
    for e in range(N_EXPERTS):
        hid = jax.nn.silu(t @ w_gate[e]) * (t @ w_up[e])
        y = y + combine[:, e:e + 1] * (hid @ w_down[e])
    return y.reshape(bsz, seq, dm)


def setup_inputs(seed: int = 0) -> dict:
    key = jax.random.key(seed)
    ks = iter(jax.random.split(key, 40))
    beta = DEEPNORM_BETA
    d = D_MODEL

    def nrm(shape, scale):
        return jax.random.normal(next(ks), shape, jnp.float32) * scale

    x = nrm((BATCH, SEQ, d), 1.0)
    mem = nrm((BATCH, MEM_LEN, d), 1.0)
    offsets = jax.random.randint(next(ks), (BATCH, 1), 0, 1024) * CHUNK
    positions = (offsets + jnp.arange(SEQ)[None, :]).astype(jnp.int32)

    diff_w = DIFF_HEADS * 2 * DIFF_HEAD_DIM
    a_w_in = jnp.concatenate([nrm((N_DIFF_LAYERS, d, 2 * diff_w), d ** -0.5),
                              nrm((N_DIFF_LAYERS, d, diff_w), beta * d ** -0.5)], axis=-1)
    a_lambda = nrm((N_DIFF_LAYERS, 4, DIFF_HEAD_DIM), 0.1)
    a_subln_g = 1.0 + nrm((N_DIFF_LAYERS, 2 * DIFF_HEAD_DIM), 0.02)
    a_w_out = nrm((N_DIFF_LAYERS, diff_w, d), beta * diff_w ** -0.5)

    b_in_w = MLA_Q_RANK + MLA_KV_RANK + MLA_ROPE_DIM + IDX_DIM + IDX_HEADS
    b_w_in = nrm((N_DSA_LAYERS, d, b_in_w), d ** -0.5)
    b_q_norm_g = 1.0 + nrm((N_DSA_LAYERS, MLA_Q_RANK), 0.02)
    b_kv_norm_g = 1.0 + nrm((N_DSA_LAYERS, MLA_KV_RANK), 0.02)
    b_w_uq = nrm((N_DSA_LAYERS, MLA_Q_RANK, MLA_HEADS * (MLA_ROPE_DIM + MLA_NOPE_DIM)), MLA_Q_RANK ** -0.5)
    b_w_qidx = nrm((N_DSA_LAYERS, MLA_Q_RANK, IDX_HEADS * IDX_DIM), MLA_Q_RANK ** -0.5)
    b_w_uk = nrm((N_DSA_LAYERS, MLA_HEADS, MLA_KV_RANK, MLA_NOPE_DIM), MLA_KV_RANK ** -0.5)
    b_w_uv = nrm((N_DSA_LAYERS, MLA_HEADS, MLA_KV_RANK, MLA_V_DIM), beta * MLA_KV_RANK ** -0.5)
    b_w_out = nrm((N_DSA_LAYERS, MLA_HEADS * MLA_V_DIM, d), beta * (MLA_HEADS * MLA_V_DIM) ** -0.5)

    mem_w = MEM_HEADS * MEM_HEAD_DIM
    mem_w_kv = jnp.concatenate([nrm((d, mem_w), d ** -0.5), nrm((d, mem_w), beta * d ** -0.5)], axis=-1)
    xa_w_q = nrm((DEPTH, d, mem_w), d ** -0.5)
    xa_w_out = nrm((DEPTH, mem_w, d), beta * mem_w ** -0.5)

    moe_w_group = nrm((DEPTH, d, N_GROUPS), d ** -0.5)
    moe_b_group = nrm((DEPTH, N_GROUPS), 0.01)
    moe_w_expert = nrm((DEPTH, d, N_EXPERTS), d ** -0.5)
    moe_b_expert = nrm((DEPTH, N_EXPERTS), 0.01)
    moe_w_gate = nrm((DEPTH, N_EXPERTS, d, EXPERT_FF), d ** -0.5)
    moe_w_up = nrm((DEPTH, N_EXPERTS, d, EXPERT_FF), beta * d ** -0.5)
    moe_w_down = nrm((DEPTH, N_EXPERTS, EXPERT_FF, d), beta * EXPERT_FF ** -0.5)

    ln_g = 1.0 + nrm((DEPTH, 3, d), 0.02)
    ln_b = nrm((DEPTH, 3, d), 0.02)

    return {"x": x, "mem": mem, "positions": positions,
            "a_w_in": a_w_in, "a_lambda": a_lambda, "a_subln_g": a_subln_g, "a_w_out": a_w_out,
            "b_w_in": b_w_in, "b_q_norm_g": b_q_norm_g, "b_kv_norm_g": b_kv_norm_g,
            "b_w_uq": b_w_uq, "b_w_qidx": b_w_qidx, "b_w_uk": b_w_uk, "b_w_uv": b_w_uv, "b_w_out": b_w_out,
            "mem_w_kv": mem_w_kv, "xa_w_q": xa_w_q, "xa_w_out": xa_w_out,
            "moe_w_group": moe_w_group, "moe_b_group": moe_b_group,
            "moe_w_expert": moe_w_expert, "moe_b_expert": moe_b_expert,
            "moe_w_gate": moe_w_gate, "moe_w_up": moe_w_up, "moe_w_down": moe_w_down,
            "ln_g": ln_g, "ln_b": ln_b}


def reference(x, mem, positions, a_w_in, a_lambda, a_subln_g, a_w_out,
              b_w_in, b_q_norm_g, b_kv_norm_g, b_w_uq, b_w_qidx, b_w_uk, b_w_uv, b_w_out,
              mem_w_kv, xa_w_q, xa_w_out,
              moe_w_group, moe_b_group, moe_w_expert, moe_b_expert, moe_w_gate, moe_w_up, moe_w_down,
              ln_g, ln_b):
    bsz = x.shape[0]
    n_mem = mem.shape[1]
    cos_d, sin_d = rope_tables(positions, DIFF_ROPE_DIM)
    cos_r, sin_r = rope_tables(positions, MLA_ROPE_DIM)
    cos_i, sin_i = rope_tables(positions, IDX_ROPE_DIM)
    mem_k, mem_v = jnp.split(mem @ mem_w_kv, 2, axis=-1)
    mem_k = mem_k.reshape(bsz, n_mem, MEM_HEADS, MEM_HEAD_DIM)
    mem_v = mem_v.reshape(bsz, n_mem, MEM_HEADS, MEM_HEAD_DIM)
    alpha = DEEPNORM_ALPHA
    h = x
    for i in range(DEPTH):
        j = i // N_MIXERS
        if i % N_MIXERS == 0:
            mix = differential_attention(h, cos_d, sin_d, a_w_in[j], a_lambda[j], a_subln_g[j], a_w_out[j],
                                         diff_lambda_init(i))
        else:
            mix = dsa_sparse_mla(h, cos_r, sin_r, cos_i, sin_i, b_w_in[j], b_q_norm_g[j], b_kv_norm_g[j],
                                 b_w_uq[j], b_w_qidx[j], b_w_uk[j], b_w_uv[j], b_w_out[j])
        h = layer_norm(alpha * h + mix, ln_g[i, 0], ln_b[i, 0])
        h = layer_norm(alpha * h + memory_cross_attention(h, mem_k, mem_v, xa_w_q[i], xa_w_out[i]),
                       ln_g[i, 1], ln_b[i, 1])
        h = layer_norm(alpha * h + hierarchical_moe(h, moe_w_group[i], moe_b_group[i], moe_w_expert[i],
                                                    moe_b_expert[i], moe_w_gate[i], moe_w_up[i], moe_w_down[i]),
                       ln_g[i, 2], ln_b[i, 2])
    return h
```

```python
import math
import contextlib
import numpy as np
import ml_dtypes
import concourse.bass as bass
import concourse.mybir as mybir
from concourse.bass_utils import run_bass_kernel_spmd

F32 = mybir.dt.float32
BF16 = mybir.dt.bfloat16
I32 = mybir.dt.int32
U8 = mybir.dt.uint8
AF = mybir.ActivationFunctionType
ALU = mybir.AluOpType
AX = mybir.AxisListType

D = 1024
DEPTH = 2
ALPHA = (2.0 * DEPTH) ** 0.25
LN_EPS = 1e-5
ROPE_THETA = 500000.0
NEG = -30000.0
ENGS = ("pe", "act", "dve", "pool", "sp")
NPOOL = 88
ST_ENG = "sp"


PSUM_NAMES = {"ps0", "psb", "pk", "pv", "pt", "pa", "ps", "po", "pl", "pm", "pA", "psc", "ptp", "pmix", "p1", "p2", "pg", "pu",
              "py", "e_pt", "p0", "pt2", "pq", "pqi", "pt7", "pw", "pscore", "plg"}


def is_psum(key):
    base = key if isinstance(key, str) else key[0]
    return base in PSUM_NAMES


class Op:
    __slots__ = ("eng", "fn", "is_dma", "deps", "sem", "val", "signal", "waits", "semidx")

    def __init__(self, eng, fn, is_dma, semidx):
        self.eng = eng
        self.fn = fn
        self.is_dma = is_dma
        self.deps = []
        self.sem = None
        self.val = 0
        self.signal = False
        self.waits = []
        self.semidx = semidx


class Sched:
    def __init__(self, nc):
        self.nc = nc
        self.ops = []
        self.last_write = {}
        self.reads_since = {}
        self.keymap = {}
        self.dmas_since = []
        self.last_on = {}

    def add(self, eng, fn, reads=(), writes=(), dma=False, semkey=None):
        semidx = None
        if dma:
            if semkey is None:
                semkey = writes[0]
            if semkey not in self.keymap:
                assert len(self.keymap) < NPOOL, "too many dma sem keys in phase"
                self.keymap[semkey] = len(self.keymap)
            semidx = self.keymap[semkey]
        op = Op(eng, fn, dma, semidx)
        reads_eff = [r for r in reads if not is_psum(r)]
        writes_eff = list(writes) + [r for r in reads if is_psum(r)]
        deps = {}
        for r in reads_eff:
            for w in self.last_write.get(r, {}).values():
                deps[id(w)] = (w, "raw")
        for r in writes_eff:
            for w in self.last_write.get(r, {}).values():
                if id(w) not in deps:
                    deps[id(w)] = (w, "waw")
            for rd in self.reads_since.get(r, ()):
                if id(rd) not in deps:
                    deps[id(rd)] = (rd, "war")
        for d, kind in deps.values():
            if (not d.is_dma) and (not dma) and d.eng == eng:
                if eng == "pe":
                    continue
                if kind != "raw" and eng != "pool":
                    continue
            op.deps.append(d)
        for r in reads_eff:
            self.reads_since.setdefault(r, []).append(op)
        for r in writes_eff:
            self.last_write.setdefault(r, {})["dma" if dma else eng] = op
            self.reads_since[r] = []
        self.ops.append(op)
        if dma:
            self.dmas_since.append(op)
        else:
            self.last_on[eng] = op
        return op

    def barrier(self):
        lasts = [o for o in self.last_on.values() if o.fn is not None] + list(self.dmas_since)
        for e in ENGS:
            op = Op(e, None, False, None)
            op.deps = list(lasts)
            self.ops.append(op)
        self.last_write = {}
        self.reads_since = {}
        self.keymap = {}
        self.dmas_since = []
        self.last_on = {}

    def emit(self):
        nc = self.nc
        for op in self.ops:
            for d in op.deps:
                d.signal = True
        stack = contextlib.ExitStack()
        eng_sem = {e: stack.enter_context(nc.semaphore("s_" + e)) for e in ENGS}
        npool = max([o.semidx for o in self.ops if o.is_dma] + [0]) + 1
        pool = [stack.enter_context(nc.semaphore("d_%d" % i)) for i in range(npool)]
        counts = {}
        for op in self.ops:
            if op.is_dma:
                k = ("d", op.semidx)
                op.sem = pool[op.semidx]
                counts[k] = counts.get(k, 0) + 16
                op.val = counts[k]
                op.signal = True
            else:
                op.sem = eng_sem[op.eng]
                if op.signal and op.fn is not None:
                    counts[op.eng] = counts.get(op.eng, 0) + 1
                op.val = counts.get(op.eng, 0)
        waited = {e: {} for e in ENGS}
        per_eng = {e: [] for e in ENGS}
        nw = 0
        for op in self.ops:
            w = waited[op.eng]
            need = {}
            for d in op.deps:
                key = id(d.sem)
                if w.get(key, 0) >= d.val:
                    continue
                if key not in need or need[key][1] < d.val:
                    need[key] = (d.sem, d.val)
            for key, (sem, val) in need.items():
                w[key] = val
                op.waits.append((sem, val))
            nw += len(op.waits)
            per_eng[op.eng].append(op)
        self.stats = {e: len(per_eng[e]) for e in ENGS}
        self.stats["waits"] = nw
        self.stats["maxsem"] = max(counts.values()) if counts else 0

        def run(eng_obj, lst):
            for op in lst:
                for sem, val in op.waits:
                    eng_obj.wait_ge(sem, val)
                if op.fn is None:
                    continue
                ins = op.fn(eng_obj)
                if op.signal:
                    ins.then_inc(op.sem, 16 if op.is_dma else 1)

        with nc.Block() as block:
            @block.tensor
            def _(e):
                run(e, per_eng["pe"])

            @block.scalar
            def _(e):
                run(e, per_eng["act"])

            @block.vector
            def _(e):
                run(e, per_eng["dve"])

            @block.gpsimd
            def _(e):
                run(e, per_eng["pool"])

            @block.sync
            def _(e):
                run(e, per_eng["sp"])
        stack.close()


def _inv_freq(rot):
    return (np.float32(ROPE_THETA) ** (-np.arange(0, rot, 2, dtype=np.float32) / np.float32(rot))).astype(np.float32)


CF_ID = 0
CF_ONES = 128
CF_INVF = 256
CF_SEL = 288
CF_HM = 288 + 1024
CF_N = CF_HM + 16
CB_ID = 0
CB_ONES = 128
CB_E = 256
CB_N = 256 + 1024


def _consts():
    cf = np.zeros((128, CF_N), np.float32)
    cf[:, CF_ID:CF_ID + 128] = np.eye(128, dtype=np.float32)
    cf[:, CF_ONES:CF_ONES + 128] = 1.0
    cf[:, CF_INVF:CF_INVF + 32] = np.concatenate([_inv_freq(16), _inv_freq(32), _inv_freq(16)])[None, :]
    cb = np.zeros((128, CB_N), np.float32)
    cb[:, CB_ID:CB_ID + 128] = np.eye(128, dtype=np.float32)
    cb[:, CB_ONES:CB_ONES + 128] = 1.0
    p = np.arange(128)
    j, q = p // 16, p % 16
    for g in range(8):
        cf[16 * g + q, CF_SEL + g * 128 + p] = 1.0
        cb[p, CB_E + g * 128 + 16 * g + q] = 1.0
    for h in range(16):
        cf[:, CF_HM + h] = (h // 2 == j).astype(np.float32)
    return cf, cb.astype(ml_dtypes.bfloat16)


class B:
    pass


class StopBuild(Exception):
    pass


def build(NT, debug=(), upto=99):
    NTL = NT // 128
    NQB = NT // 512
    nc = bass.Bass("TRN2", target_bir_lowering=False)
    S = Sched(nc)
    st = contextlib.ExitStack()

    def din(name, shape, dt=F32):
        return nc.dram_tensor(name, list(shape), dt, kind="ExternalInput").ap()

    def dscr(name, shape, dt):
        return nc.dram_tensor(name, list(shape), dt, kind="Internal").ap()

    x = din("x", [NT, D])
    mem = din("mem", [256, D])
    positions = din("positions", [NTL, 128], I32)
    a_w_in = din("a_w_in", [D, 3072]); a_lambda = din("a_lambda", [4, 64]); a_subln_g = din("a_subln_g", [128, 1])
    a_w_out = din("a_w_out", [D, D])
    b_w_in = din("b_w_in", [D, 624]); b_q_norm_g = din("b_q_norm_g", [256]); b_kv_norm_g = din("b_kv_norm_g", [256])
    b_w_uq = din("b_w_uq", [256, 1024]); b_w_qidx = din("b_w_qidx", [256, 1024])
    b_w_uk = din("b_w_uk", [8, 256, 96]); b_w_uv = din("b_w_uv", [8, 256, 128]); b_w_out = din("b_w_out", [D, D])
    mem_w_kv = din("mem_w_kv", [D, 2048]); xa_w_q = din("xa_w_q", [2, D, D]); xa_w_out = din("xa_w_out", [2, D, D])
    moe_w_group = din("moe_w_group", [2, D, 4]); moe_b_group = din("moe_b_group", [2, 4])
    moe_w_expert = din("moe_w_expert", [2, D, 16]); moe_b_expert = din("moe_b_expert", [2, 16])
    moe_w_gate = din("moe_w_gate", [2, 16, D, 512]); moe_w_up = din("moe_w_up", [2, 16, D, 512])
    moe_w_down = din("moe_w_down", [2, 16, 512, D])
    ln_g = din("ln_g", [2, 3, D]); ln_b = din("ln_b", [2, 3, D])
    cstf = din("cstf", [128, CF_N]); cstb = din("cstb", [128, CB_N], BF16)
    out = nc.dram_tensor("out", [NT, D], F32, kind="ExternalOutput").ap()
    dbg = {k: nc.dram_tensor("dbg_" + k, [NT, D], F32, kind="ExternalOutput").ap() for k in debug}

    Wb = {
        "a_w_in": dscr("wb_a_w_in", [D, 3072], BF16), "a_w_out": dscr("wb_a_w_out", [D, D], BF16),
        "b_w_in": dscr("wb_b_w_in", [D, 624], BF16), "b_w_uq": dscr("wb_b_w_uq", [256, 1024], BF16),
        "b_w_qidx": dscr("wb_b_w_qidx", [256, 1024], BF16), "b_w_uk": dscr("wb_b_w_uk", [8, 256, 96], BF16),
        "b_w_uv": dscr("wb_b_w_uv", [8, 256, 128], BF16), "b_w_out": dscr("wb_b_w_out", [D, D], BF16),
        "mem_w_kv": dscr("wb_mem_w_kv", [D, 2048], BF16), "xa_w_q": dscr("wb_xa_w_q", [2, D, D], BF16),
        "xa_w_out": dscr("wb_xa_w_out", [2, D, D], BF16),
        "moe_w_gate": dscr("wb_moe_w_gate", [2, 16, D, 512], BF16), "moe_w_up": dscr("wb_moe_w_up", [2, 16, D, 512], BF16),
        "moe_w_down": dscr("wb_moe_w_down", [2, 16, 512, D], BF16),
    }
    H = [dscr("H0", [NT, D], F32), dscr("H1", [NT, D], F32)]
    HT = dscr("HT", [128, 8, NT], BF16)
    QT = dscr("QT", [128, 8, NT], BF16)
    KT = dscr("KT", [128, 8, NT], BF16)
    V = dscr("V", [NT, D], BF16)
    OT = dscr("OT", [128, 8, NT], BF16)
    ROPE = dscr("ROPE", [NT, 128], F32)
    KVT = dscr("KVT", [128, 3, NT], BF16)
    CKV = dscr("CKV", [NT, 256], BF16)
    KIT = dscr("KIT", [128, NT], BF16)
    QFT = dscr("QFT", [128, 8, 3, NT], BF16)
    QIT = dscr("QIT", [128, NTL, 1024], BF16)
    WIX = dscr("WIX", [NT, 16], F32)
    NM = dscr("NM", [NT, NT], BF16)

    ARENA = 200 * 1024
    arena = st.enter_context(nc.sbuf_tensor("arena", [128, ARENA], U8))
    PS = st.enter_context(nc.psum_tensor("ps", [128, 4096], F32))
    state = {"off": 0, "mark": 0, "phase": "p0"}

    def sb(name, shape, dt):
        esz = 4 if dt in (F32, I32) else 2
        n = int(np.prod(shape[1:])) * esz
        n = (n + 63) // 64 * 64
        off = state["off"]
        assert off + n <= ARENA, "arena overflow in %s: %s" % (state["phase"], name)
        state["off"] = off + n
        v = arena[:, off:off + n].bitcast(dt)[:, 0:int(np.prod(shape[1:]))]
        if len(shape) == 3:
            v = v.rearrange("p (a b) -> p a b", a=shape[1])
        elif len(shape) == 4:
            v = v.rearrange("p (a b c) -> p a b c", a=shape[1], b=shape[2])
        return v

    def bank(i, n=1):
        return PS[:, i * 512:(i + n) * 512]

    def bank_bf(i):
        return PS[:, i * 512:(i + 1) * 512].bitcast(BF16)

    def phase(name):
        state["np"] = state.get("np", 0) + 1
        if upto >= 0 and state["np"] > upto:
            raise StopBuild()
        S.barrier()
        state["off"] = state["mark"]
        state["phase"] = name

    def mm(o, lhsT, rhs, start, stop, R, W):
        S.add("pe", lambda e: e.matmul(o, lhsT=lhsT, rhs=rhs, start=start, stop=stop), reads=R, writes=W)

    def tr(o, i, ident, R, W):
        S.add("pe", lambda e: e.transpose(out=o, in_=i, identity=ident), reads=R, writes=W)

    def act(o, i, func, R, W, bias=None, scale=None, accum=None, eng="act"):
        kw = {}
        if bias is not None:
            kw["bias"] = bias
        if scale is not None:
            kw["scale"] = scale
        if accum is not None:
            kw["accum_out"] = accum
        S.add("act", lambda e: e.activation(out=o, in_=i, func=func, **kw), reads=R, writes=W)

    def tt(o, a, b, op, R, W, eng="dve"):
        S.add(eng, lambda e: e.tensor_tensor(out=o, in0=a, in1=b, op=op), reads=R, writes=W)

    def ts(o, a, s1, s2, op0, op1, R, W, accum=None, eng="dve"):
        kw = {}
        if accum is not None:
            kw["accum_out"] = accum
        if op1 is None:
            S.add(eng, lambda e: e.tensor_scalar(out=o, in0=a, scalar1=s1, scalar2=None, op0=op0, **kw), reads=R, writes=W)
        else:
            S.add(eng, lambda e: e.tensor_scalar(out=o, in0=a, scalar1=s1, scalar2=s2, op0=op0, op1=op1, **kw), reads=R, writes=W)

    def stt(o, a, s, b, op0, op1, R, W):
        S.add("dve", lambda e: e.scalar_tensor_tensor(out=o, in0=a, scalar=s, in1=b, op0=op0, op1=op1), reads=R, writes=W)

    def cp(o, i, R, W, eng="dve"):
        if eng == "act":
            S.add("act", lambda e: e.activation(out=o, in_=i, func=AF.Copy), reads=R, writes=W)
        else:
            S.add(eng, lambda e: e.tensor_copy(out=o, in_=i), reads=R, writes=W)

    def red(o, i, op, R, W, negate=False):
        S.add("dve", lambda e: e.tensor_reduce(out=o, in_=i, axis=AX.X, op=op, negate=negate), reads=R, writes=W)

    def recip(o, i, R, W):
        S.add("dve", lambda e: e.reciprocal(out=o, in_=i), reads=R, writes=W)

    def memset(o, v, W, eng="pool", R=()):
        S.add(eng, lambda e: e.memset(o, v), reads=R, writes=W)

    def dma(eng, o, i, R, W, key=None, slow=False):
        if slow:
            S.add(eng, lambda e: e.dma_start(out=o, in_=i, allow_slow_non_contiguous=True), reads=R, writes=W, dma=True, semkey=key)
        else:
            S.add(eng, lambda e: e.dma_start(out=o, in_=i), reads=R, writes=W, dma=True, semkey=key)

    cf = sb("cf", [128, CF_N], F32)
    cb = sb("cb", [128, CB_N], BF16)
    epsc = sb("epsc", [128, 1], F32)
    memKT = sb("memKT", [128, 8, 256], BF16)
    memV = sb("memV", [128, 2, 1024], BF16)
    comb = sb("comb", [128, NTL, 16], F32)
    gbc = sb("gbc", [128, 1024], F32)
    bbc = sb("bbc", [128, 1024], F32)
    neglam = sb("neglam", [128, 1], F32)
    gsc = sb("gsc", [128, 1], F32)
    state["mark"] = state["off"]
    ident = cb[:, CB_ID:CB_ID + 128]
    ones_b = cb[:, CB_ONES:CB_ONES + 128]
    ident32 = cf[:, CF_ID:CF_ID + 128]
    ones32 = cf[:, CF_ONES:CF_ONES + 128]

    def sub(k):
        if upto == -k:
            raise StopBuild()

    def p0():
        dma("sp", cf, cstf, [], ["cf"])
        dma("sp", cb, cstb, [], ["cb"])
        memset(epsc, LN_EPS, ["epsc"])
        ci = [0]

        def cast(dst, src):
            dma("pool", dst, src, [], [("cast", ci[0])], key=("cast", ci[0] % 8))
            ci[0] += 1

        cast(Wb["a_w_in"], a_w_in); cast(Wb["a_w_out"], a_w_out); cast(Wb["mem_w_kv"], mem_w_kv)
        for i in range(2):
            cast(Wb["xa_w_q"][i], xa_w_q[i]); cast(Wb["xa_w_out"][i], xa_w_out[i])
        cast(Wb["b_w_in"], b_w_in); cast(Wb["b_w_uq"], b_w_uq); cast(Wb["b_w_qidx"], b_w_qidx)
        cast(Wb["b_w_uk"].rearrange("h r n -> (h r) n"), b_w_uk.rearrange("h r n -> (h r) n"))
        cast(Wb["b_w_uv"].rearrange("h r n -> (h r) n"), b_w_uv.rearrange("h r n -> (h r) n"))
        cast(Wb["b_w_out"], b_w_out)
        for i in range(2):
            for e in range(16):
                cast(Wb["moe_w_gate"][i, e], moe_w_gate[i, e]); cast(Wb["moe_w_up"][i, e], moe_w_up[i, e])
                cast(Wb["moe_w_down"][i, e], moe_w_down[i, e])

        sub(1)
        posi = sb("posi", [128, 128], I32)
        posf = sb("posf", [128, 128], F32)
        post = sb("post", [128, NTL], F32)
        ang = sb("ang", [128, NTL, 32], F32)
        kk = sb("kk", [128, NTL, 32], F32)
        sn = sb("sn", [128, NTL, 32], F32)
        cs = sb("cs", [128, NTL, 32], F32)
        tab = sb("tab", [128, NTL, 128], F32)
        dma("sp", posi[0:NTL, :], positions, [], ["posi"])
        cp(posf[0:NTL, :], posi[0:NTL, :], ["posi"], ["posf"])
        S.add("pe", lambda e: e.transpose(out=bank(0)[:, 0:NTL], in_=posf[0:NTL, :], identity=ident32[0:NTL, 0:NTL]),
              reads=["posf", "cf"], writes=["ps0"])
        cp(post, bank(0)[:, 0:NTL], ["ps0"], ["post"])
        invf = cf[:, CF_INVF:CF_INVF + 32]
        tt(ang, post.unsqueeze(2).to_broadcast([128, NTL, 32]), invf.unsqueeze(1).to_broadcast([128, NTL, 32]), ALU.mult,
           ["post", "cf"], ["ang"])
        MAGIC = 12582912.0
        TWO_PI = 2.0 * math.pi
        C1 = 6.28125
        C2 = float(np.float32(TWO_PI - C1))
        ts(kk, ang, 1.0 / TWO_PI, MAGIC, ALU.mult, ALU.add, ["ang"], ["kk"])
        ts(kk, kk, -MAGIC, None, ALU.add, None, ["kk"], ["kk"])
        stt(ang, kk, -C1, ang, ALU.mult, ALU.add, ["kk", "ang"], ["ang"])
        stt(ang, kk, -C2, ang, ALU.mult, ALU.add, ["kk", "ang"], ["ang"])
        ts(ang, ang, math.pi, -math.pi, ALU.min, ALU.max, ["ang"], ["ang"])
        act(sn, ang, AF.Sin, ["ang"], ["sn"])
        stt(kk, ang, -1.0, ang, ALU.mult, ALU.max, ["ang"], ["kk"])
        ts(kk, kk, -1.0, math.pi / 2, ALU.mult, ALU.add, ["kk"], ["kk"])
        act(cs, kk, AF.Sin, ["kk"], ["cs"])
        for (o0, f0, nf) in ((0, 0, 8), (32, 8, 16), (96, 24, 8)):
            cp(tab[:, :, o0:o0 + nf], cs[:, :, f0:f0 + nf], ["cs"], [("tab", o0, 0)])
            cp(tab[:, :, o0 + nf:o0 + 2 * nf], cs[:, :, f0:f0 + nf], ["cs"], [("tab", o0, 1)])
            ts(tab[:, :, o0 + 2 * nf:o0 + 3 * nf], sn[:, :, f0:f0 + nf], -1.0, None, ALU.mult, None, ["sn"], [("tab", o0, 2)])
            cp(tab[:, :, o0 + 3 * nf:o0 + 4 * nf], sn[:, :, f0:f0 + nf], ["sn"], [("tab", o0, 3)])
        dma("sp", ROPE.rearrange("(t p) c -> p t c", p=128), tab,
            [("tab", o0, k) for o0 in (0, 32, 96) for k in range(4)], ["ROPE"])

        sub(2)
        lam_init0 = 0.8 - 0.6 * math.exp(-0.3 * 0)
        lamb = sb("lamb", [128, 4, 64], F32)
        lamp = sb("lamp", [128, 2, 64], F32)
        lams = sb("lams", [128, 2], F32)
        dma("sp", lamb.rearrange("p a b -> p (a b)"), a_lambda.rearrange("a b -> (a b)").partition_broadcast(128), [], ["lamb"])
        tt(lamp[:, 0, :], lamb[:, 0, :], lamb[:, 1, :], ALU.mult, ["lamb"], [("lamp", 0)])
        tt(lamp[:, 1, :], lamb[:, 2, :], lamb[:, 3, :], ALU.mult, ["lamb"], [("lamp", 1)])
        red(lams, lamp, ALU.add, [("lamp", 0), ("lamp", 1)], ["lams"])
        act(lams, lams, AF.Exp, ["lams"], ["lams"])
        tt(neglam, lams[:, 1:2], lams[:, 0:1], ALU.subtract, ["lams"], ["neglam"])
        ts(neglam, neglam, -lam_init0, None, ALU.add, None, ["neglam"], ["neglam"])
        dma("sp", gsc, a_subln_g, [], ["gsc"])
        ts(gsc, gsc, 1.0 - lam_init0, None, ALU.mult, None, ["gsc"], ["gsc"])

        sub(3)
        S.barrier()
        state["phase"] = "p0b"
        wkv = sb("wkv", [128, 8, 2048], BF16)
        memf = sb("memf", [128, 2, 1024], F32)
        memb = sb("memb", [128, 2, 1024], BF16)
        memT = sb("memT", [128, 8, 256], BF16)
        dma("sp", wkv, Wb["mem_w_kv"].rearrange("(c p) n -> p c n", p=128), [], ["wkv"])
        dma("sp", memf, mem.rearrange("(t p) d -> p t d", p=128), [], ["memf"])
        cp(memb, memf, ["memf"], ["memb"], eng="act")
        for mt in range(2):
            ptv = bank_bf(mt).rearrange("p (c t) -> p c t", c=8)
            for c in range(8):
                tr(ptv[:, c, :], memb[:, mt, c * 128:(c + 1) * 128], ident, ["memb"], [("psb", mt)])
            cp(memT[:, :, mt * 128:(mt + 1) * 128], ptv, [("psb", mt)], [("memT", mt)])
        for j in range(8):
            pk = bank(2 + j % 2)[:, 0:256]
            for k in range(8):
                mm(pk, wkv[:, k, j * 128:(j + 1) * 128], memT[:, k, :], k == 0, k == 7, ["wkv", ("memT", 0), ("memT", 1)], [("pk", j % 2)])
            cp(memKT[:, j, :], pk, [("pk", j % 2)], [("memKT", j)], eng="act" if j % 2 else "dve")
        for mt in range(2):
            for hf in range(2):
                pv = bank(4 + (mt * 2 + hf) % 2)
                for k in range(8):
                    mm(pv, memT[:, k, mt * 128:(mt + 1) * 128], wkv[:, k, 1024 + hf * 512:1024 + (hf + 1) * 512], k == 0, k == 7,
                       ["wkv", ("memT", 0), ("memT", 1)], [("pv", (mt * 2 + hf) % 2)])
                cp(memV[:, mt, hf * 512:(hf + 1) * 512], pv, [("pv", (mt * 2 + hf) % 2)], [("memV", mt, hf)], eng="act" if hf else "dve")


    def load_ln(li, j):
        dma("sp", gbc, ln_g[li, j].partition_broadcast(128), [], ["gbc"])
        dma("sp", bbc, ln_b[li, j].partition_broadcast(128), [], ["bbc"])

    class Epi:
        def __init__(self, pt_bank, nbuf=2):
            self.nb = nbuf
            self.hsb = [sb("e_h%d" % i, [128, 1024], F32) for i in range(nbuf)]
            self.z = [sb("e_z%d" % i, [128, 1024], F32) for i in range(nbuf)]
            self.hb = [sb("e_hb%d" % i, [128, 1024], BF16) for i in range(nbuf)]
            self.hT = [sb("e_hT%d" % i, [128, 8, 128], BF16) for i in range(nbuf)]
            self.st = [sb("e_st%d" % i, [128, 2, 6], F32) for i in range(nbuf)]
            self.mv = [sb("e_mv%d" % i, [128, 2], F32) for i in range(nbuf)]
            self.rs = [sb("e_rs%d" % i, [128, 1], F32) for i in range(nbuf)]
            self.ptb = pt_bank
            self.n = 0

        def run(self, t, mix, mixR, h_src, h_dst, dbg_dst=None):
            b = self.n % self.nb
            self.n += 1
            hsb, z, hb, hT, stt_, mv, rs = self.hsb[b], self.z[b], self.hb[b], self.hT[b], self.st[b], self.mv[b], self.rs[b]
            k = "e%d" % b
            rows = slice(t * 128, (t + 1) * 128)
            dma("sp", hsb, h_src[rows, :], [("Hsrc", t)], [k + "h"])
            stt(z, hsb, ALPHA, mix, ALU.mult, ALU.add, [k + "h"] + mixR, [k + "z"])
            for c in range(2):
                S.add("dve", lambda e, c=c: e.bn_stats(out=stt_[:, c, :], in_=z[:, c * 512:(c + 1) * 512]), reads=[k + "z"], writes=[(k + "st", c)])
            S.add("dve", lambda e: e.bn_aggr(out=mv, in_=stt_.rearrange("p a b -> p (a b)")), reads=[(k + "st", 0), (k + "st", 1)], writes=[k + "mv"])
            act(rs, mv[:, 1:2], AF.Sqrt, [k + "mv"], [k + "rs"], bias=epsc)
            recip(rs, rs, [k + "rs"], [k + "rs"])
            ts(z, z, mv[:, 0:1], rs, ALU.subtract, ALU.mult, [k + "z", k + "mv", k + "rs"], [k + "z"])
            tt(z, z, gbc, ALU.mult, [k + "z", "gbc"], [k + "z"], eng="pool")
            tt(hsb, z, bbc, ALU.add, [k + "z", "bbc"], [k + "h"])
            dma(ST_ENG, h_dst[rows, :], hsb, [k + "h"], [("Hdst", t)], key=k + "hst")
            if dbg_dst is not None:
                dma(ST_ENG, dbg_dst[rows, :], hsb, [k + "h"], [("dbg", t)], key=k + "dbg")
            cp(hb, hsb, [k + "h"], [k + "hb"], eng="act")
            ptv = bank_bf(self.ptb).rearrange("p (c t) -> p c t", c=8)
            for c in range(8):
                tr(ptv[:, c, :], hb[:, c * 128:(c + 1) * 128], ident, [k + "hb"], ["e_pt"])
            cp(hT, ptv, ["e_pt"], [k + "hT"], eng="act")
            dma(ST_ENG, HT[:, :, rows], hT, [k + "hT"], [("HT", t)], key=k + "hTst")

    def t0():
        phase("t0")
        xs = [sb("xs%d" % i, [128, 1024], F32) for i in range(2)]
        xb = [sb("xb%d" % i, [128, 1024], BF16) for i in range(2)]
        xT = [sb("xT%d" % i, [128, 8, 128], BF16) for i in range(2)]
        for t in range(NTL):
            b = t % 2
            rows = slice(t * 128, (t + 1) * 128)
            dma("sp", xs[b], x[rows, :], [], [("xs", b)])
            cp(xb[b], xs[b], [("xs", b)], [("xb", b)], eng="act")
            ptv = bank_bf(b).rearrange("p (c t) -> p c t", c=8)
            for c in range(8):
                tr(ptv[:, c, :], xb[b][:, c * 128:(c + 1) * 128], ident, [("xb", b)], [("pt", b)])
            cp(xT[b], ptv, [("pt", b)], [("xT", b)])
            dma(ST_ENG, HT[:, :, rows], xT[b], [("xT", b)], [("HT", t)], key=("xTst", b))


    def rope(dst, src, nh, dh, rt, off, hf, tA, tB, R, W, key):
        cc = rt[:, off:off + 2 * hf].unsqueeze(1).to_broadcast([128, nh, 2 * hf])
        s1 = rt[:, off + 2 * hf:off + 3 * hf].unsqueeze(1).to_broadcast([128, nh, hf])
        s2 = rt[:, off + 3 * hf:off + 4 * hf].unsqueeze(1).to_broadcast([128, nh, hf])
        tt(tA[:, 0:nh, 0:2 * hf], src[:, :, 0:2 * hf], cc, ALU.mult, R, [key + "A"])
        tt(tB[:, 0:nh, 0:hf], src[:, :, hf:2 * hf], s1, ALU.mult, R, [key + "B0"])
        tt(tB[:, 0:nh, hf:2 * hf], src[:, :, 0:hf], s2, ALU.mult, R, [key + "B1"])
        tt(dst[:, :, 0:2 * hf], tA[:, 0:nh, 0:2 * hf], tB[:, 0:nh, 0:2 * hf], ALU.add, [key + "A", key + "B0", key + "B1"], W)
        if dh > 2 * hf:
            cp(dst[:, :, 2 * hf:dh], src[:, :, 2 * hf:dh], R, W, eng="act")

    def diff_attention(h_src, h_dst, dbg_dst):
        phase("a1")
        wA = sb("wA", [128, 8, 3072], BF16)
        hts = [sb("hts%d" % i, [128, 8, 128], BF16) for i in range(2)]
        rts = [sb("rts%d" % i, [128, 128], F32) for i in range(2)]
        tok = [sb("tok%d" % i, [128, 16, 64], BF16) for i in range(2)]
        vtok = [sb("vtok%d" % i, [128, 1024], BF16) for i in range(2)]
        tA = sb("tA", [128, 16, 32], F32)
        tB = sb("tB", [128, 16, 32], F32)
        oT = [sb("oT%d" % i, [128, 8, 128], BF16) for i in range(2)]
        dma("sp", wA, Wb["a_w_in"].rearrange("(c p) n -> p c n", p=128), [], ["wA"])
        n = 0
        for t in range(NTL):
            b = t % 2
            rows = slice(t * 128, (t + 1) * 128)
            dma("sp", hts[b], HT[:, :, rows], [], [("hts", b)])
            dma("sp", rts[b], ROPE[rows, :], [], [("rts", b)])
            for which in range(3):
                pb = 2 * (n % 2)
                pa = bank(pb, 2)
                for hf in range(2):
                    for k in range(8):
                        mm(pa[:, hf * 512:(hf + 1) * 512], hts[b][:, k, :], wA[:, k, which * 1024 + hf * 512: which * 1024 + (hf + 1) * 512],
                           k == 0, k == 7, [("hts", b), "wA"], [("pa", pb, hf)])
                paR = [("pa", pb, 0), ("pa", pb, 1)]
                if which < 2:
                    tb_ = tok[n % 2]
                    rope(tb_, pa.rearrange("p (h d) -> p h d", h=16), 16, 64, rts[b], 0, 8, tA, tB, paR + [("rts", b)], [("tok", n % 2)], "r")
                    ptb = 4 + n % 2
                    ptv = bank_bf(ptb).rearrange("p (c t) -> p c t", c=8)
                    tf = tb_.rearrange("p h d -> p (h d)")
                    for c in range(8):
                        tr(ptv[:, c, :], tf[:, c * 128:(c + 1) * 128], ident, [("tok", n % 2)], [("pt", ptb)])
                    ob = oT[n % 2]
                    if which == 0:
                        act(ob, ptv, AF.Copy, [("pt", ptb)], [("oT", n % 2)], scale=0.125)
                    else:
                        cp(ob, ptv, [("pt", ptb)], [("oT", n % 2)])
                    dma(ST_ENG, (QT if which == 0 else KT)[:, :, rows], ob, [("oT", n % 2)], [("QK", which, t)], key=("oTst", n % 2))
                else:
                    cp(vtok[b], pa, paR, [("vtok", b)], eng="act")
                    dma(ST_ENG, V[rows, :], vtok[b], [("vtok", b)], [("V", t)], key=("vst", b))
                n += 1

        phase("a2")
        KTh = sb("KTh", [128, NT], BF16)
        QTh = sb("QTh", [128, NT], BF16)
        Vh = sb("Vh", [128, NTL, 128], BF16)
        pT = [[sb("pT%d_%d" % (c, i), [128, 512], BF16) for i in range(3)] for c in range(2)]
        r0 = sb("r0", [128, 512], F32); r1 = sb("r1", [128, 512], F32)
        a0 = sb("a0", [128, 512], F32); a1 = sb("a1", [128, 512], F32)
        od = sb("od", [128, 512], F32); sq = sb("sq", [128, 512], F32); rstd = sb("rstd", [128, 512], F32)
        on = [sb("on%d" % i, [128, 512], BF16) for i in range(2)]
        nst = 0
        npt = 0
        for h in range(8):
            dma("sp", KTh, KT[:, h, :], [], ["KTh"])
            dma("sp", QTh, QT[:, h, :], [], ["QTh"])
            dma("sp", Vh, V[:, h * 128:(h + 1) * 128].rearrange("(t p) e -> p t e", p=128), [], ["Vh"])
            for qb in range(NQB):
                nj = 4 * qb + 4
                for j in range(nj):
                    c0 = 0 if j < 4 * qb else 128 * (j - 4 * qb)
                    for c in range(2):
                        sbk = (nst % 2) * 2 + c
                        ps_ = bank(sbk)
                        pr = slice(c * 64, (c + 1) * 64)
                        mm(ps_[:, c0:512], KTh[pr, j * 128:(j + 1) * 128], QTh[pr, qb * 512 + c0:(qb + 1) * 512], True, True,
                           ["KTh", "QTh"], [("ps", sbk)])
                        pt_ = pT[c][npt % 3]
                        pk = ("pT", c, npt % 3)
                        act(pt_[:, c0:512], ps_[:, c0:512], AF.Exp, [("ps", sbk)], [pk])
                        if j >= 4 * qb:
                            memset(pt_[64:128, c0:c0 + 64], 0.0, [pk])
                        mm(bank(4 + c)[:, c0:512], Vh[:, j, :], pt_[:, c0:512], j == 0, j == nj - 1, ["Vh", pk], [("po", c)])
                        mm(bank(6 + c)[:, c0:512], ones_b, pt_[:, c0:512], j == 0, j == nj - 1, [pk], [("pl", c)])
                    nst += 1
                    npt += 1
                recip(r0, bank(6), [("pl", 0)], ["r0"])
                recip(r1, bank(7), [("pl", 1)], ["r1"])
                tt(a0, bank(4), r0, ALU.mult, [("po", 0), "r0"], ["a0"])
                tt(a1, bank(5), r1, ALU.mult, [("po", 1), "r1"], ["a1"])
                stt(od, a1, neglam, a0, ALU.mult, ALU.add, ["a0", "a1"], ["od"])
                act(sq, od, AF.Square, ["od"], ["sq"])
                sbk = (nst % 2) * 2
                nst += 1
                mm(bank(sbk), ones32, sq, True, True, ["sq"], [("ps", sbk)])
                act(rstd, bank(sbk), AF.Sqrt, [("ps", sbk)], ["rstd"], bias=epsc, scale=1.0 / 128.0)
                recip(rstd, rstd, ["rstd"], ["rstd"])
                ob = on[(h * NQB + qb) % 2]
                stt(ob, od, gsc, rstd, ALU.mult, ALU.mult, ["od", "rstd"], [("on", (h * NQB + qb) % 2)])
                dma(ST_ENG, OT[:, h, qb * 512:(qb + 1) * 512], ob, [("on", (h * NQB + qb) % 2)], [("OT", h, qb)], key=("onst", (h * NQB + qb) % 2))

        out_proj(Wb["a_w_out"], h_src, h_dst, 0, 0, dbg_dst)

    def out_proj(wdram, h_src, h_dst, li, lj, dbg_dst):
        phase("a3")
        wO = sb("wO", [128, 8, 1024], BF16)
        ots = [sb("ots%d" % i, [128, 8, 128], BF16) for i in range(2)]
        ep = Epi(6)
        dma("sp", wO, wdram.rearrange("(c p) n -> p c n", p=128), [], ["wO"])
        load_ln(li, lj)
        for t in range(NTL):
            b = t % 2
            rows = slice(t * 128, (t + 1) * 128)
            dma("sp", ots[b], OT[:, :, rows], [], [("ots", b)])
            pm = bank(2 * b, 2)
            for hf in range(2):
                for c in range(8):
                    mm(pm[:, hf * 512:(hf + 1) * 512], ots[b][:, c, :], wO[:, c, hf * 512:(hf + 1) * 512], c == 0, c == 7,
                       [("ots", b), "wO"], [("pm", b, hf)])
            ep.run(t, pm, [("pm", b, 0), ("pm", b, 1)], h_src, h_dst, dbg_dst)

    def cross_attention(li, h_src, h_dst, dbg_dst):
        phase("x%d" % li)
        wq = sb("wq", [128, 8, 1024], BF16)
        wo = sb("wo", [128, 8, 1024], BF16)
        hts = [sb("hts%d" % i, [128, 8, 128], BF16) for i in range(2)]
        qT = sb("qT", [128, 8, 128], BF16)
        nmx = sb("nmx", [128, 4], F32)
        lsum = sb("lsum", [128, 4], F32)
        P = sb("P", [128, 4, 256], BF16)
        Pn = sb("Pn", [128, 4, 256], BF16)
        PT = sb("PT", [128, 8, 128], BF16)
        oT = sb("oT", [128, 8, 128], BF16)
        ep = Epi(7)
        dma("sp", wq, Wb["xa_w_q"][li].rearrange("(c p) n -> p c n", p=128), [], ["wq"])
        dma("sp", wo, Wb["xa_w_out"][li].rearrange("(c p) n -> p c n", p=128), [], ["wo"])
        load_ln(li, 1)
        pA = bank(0, 2).rearrange("p (c t) -> p c t", c=8)
        psc = bank(2, 2).rearrange("p (h m) -> p h m", h=4)
        ptp = bank_bf(4).rearrange("p (c t) -> p c t", c=8)
        pmix = bank(5, 2)
        for t in range(NTL):
            b = t % 2
            rows = slice(t * 128, (t + 1) * 128)
            dma("sp", hts[b], HT[:, :, rows], [], [("hts", b)])
            for j in range(8):
                for k in range(8):
                    mm(pA[:, j, :], wq[:, k, j * 128:(j + 1) * 128], hts[b][:, k, :], k == 0, k == 7, ["wq", ("hts", b)], [("pA", j // 4)])
            act(qT, pA, AF.Copy, [("pA", 0), ("pA", 1)], ["qT"], scale=1.0 / 16.0)
            for hh in range(4):
                for dc in range(2):
                    mm(psc[:, hh, :], qT[:, 2 * hh + dc, :], memKT[:, 2 * hh + dc, :], dc == 0, dc == 1, ["qT"], [("psc", hh // 2)])
            red(nmx, psc, ALU.max, [("psc", 0), ("psc", 1)], ["nmx"], negate=True)
            for hh in range(4):
                act(P[:, hh, :], psc[:, hh, :], AF.Exp, [("psc", hh // 2), "nmx"], [("P", hh)], bias=nmx[:, hh:hh + 1], accum=lsum[:, hh:hh + 1])
            recip(lsum, lsum, [("P", hh) for hh in range(4)], ["lsum"])
            tt(Pn, P, lsum.unsqueeze(2).to_broadcast([128, 4, 256]), ALU.mult, [("P", hh) for hh in range(4)] + ["lsum"], ["Pn"])
            for hh in range(4):
                for mc in range(2):
                    tr(ptp[:, hh * 2 + mc, :], Pn[:, hh, mc * 128:(mc + 1) * 128], ident, ["Pn"], ["ptp"])
            cp(PT, ptp, ["ptp"], ["PT"])
            for hh in range(4):
                for dch in range(2):
                    for mc in range(2):
                        mm(pA[:, hh * 2 + dch, :], memV[:, mc, hh * 256 + dch * 128: hh * 256 + (dch + 1) * 128], PT[:, hh * 2 + mc, :],
                           mc == 0, mc == 1, ["PT"], [("pA", (hh * 2 + dch) // 4)])
            cp(oT, pA, [("pA", 0), ("pA", 1)], ["oT"], eng="act")
            for hf in range(2):
                for c in range(8):
                    mm(pmix[:, hf * 512:(hf + 1) * 512], oT[:, c, :], wo[:, c, hf * 512:(hf + 1) * 512], c == 0, c == 7, ["oT", "wo"], [("pmix", hf)])
            ep.run(t, pmix, [("pmix", 0), ("pmix", 1)], h_src, h_dst, dbg_dst)

    def moe(li, h_src, h_dst, dbg_dst):
        phase("m1_%d" % li)
        wr = sb("wr", [128, 8, 20], F32)
        whi = sb("whi", [128, 8, 20], BF16)
        wlo = sb("wlo", [128, 8, 20], BF16)
        rb = sb("rb", [128, 20], F32)
        LG = sb("LG", [128, NTL, 20], F32)
        hs = [sb("hs%d" % i, [128, 1024], F32) for i in range(2)]
        hhi = [sb("hhi%d" % i, [128, 1024], BF16) for i in range(2)]
        hlo = [sb("hlo%d" % i, [128, 1024], BF16) for i in range(2)]
        hiT = [sb("hiT%d" % i, [128, 8, 128], BF16) for i in range(2)]
        loT = [sb("loT%d" % i, [128, 8, 128], BF16) for i in range(2)]
        dma("sp", wr[:, :, 0:4], moe_w_group[li].rearrange("(c p) n -> p c n", p=128), [], [("wr", 0)], slow=True)
        dma("sp", wr[:, :, 4:20], moe_w_expert[li].rearrange("(c p) n -> p c n", p=128), [], [("wr", 1)], slow=True)
        dma("sp", rb[:, 0:4], moe_b_group[li].partition_broadcast(128), [], [("rb", 0)])
        dma("sp", rb[:, 4:20], moe_b_expert[li].partition_broadcast(128), [], [("rb", 1)])
        cp(whi, wr, [("wr", 0), ("wr", 1)], ["whi"])
        tt(wlo, wr, whi, ALU.subtract, [("wr", 0), ("wr", 1), "whi"], ["wlo"])
        for t in range(NTL):
            b = t % 2
            rows = slice(t * 128, (t + 1) * 128)
            dma("sp", hs[b], h_src[rows, :], [], [("hs", b)])
            cp(hhi[b], hs[b], [("hs", b)], [("hhi", b)], eng="act")
            tt(hlo[b], hs[b], hhi[b], ALU.subtract, [("hs", b), ("hhi", b)], [("hlo", b)])
            p1 = bank_bf(2 * b).rearrange("p (c t) -> p c t", c=8)
            p2 = bank_bf(2 * b + 1).rearrange("p (c t) -> p c t", c=8)
            for c in range(8):
                tr(p1[:, c, :], hhi[b][:, c * 128:(c + 1) * 128], ident, [("hhi", b)], [("p1", b)])
            for c in range(8):
                tr(p2[:, c, :], hlo[b][:, c * 128:(c + 1) * 128], ident, [("hlo", b)], [("p2", b)])
            cp(hiT[b], p1, [("p1", b)], [("hiT", b)], eng="act")
            cp(loT[b], p2, [("p2", b)], [("loT", b)])
            pl = bank(4 + b)[:, 0:20]
            n = 0
            for (aT, w_, ka, kw_) in ((hiT[b], whi, ("hiT", b), "whi"), (hiT[b], wlo, ("hiT", b), "wlo"), (loT[b], whi, ("loT", b), "whi")):
                for k in range(8):
                    mm(pl, aT[:, k, :], w_[:, k, :], n == 0, n == 23, [ka, kw_], [("pl", b)])
                    n += 1
            tt(LG[:, t, :], pl, rb, ALU.add, [("pl", b), ("rb", 0), ("rb", 1)], [("LG", t)])
        T = NTL
        G = LG[:, :, 0:4]
        E = LG[:, :, 4:20].rearrange("p t (g j) -> p t g j", g=4)
        LGR = [("LG", t) for t in range(NTL)]
        gmax = sb("gmax", [128, T], F32); goh = sb("goh", [128, T, 4], F32); ge = sb("ge", [128, T, 4], F32)
        gsum = sb("gsum", [128, T], F32); tmp4 = sb("tmp4", [128, T, 4, 4], F32); esel = sb("esel", [128, T, 4], F32)
        m1 = sb("m1", [128, T], F32); oh1 = sb("oh1", [128, T, 4], F32); e2 = sb("e2", [128, T, 4], F32)
        m2 = sb("m2", [128, T], F32); oh2 = sb("oh2", [128, T, 4], F32); dd = sb("dd", [128, T], F32)
        w1 = sb("w1", [128, T], F32); w2 = sb("w2", [128, T], F32); cin = sb("cin", [128, T, 4], F32); cin2 = sb("cin2", [128, T, 4], F32)

        def bc3(a):
            return a.unsqueeze(2).to_broadcast([128, T, 4])

        red(gmax, G, ALU.max, LGR, ["gmax"])
        tt(goh, G, bc3(gmax), ALU.is_equal, LGR + ["gmax"], ["goh"])
        tt(ge, G, bc3(gmax), ALU.subtract, LGR + ["gmax"], ["ge"])
        act(ge, ge, AF.Exp, ["ge"], ["ge"])
        red(gsum, ge, ALU.add, ["ge"], ["gsum"])
        recip(gsum, gsum, ["gsum"], ["gsum"])
        tt(tmp4, E, goh.unsqueeze(3).to_broadcast([128, T, 4, 4]), ALU.mult, LGR + ["goh"], ["tmp4"])
        red(esel, tmp4.rearrange("p t g j -> p t j g"), ALU.add, ["tmp4"], ["esel"])
        red(m1, esel, ALU.max, ["esel"], ["m1"])
        tt(oh1, esel, bc3(m1), ALU.is_equal, ["esel", "m1"], ["oh1"])
        stt(e2, oh1, -1e30, esel, ALU.mult, ALU.add, ["oh1", "esel"], ["e2"])
        red(m2, e2, ALU.max, ["e2"], ["m2"])
        tt(oh2, e2, bc3(m2), ALU.is_equal, ["e2", "m2"], ["oh2"])
        tt(dd, m2, m1, ALU.subtract, ["m1", "m2"], ["dd"])
        act(dd, dd, AF.Exp, ["dd"], ["dd"])
        ts(w1, dd, 1.0, None, ALU.add, None, ["dd"], ["w1"])
        recip(w1, w1, ["w1"], ["w1"])
        tt(w2, dd, w1, ALU.mult, ["dd", "w1"], ["w2"])
        tt(w1, w1, gsum, ALU.mult, ["w1", "gsum"], ["w1"])
        tt(w2, w2, gsum, ALU.mult, ["w2", "gsum"], ["w2"])
        tt(cin, oh1, bc3(w1), ALU.mult, ["oh1", "w1"], ["cin"])
        tt(cin2, oh2, bc3(w2), ALU.mult, ["oh2", "w2"], ["cin2"])
        tt(cin, cin, cin2, ALU.add, ["cin", "cin2"], ["cin"])
        tt(comb.rearrange("p t (g j) -> p t g j", g=4), goh.unsqueeze(3).to_broadcast([128, T, 4, 4]),
           cin.unsqueeze(2).to_broadcast([128, T, 4, 4]), ALU.mult, ["goh", "cin"], ["comb"])

        phase("m2_%d" % li)
        TB = 1024
        hTb = sb("hTb", [128, 8, TB], BF16)
        yacc = sb("yacc", [128, 8, 1024], F32)
        wg = [sb("wg%d" % i, [128, 8, 512], BF16) for i in range(2)]
        wu = [sb("wu%d" % i, [128, 8, 512], BF16) for i in range(2)]
        wd = [sb("wd%d" % i, [128, 4, 1024], BF16) for i in range(2)]
        sg = [sb("sg%d" % i, [128, 512], F32) for i in range(2)]
        hid = [sb("hid%d" % i, [128, 4, 512], BF16) for i in range(2)]
        ep = Epi(6)
        load_ln(li, 2)
        nh = 0
        ny = 0
        for blk in range(NT // TB):
            dma("sp", hTb, HT[:, :, blk * TB:(blk + 1) * TB], [], ["hTb"])
            memset(yacc, 0.0, ["yacc"] + [("yacc", tl, dh) for tl in range(8) for dh in range(2)])
            for e in range(16):
                wbuf = e % 2
                dma("sp", wg[wbuf], Wb["moe_w_gate"][li, e].rearrange("(c p) n -> p c n", p=128), [], [("wg", wbuf)])
                dma("sp", wu[wbuf], Wb["moe_w_up"][li, e].rearrange("(c p) n -> p c n", p=128), [], [("wu", wbuf)])
                dma("sp", wd[wbuf], Wb["moe_w_down"][li, e].rearrange("(c p) n -> p c n", p=128), [], [("wd", wbuf)])
                for half in range(TB // 512):
                    hb_ = hid[nh % 2]
                    hk = ("hid", nh % 2)
                    for fc in range(4):
                        pg = bank(0 + (fc % 2))
                        pu = bank(2 + (fc % 2))
                        for k in range(8):
                            mm(pg, wg[wbuf][:, k, fc * 128:(fc + 1) * 128], hTb[:, k, half * 512:(half + 1) * 512], k == 0, k == 7,
                               [("wg", wbuf), "hTb"], [("pg", fc % 2)])
                        for k in range(8):
                            mm(pu, wu[wbuf][:, k, fc * 128:(fc + 1) * 128], hTb[:, k, half * 512:(half + 1) * 512], k == 0, k == 7,
                               [("wu", wbuf), "hTb"], [("pu", fc % 2)])
                        act(sg[fc % 2], pg, AF.Silu, [("pg", fc % 2)], [("sg", fc % 2)])
                        tt(hb_[:, fc, :], sg[fc % 2], pu, ALU.mult, [("sg", fc % 2), ("pu", fc % 2)], [hk + (fc,)])
                    hR = [hk + (fc,) for fc in range(4)]
                    for tt_ in range(4):
                        tl = half * 4 + tt_
                        tg = blk * 8 + tl
                        for dh in range(2):
                            py = bank(4 + ny % 2)
                            for fc in range(4):
                                mm(py, hb_[:, fc, tt_ * 128:(tt_ + 1) * 128], wd[wbuf][:, fc, dh * 512:(dh + 1) * 512], fc == 0, fc == 3,
                                   hR + [("wd", wbuf)], [("py", ny % 2)])
                            ya = yacc[:, tl, dh * 512:(dh + 1) * 512]
                            stt(ya, py, comb[:, tg, e:e + 1], ya, ALU.mult, ALU.add, [("py", ny % 2), "yacc", ("yacc", tl, dh)], [("yacc", tl, dh)])
                            ny += 1
                    nh += 1
            for tl in range(8):
                ep.run(blk * 8 + tl, yacc[:, tl, :], [("yacc", tl, 0), ("yacc", tl, 1)], h_src, h_dst, dbg_dst)

    def dsa(h_src, h_dst, dbg_dst):
        phase("d1")
        wI = sb("wI", [128, 8, 624], BF16)
        wUQ = sb("wUQ", [128, 2, 1024], BF16)
        wQI = sb("wQI", [128, 2, 1024], BF16)
        wUKT = sb("wUKT", [128, 8, 256], BF16)
        gq = sb("gq", [128, 256], F32)
        gkv = sb("gkv", [128, 256], F32)
        hts = [sb("hts%d" % i, [128, 8, 128], BF16) for i in range(2)]
        rts = [sb("rts%d" % i, [128, 128], F32) for i in range(2)]
        sqj = sb("sqj", [128, 256], F32)
        ssq = sb("ssq", [128, 2], F32)
        cqn = sb("cqn", [128, 256], BF16)
        ckn = sb("ckn", [128, 256], BF16)
        cqT = sb("cqT", [128, 2, 128], BF16)
        kvTs = sb("kvTs", [128, 3, 128], BF16)
        krp = sb("krp", [128, 1, 128], BF16)
        kix = sb("kix", [128, 2, 64], BF16)
        kiT = sb("kiT", [128, 128], BF16)
        wix = sb("wix", [128, 16], F32)
        tA = sb("tA", [128, 16, 32], F32)
        tB = sb("tB", [128, 16, 32], F32)
        qtk = sb("qtk", [128, 8, 128], BF16)
        qiT = sb("qiT", [128, 8, 8, 16], BF16)
        qT = sb("qT", [128, 8, 128], BF16)
        qfT = sb("qfT", [128, 8, 3, 128], BF16)
        qiks = [sb("qik%d" % i, [128, 16, 64], BF16) for i in range(2)]
        dma("sp", wI, Wb["b_w_in"].rearrange("(c p) n -> p c n", p=128), [], ["wI"])
        dma("sp", wUQ, Wb["b_w_uq"].rearrange("(c p) n -> p c n", p=128), [], ["wUQ"])
        dma("sp", wQI, Wb["b_w_qidx"].rearrange("(c p) n -> p c n", p=128), [], ["wQI"])
        wukn = sb("wukn", [128, 8, 2, 128], BF16)
        memset(wukn, 0.0, ["wukn"])
        for rc in range(2):
            dma("sp", wukn[:, :, rc, 32:128], Wb["b_w_uk"][:, rc * 128:(rc + 1) * 128, :].rearrange("h p n -> p h n"), [], ["wukn", ("wukn", rc)], key=("wukn", rc))
        for hh in range(8):
            ptw = bank_bf(hh % 2).rearrange("p (c t) -> p c t", c=8)
            for rc in range(2):
                tr(ptw[:, rc, :], wukn[:, hh, rc, :], ident, ["wukn", ("wukn", 0), ("wukn", 1)], ["p%d" % (hh % 2)])
            cp(wUKT[:, hh, :].rearrange("p (c t) -> p c t", c=2), ptw[:, 0:2, :], ["p%d" % (hh % 2)], [("wUKT", hh)])
        wUKR = [("wUKT", hh) for hh in range(8)]
        dma("sp", gq, b_q_norm_g.partition_broadcast(128), [], ["gq"])
        dma("sp", gkv, b_kv_norm_g.partition_broadcast(128), [], ["gkv"])
        memset(kvTs, 0.0, ["kvTs"])
        memset(krp, 0.0, ["krp"])
        memset(qfT, 0.0, ["qfT"])
        sub(11)
        for t in range(NTL):
            b = t % 2
            rows = slice(t * 128, (t + 1) * 128)
            dma("sp", hts[b], HT[:, :, rows], [], [("hts", b)])
            dma("sp", rts[b], ROPE[rows, :], [], [("rts", b)])
            p0 = bank(0)
            p1 = bank(1)[:, 0:112]
            for k in range(8):
                mm(p0, hts[b][:, k, :], wI[:, k, 0:512], k == 0, k == 7, [("hts", b), "wI"], ["p0"])
            for k in range(8):
                mm(p1, hts[b][:, k, :], wI[:, k, 512:624], k == 0, k == 7, [("hts", b), "wI"], ["p1"])
            for i, (gg, dst, nm_) in enumerate(((gq, cqn, "cqn"), (gkv, ckn, "ckn"))):
                act(sqj, p0[:, i * 256:(i + 1) * 256], AF.Square, ["p0"], ["sqj"], accum=ssq[:, i:i + 1])
                act(ssq[:, i:i + 1], ssq[:, i:i + 1], AF.Sqrt, ["sqj"], [("ssq", i)], bias=epsc, scale=1.0 / 256.0)
                recip(ssq[:, i:i + 1], ssq[:, i:i + 1], [("ssq", i)], [("ssq", i)])
                stt(dst, p0[:, i * 256:(i + 1) * 256], ssq[:, i:i + 1], gg, ALU.mult, ALU.mult, ["p0", ("ssq", i), "gq", "gkv"], [nm_])
            dma(ST_ENG, CKV[rows, :], ckn, ["ckn"], [("CKV", t)], key="ckvst")
            sub(12)
            rope(krp[:, :, 0:32], p1[:, 0:32].rearrange("p (h d) -> p h d", h=1), 1, 32, rts[b], 32, 16, tA, tB, ["p1", ("rts", b)], ["krp"], "rk")
            rope(kix[:, 0:1, :], p1[:, 32:96].rearrange("p (h d) -> p h d", h=1), 1, 64, rts[b], 96, 8, tA, tB, ["p1", ("rts", b)], [("kix", 0)], "ri")
            cp(kix[:, 1:2, :], kix[:, 0:1, :], [("kix", 0)], [("kix", 1)])
            ts(wix, p1[:, 96:112], 1.0 / 32.0, None, ALU.mult, None, ["p1"], ["wix"])
            dma(ST_ENG, WIX[rows, :], wix, ["wix"], [("WIX", t)], key="wixst")
            sub(13)
            pt = bank_bf(2).rearrange("p (c t) -> p c t", c=8)
            for c in range(2):
                tr(pt[:, c, :], cqn[:, c * 128:(c + 1) * 128], ident, ["cqn"], ["pt2"])
            sub(1310)
            for c in range(2):
                tr(pt[:, 2 + c, :], ckn[:, c * 128:(c + 1) * 128], ident, ["ckn", ("CKV", t)], ["pt2"])
            sub(1311)
            tr(pt[:, 4, :], krp.rearrange("p h d -> p (h d)"), ident, ["krp"], ["pt2"])
            tr(pt[:, 5, :], kix.rearrange("p a d -> p (a d)"), ident, [("kix", 0), ("kix", 1)], ["pt2"])
            sub(131)
            cp(cqT, pt[:, 0:2, :], ["pt2"], ["cqT"])
            cp(kvTs[:, 0:2, :], pt[:, 2:4, :], ["pt2"], [("kvTs", 0)], eng="act")
            cp(kvTs[:, 2, :], pt[:, 4, :], ["pt2"], [("kvTs", 1)])
            cp(kiT, pt[:, 5, :], ["pt2"], ["kiT"], eng="act")
            sub(132)
            dma(ST_ENG, KVT[:, :, rows], kvTs, ["kvTs", ("kvTs", 0), ("kvTs", 1)], [("KVT", t)], key="kvtst")
            sub(133)
            dma(ST_ENG, KIT[:, rows], kiT, ["kiT"], [("KIT", t)], key="kitst")
            sub(14)
            pq = bank(3, 2)
            for hf in range(2):
                for c in range(2):
                    mm(pq[:, hf * 512:(hf + 1) * 512], cqT[:, c, :], wUQ[:, c, hf * 512:(hf + 1) * 512], c == 0, c == 1, ["cqT", "wUQ"], [("pq", hf)])
            rope(qtk, pq.rearrange("p (h d) -> p h d", h=8), 8, 128, rts[b], 32, 16, tA, tB, [("pq", 0), ("pq", 1), ("rts", b)], ["qtk"], "rq")
            sub(15)
            pqi = bank(5, 2)
            for hf in range(2):
                for c in range(2):
                    mm(pqi[:, hf * 512:(hf + 1) * 512], cqT[:, c, :], wQI[:, c, hf * 512:(hf + 1) * 512], c == 0, c == 1, ["cqT", "wQI"], [("pqi", hf)])
            qik = qiks[b]
            rope(qik, pqi.rearrange("p (h d) -> p h d", h=16), 16, 64, rts[b], 96, 8, tA, tB, [("pqi", 0), ("pqi", 1), ("rts", b)], [("qik", b)], "rqi")
            sub(16)
            pt7 = bank_bf(7).rearrange("p (c t) -> p c t", c=8)
            qf = qtk.rearrange("p h d -> p (h d)")
            for hh in range(8):
                tr(pt7[:, hh, :], qf[:, hh * 128:(hh + 1) * 128], ident, ["qtk"], ["pt7"])
            cp(qT, pt7, ["pt7"], ["qT"])
            qif = qik.rearrange("p h d -> p (h d)")
            for c in range(8):
                tr(pt7[:, c, :], qif[:, c * 128:(c + 1) * 128], ident, [("qik", b)], ["pt7"])
            cp(qiT.rearrange("p g j q -> p j g q"), pt7.rearrange("p j (g q) -> p j g q", g=8), ["pt7"], ["qiT"])
            dma(ST_ENG, QIT[:, t, :], qiT.rearrange("p g j q -> p (g j q)"), ["qiT"], [("QIT", t)], key="qitst")
            sub(17)
            sc = 1.0 / math.sqrt(128.0)
            pl_ = bank(0, 2).rearrange("p (c t) -> p c t", c=8)
            for half in range(2):
                for hh4 in range(4):
                    hh = half * 4 + hh4
                    for rc in range(2):
                        mm(pl_[:, hh4 * 2 + rc, :], wUKT[:, hh, rc * 128:(rc + 1) * 128], qT[:, hh, :], True, True,
                           wUKR + ["qT"], ["p0" if (hh4 * 2 + rc) < 4 else "p1"])
                for hh4 in range(4):
                    hh = half * 4 + hh4
                    act(qfT[:, hh, 0:2, :], pl_[:, hh4 * 2:hh4 * 2 + 2, :], AF.Copy, ["p0", "p1"], [("qfT", hh)], scale=sc)
            act(qfT[0:32, :, 2, :], qT[0:32, :, :], AF.Copy, ["qT"], [("qfTr")], scale=sc)
            dma(ST_ENG, QFT[:, :, :, rows], qfT, ["qfT", "qfTr"] + [("qfT", hh) for hh in range(8)], [("QFT", t)], key="qftst")

        phase("d2")
        KIs = sb("KIs", [128, NT], BF16)
        qis = [sb("qis%d" % i, [128, 1024], BF16) for i in range(2)]
        wxs = [sb("wxs%d" % i, [128, 16], F32) for i in range(2)]
        wsel = sb("wsel", [128, 8, 16], F32)
        wc = sb("wc", [128, 8, 2], F32)
        Wblk = [sb("Wblk%d" % i, [128, 16, 128], BF16) for i in range(2)]
        Rb = [sb("Rb%d" % i, [128, 512], BF16) for i in range(4)]
        sc_ = [sb("sc%d" % i, [128, NT], F32) for i in range(2)]
        junk = sb("junk", [128, NT], BF16)
        nmk = [sb("nmk%d" % i, [128, NT], BF16) for i in range(2)]
        lo = [sb("lo%d" % i, [128, 1], F32) for i in range(2)]
        hi = [sb("hi%d" % i, [128, 1], F32) for i in range(2)]
        stp = [sb("stp%d" % i, [128, 20], F32) for i in range(2)]
        mid = [sb("mid%d" % i, [128, 1], F32) for i in range(2)]
        cnt = [sb("cnt%d" % i, [128, 1], F32) for i in range(2)]
        geb = [sb("geb%d" % i, [128, 1], F32) for i in range(2)]
        NIT = 18
        hmask = cf[:, CF_HM:CF_HM + 16]
        dma("sp", KIs, KIT, [], ["KIs"])
        nlg = 0
        for t in range(NTL):
            b = t % 2
            rows = slice(t * 128, (t + 1) * 128)
            nk = 128 * (t + 1)
            dma("sp", qis[b], QIT[:, t, :], [], [("qis", b)])
            dma("sp", wxs[b], WIX[rows, :], [], [("wxs", b)])
            pw = bank(6)[:, 0:128].rearrange("p (g h) -> p g h", g=8)
            for g in range(8):
                mm(pw[:, g, :], cf[:, CF_SEL + g * 128:CF_SEL + (g + 1) * 128], wxs[b], True, True, [("wxs", b)], ["pw"])
            tt(wsel, pw, hmask.unsqueeze(1).to_broadcast([128, 8, 16]), ALU.mult, ["pw"], ["wsel"])
            red(wc, wsel.rearrange("p g (j r) -> p g r j", r=2), ALU.add, ["wsel"], ["wc"])
            for g in range(8):
                for par in range(2):
                    ts(Wblk[b][:, g * 2 + par, :], cb[:, CB_E + g * 128:CB_E + (g + 1) * 128], wc[:, g, par:par + 1], None, ALU.mult, None,
                       ["wc"], [("Wblk", b, g * 2 + par)], eng="pool")
            WR = [("Wblk", b, i) for i in range(16)]
            nkb = (nk + 511) // 512
            for kb in range(nkb):
                kw = min(512, nk - kb * 512)
                ks = slice(kb * 512, kb * 512 + kw)
                pscore = bank(4 + kb % 2)
                for g in range(8):
                    for par in range(2):
                        i = g * 2 + par
                        plg = bank(nlg % 4)
                        pr = slice(par * 64, (par + 1) * 64)
                        mm(plg[:, 0:kw], qis[b][pr, g * 128:(g + 1) * 128], KIs[pr, ks], True, True, [("qis", b), "KIs"], [("plg", nlg % 4)])
                        rb_ = Rb[nlg % 4]
                        if nlg % 2 == 0:
                            act(rb_[:, 0:kw], plg[:, 0:kw], AF.Relu, [("plg", nlg % 4)], [("Rb", nlg % 4)])
                        else:
                            ts(rb_[:, 0:kw], plg[:, 0:kw], 0.0, None, ALU.max, None, [("plg", nlg % 4)], [("Rb", nlg % 4)])
                        mm(pscore[:, 0:kw], Wblk[b][:, i, :], rb_[:, 0:kw], i == 0, i == 15, WR + [("Rb", nlg % 4)], [("pscore", kb % 2)])
                        nlg += 1
                cp(sc_[b][:, ks], pscore[:, 0:kw], [("pscore", kb % 2)], [("sc", b, kb)], eng="act" if kb % 2 else "dve")
            scR = [("sc", b, kb) for kb in range(nkb)]
            scK = ("scall", b)
            memset(sc_[b][0:64, t * 128 + 64:(t + 1) * 128], -1e30, [scK], R=scR)
            sv = sc_[b][:, 0:nk]
            if t >= 2:
                red(lo[b], sc_[b][:, 0:nk - 64], ALU.min, scR + [scK], [("lo", b)])
                red(hi[b], sv, ALU.max, scR + [scK], [("hi", b)])
                tt(mid[b], hi[b], lo[b], ALU.subtract, [("lo", b), ("hi", b)], [("mid", b)])
                for i in range(NIT):
                    ts(stp[b][:, i:i + 1], mid[b], 2.0 ** -(i + 1), None, ALU.mult, None, [("mid", b)], [("stp", b, i)], eng="pool")
                for i in range(NIT):
                    tt(mid[b], lo[b], stp[b][:, i:i + 1], ALU.add, [("lo", b), ("stp", b, i)], [("mid", b)])
                    ts(junk[:, 0:nk], sv, mid[b], 0.0, ALU.is_ge, ALU.add, scR + [scK, ("mid", b)], ["junk", ("cnt", b)], accum=cnt[b])
                    ts(geb[b], cnt[b], 255.5, stp[b][:, i:i + 1], ALU.is_ge, ALU.mult, [("cnt", b), ("stp", b, i)], [("geb", b)])
                    tt(lo[b], lo[b], geb[b], ALU.add, [("lo", b), ("geb", b)], [("lo", b)])
            else:
                memset(lo[b], -1e29, [("lo", b)], eng="dve")
            ts(nmk[b][:, 0:nk], sv, lo[b], NEG, ALU.is_lt, ALU.mult, scR + [scK, ("lo", b)], [("nmk", b)])
            dma(ST_ENG, NM[rows, 0:nk], nmk[b][:, 0:nk], [("nmk", b)], [("NM", t)], key=("nmst", b))

        phase("d3")
        KVs = sb("KVs", [128, 3, NT], BF16)
        CKs = sb("CKs", [128, NTL, 256], BF16)
        wUV = sb("wUV", [128, 8, 2, 128], BF16)
        qfs = [sb("qfs%d" % i, [128, 8, 3, 512], BF16) for i in range(2)]
        nms = [sb("nms%d" % i, [128, 4, 128], BF16) for i in range(3)]
        pT = [sb("pT%d" % i, [128, 512], BF16) for i in range(3)]
        rl = sb("rl", [128, 512], F32)
        olat = sb("olat", [128, 2, 512], BF16)
        on = [sb("on%d" % i, [128, 512], BF16) for i in range(2)]
        dma("sp", KVs, KVT, [], ["KVs"])
        dma("sp", CKs, CKV.rearrange("(t p) r -> p t r", p=128), [], ["CKs"])
        for hh in range(8):
            dma("sp", wUV[:, hh, :, :], Wb["b_w_uv"][hh].rearrange("(c p) v -> p c v", p=128), [], [("wUV", hh)])
        nst = 0
        for qb in range(NQB):
            qbuf = qb % 2
            dma("sp", qfs[qbuf], QFT[:, :, :, qb * 512:(qb + 1) * 512], [], [("qfs", qbuf)])
            nj = 4 * qb + 4
            for hh in range(8):
                for j in range(nj):
                    c0 = 0 if j < 4 * qb else 128 * (j - 4 * qb)
                    s0 = c0 // 128
                    nb_ = nms[nst % 3]
                    nk_ = ("nms", nst % 3)
                    dma("sp", nb_[:, s0:4, :], NM[qb * 512 + c0:(qb + 1) * 512, j * 128:(j + 1) * 128].rearrange("(s p) k -> p s k", p=128),
                        [], [nk_])
                    ps_ = bank(nst % 2)
                    mm(ps_[:, c0:512], KVs[:, 0, j * 128:(j + 1) * 128], qfs[qbuf][:, hh, 0, c0:512], True, False, ["KVs", ("qfs", qbuf)], [("ps", nst % 2)])
                    mm(ps_[:, c0:512], KVs[:, 1, j * 128:(j + 1) * 128], qfs[qbuf][:, hh, 1, c0:512], False, False, ["KVs", ("qfs", qbuf)], [("ps", nst % 2)])
                    mm(ps_[:, c0:512], KVs[:, 2, j * 128:(j + 1) * 128], qfs[qbuf][:, hh, 2, c0:512], False, False, ["KVs", ("qfs", qbuf)], [("ps", nst % 2)])
                    for s in range(s0, 4):
                        mm(ps_[:, s * 128:(s + 1) * 128], nb_[:, s, :], ident, False, s == 3, [nk_], [("ps", nst % 2)])
                    pt_ = pT[nst % 3]
                    pk = ("pT", nst % 3)
                    act(pt_[:, c0:512], ps_[:, c0:512], AF.Exp, [("ps", nst % 2)], [pk])
                    for rc in range(2):
                        mm(bank(2 + rc)[:, c0:512], CKs[:, j, rc * 128:(rc + 1) * 128], pt_[:, c0:512], j == 0, j == nj - 1, ["CKs", pk], [("po", rc)])
                    mm(bank(4)[:, c0:512], ones_b, pt_[:, c0:512], j == 0, j == nj - 1, [pk], ["pl"])
                    nst += 1
                recip(rl, bank(4), ["pl"], ["rl"])
                for rc in range(2):
                    tt(olat[:, rc, :], bank(2 + rc), rl, ALU.mult, [("po", rc), "rl"], [("olat", rc)])
                pv = bank(5 + hh % 2)
                for rc in range(2):
                    mm(pv, wUV[:, hh, rc, :], olat[:, rc, :], rc == 0, rc == 1, [("wUV", hh), ("olat", 0), ("olat", 1)], [("pv", hh % 2)])
                ob = on[hh % 2]
                cp(ob, pv, [("pv", hh % 2)], [("on", hh % 2)], eng="act")
                dma(ST_ENG, OT[:, hh, qb * 512:(qb + 1) * 512], ob, [("on", hh % 2)], [("OT", hh, qb)], key=("onst", hh % 2))

        out_proj(Wb["b_w_out"], h_src, h_dst, 1, 0, dbg_dst)

    try:
        p0()
        t0()
        diff_attention(x, H[0], dbg.get("h0_0"))
        cross_attention(0, H[0], H[1], dbg.get("h0_1"))
        moe(0, H[1], H[0], dbg.get("h0_2"))
        dsa(H[0], H[1], dbg.get("h1_0"))
        cross_attention(1, H[1], H[0], dbg.get("h1_1"))
        moe(1, H[0], out, None)
    except StopBuild:
        pass
    S.barrier()
    S.emit()
    st.close()
    return nc, S


_CACHE = {}


def make_in_maps(inputs, NT, ncores):
    cf, cb = _consts()
    maps = []
    f = lambda a: np.ascontiguousarray(a, dtype=np.float32)
    for b in range(ncores):
        m = {
            "x": f(inputs["x"][b, :NT]), "mem": f(inputs["mem"][b]),
            "positions": np.ascontiguousarray(inputs["positions"][b, :NT].reshape(NT // 128, 128).astype(np.int32)),
            "a_w_in": f(inputs["a_w_in"][0]), "a_lambda": f(inputs["a_lambda"][0]),
            "a_subln_g": f(inputs["a_subln_g"][0].reshape(128, 1)), "a_w_out": f(inputs["a_w_out"][0]),
            "b_w_in": f(inputs["b_w_in"][0]), "b_q_norm_g": f(inputs["b_q_norm_g"][0]), "b_kv_norm_g": f(inputs["b_kv_norm_g"][0]),
            "b_w_uq": f(inputs["b_w_uq"][0]), "b_w_qidx": f(inputs["b_w_qidx"][0]), "b_w_uk": f(inputs["b_w_uk"][0]),
            "b_w_uv": f(inputs["b_w_uv"][0]), "b_w_out": f(inputs["b_w_out"][0]),
            "mem_w_kv": f(inputs["mem_w_kv"]), "xa_w_q": f(inputs["xa_w_q"]), "xa_w_out": f(inputs["xa_w_out"]),
            "moe_w_group": f(inputs["moe_w_group"]), "moe_b_group": f(inputs["moe_b_group"]),
            "moe_w_expert": f(inputs["moe_w_expert"]), "moe_b_expert": f(inputs["moe_b_expert"]),
            "moe_w_gate": f(inputs["moe_w_gate"]), "moe_w_up": f(inputs["moe_w_up"]), "moe_w_down": f(inputs["moe_w_down"]),
            "ln_g": f(inputs["ln_g"]), "ln_b": f(inputs["ln_b"]),
            "cstf": cf, "cstb": cb,
        }
        maps.append(m)
    return maps


def kernel(**inputs):
    NT = inputs["x"].shape[1]
    nb = inputs["x"].shape[0]
    if NT not in _CACHE:
        _CACHE[NT] = build(NT)[0]
    nc = _CACHE[NT]
    maps = make_in_maps(inputs, NT, nb)
    res = run_bass_kernel_spmd(nc, maps, core_ids=list(range(nb)))
    return np.stack([np.asarray(r["out"], dtype=np.float32) for r in res.results], axis=0)
```

```python
import math
import contextlib
import numpy as np
import ml_dtypes
import concourse.bass as bass
import concourse.mybir as mybir
from concourse.bass_utils import run_bass_kernel_spmd

F32 = mybir.dt.float32
BF16 = mybir.dt.bfloat16
I32 = mybir.dt.int32
U8 = mybir.dt.uint8
AF = mybir.ActivationFunctionType
ALU = mybir.AluOpType
AX = mybir.AxisListType

D = 1024
DEPTH = 2
ALPHA = (2.0 * DEPTH) ** 0.25
LN_EPS = 1e-5
ROPE_THETA = 500000.0
NEG = -30000.0
ENGS = ("pe", "act", "dve", "pool", "sp")
NPOOL = 88
ST_ENG = "sp"


PSUM_NAMES = {"ps0", "psb", "pk", "pv", "pt", "pa", "ps", "po", "pl", "pm", "pA", "psc", "ptp", "pmix", "p1", "p2", "pg", "pu",
              "py", "e_pt", "p0", "pt2", "pq", "pqi", "pt7", "pw", "pscore", "plg"}


def is_psum(key):
    base = key if isinstance(key, str) else key[0]
    return base in PSUM_NAMES


class Op:
    __slots__ = ("eng", "fn", "is_dma", "deps", "sem", "val", "signal", "waits", "semidx")

    def __init__(self, eng, fn, is_dma, semidx):
        self.eng = eng
        self.fn = fn
        self.is_dma = is_dma
        self.deps = []
        self.sem = None
        self.val = 0
        self.signal = False
        self.waits = []
        self.semidx = semidx


class Sched:
    def __init__(self, nc):
        self.nc = nc
        self.ops = []
        self.last_write = {}
        self.reads_since = {}
        self.keymap = {}
        self.dmas_since = []
        self.last_on = {}

    def add(self, eng, fn, reads=(), writes=(), dma=False, semkey=None):
        semidx = None
        if dma:
            if semkey is None:
                semkey = writes[0]
            if semkey not in self.keymap:
                assert len(self.keymap) < NPOOL, "too many dma sem keys in phase"
                self.keymap[semkey] = len(self.keymap)
            semidx = self.keymap[semkey]
        op = Op(eng, fn, dma, semidx)
        reads_eff = [r for r in reads if not is_psum(r)]
        writes_eff = list(writes) + [r for r in reads if is_psum(r)]
        deps = {}
        for r in reads_eff:
            for w in self.last_write.get(r, {}).values():
                deps[id(w)] = (w, "raw")
        for r in writes_eff:
            for w in self.last_write.get(r, {}).values():
                if id(w) not in deps:
                    deps[id(w)] = (w, "waw")
            for rd in self.reads_since.get(r, ()):
                if id(rd) not in deps:
                    deps[id(rd)] = (rd, "war")
        for d, kind in deps.values():
            if (not d.is_dma) and (not dma) and d.eng == eng:
                if eng == "pe":
                    continue
                if kind != "raw" and eng != "pool":
                    continue
            op.deps.append(d)
        for r in reads_eff:
            self.reads_since.setdefault(r, []).append(op)
        for r in writes_eff:
            self.last_write.setdefault(r, {})["dma" if dma else eng] = op
            self.reads_since[r] = []
        self.ops.append(op)
        if dma:
            self.dmas_since.append(op)
        else:
            self.last_on[eng] = op
        return op

    def barrier(self):
        lasts = [o for o in self.last_on.values() if o.fn is not None] + list(self.dmas_since)
        for e in ENGS:
            op = Op(e, None, False, None)
            op.deps = list(lasts)
            self.ops.append(op)
        self.last_write = {}
        self.reads_since = {}
        self.keymap = {}
        self.dmas_since = []
        self.last_on = {}

    def emit(self):
        nc = self.nc
        for op in self.ops:
            for d in op.deps:
                d.signal = True
        stack = contextlib.ExitStack()
        eng_sem = {e: stack.enter_context(nc.semaphore("s_" + e)) for e in ENGS}
        npool = max([o.semidx for o in self.ops if o.is_dma] + [0]) + 1
        pool = [stack.enter_context(nc.semaphore("d_%d" % i)) for i in range(npool)]
        counts = {}
        for op in self.ops:
            if op.is_dma:
                k = ("d", op.semidx)
                op.sem = pool[op.semidx]
                counts[k] = counts.get(k, 0) + 16
                op.val = counts[k]
                op.signal = True
            else:
                op.sem = eng_sem[op.eng]
                if op.signal and op.fn is not None:
                    counts[op.eng] = counts.get(op.eng, 0) + 1
                op.val = counts.get(op.eng, 0)
        waited = {e: {} for e in ENGS}
        per_eng = {e: [] for e in ENGS}
        nw = 0
        for op in self.ops:
            w = waited[op.eng]
            need = {}
            for d in op.deps:
                key = id(d.sem)
                if w.get(key, 0) >= d.val:
                    continue
                if key not in need or need[key][1] < d.val:
                    need[key] = (d.sem, d.val)
            for key, (sem, val) in need.items():
                w[key] = val
                op.waits.append((sem, val))
            nw += len(op.waits)
            per_eng[op.eng].append(op)
        self.stats = {e: len(per_eng[e]) for e in ENGS}
        self.stats["waits"] = nw
        self.stats["maxsem"] = max(counts.values()) if counts else 0

        def run(eng_obj, lst):
            for op in lst:
                for sem, val in op.waits:
                    eng_obj.wait_ge(sem, val)
                if op.fn is None:
                    continue
                ins = op.fn(eng_obj)
                if op.signal:
                    ins.then_inc(op.sem, 16 if op.is_dma else 1)

        with nc.Block() as block:
            @block.tensor
            def _(e):
                run(e, per_eng["pe"])

            @block.scalar
            def _(e):
                run(e, per_eng["act"])

            @block.vector
            def _(e):
                run(e, per_eng["dve"])

            @block.gpsimd
            def _(e):
                run(e, per_eng["pool"])

            @block.sync
            def _(e):
                run(e, per_eng["sp"])
        stack.close()


def _inv_freq(rot):
    return (np.float32(ROPE_THETA) ** (-np.arange(0, rot, 2, dtype=np.float32) / np.float32(rot))).astype(np.float32)


CF_ID = 0
CF_ONES = 128
CF_INVF = 256
CF_SEL = 288
CF_HM = 288 + 1024
CF_N = CF_HM + 16
CB_ID = 0
CB_ONES = 128
CB_E = 256
CB_N = 256 + 1024


def _consts():
    cf = np.zeros((128, CF_N), np.float32)
    cf[:, CF_ID:CF_ID + 128] = np.eye(128, dtype=np.float32)
    cf[:, CF_ONES:CF_ONES + 128] = 1.0
    cf[:, CF_INVF:CF_INVF + 32] = np.concatenate([_inv_freq(16), _inv_freq(32), _inv_freq(16)])[None, :]
    cb = np.zeros((128, CB_N), np.float32)
    cb[:, CB_ID:CB_ID + 128] = np.eye(128, dtype=np.float32)
    cb[:, CB_ONES:CB_ONES + 128] = 1.0
    p = np.arange(128)
    j, q = p // 16, p % 16
    for g in range(8):
        cf[16 * g + q, CF_SEL + g * 128 + p] = 1.0
        cb[p, CB_E + g * 128 + 16 * g + q] = 1.0
    for h in range(16):
        cf[:, CF_HM + h] = (h // 2 == j).astype(np.float32)
    return cf, cb.astype(ml_dtypes.bfloat16)


class B:
    pass


class StopBuild(Exception):
    pass


def build(NT, debug=(), upto=99):
    NTL = NT // 128
    NQB = NT // 512
    nc = bass.Bass("TRN2", target_bir_lowering=False)
    S = Sched(nc)
    st = contextlib.ExitStack()

    def din(name, shape, dt=F32):
        return nc.dram_tensor(name, list(shape), dt, kind="ExternalInput").ap()

    def dscr(name, shape, dt):
        return nc.dram_tensor(name, list(shape), dt, kind="Internal").ap()

    x = din("x", [NT, D])
    mem = din("mem", [256, D])
    positions = din("positions", [NTL, 128], I32)
    a_w_in = din("a_w_in", [D, 3072]); a_lambda = din("a_lambda", [4, 64]); a_subln_g = din("a_subln_g", [128, 1])
    a_w_out = din("a_w_out", [D, D])
    b_w_in = din("b_w_in", [D, 624]); b_q_norm_g = din("b_q_norm_g", [256]); b_kv_norm_g = din("b_kv_norm_g", [256])
    b_w_uq = din("b_w_uq", [256, 1024]); b_w_qidx = din("b_w_qidx", [256, 1024])
    b_w_uk = din("b_w_uk", [8, 256, 96]); b_w_uv = din("b_w_uv", [8, 256, 128]); b_w_out = din("b_w_out", [D, D])
    mem_w_kv = din("mem_w_kv", [D, 2048]); xa_w_q = din("xa_w_q", [2, D, D]); xa_w_out = din("xa_w_out", [2, D, D])
    moe_w_group = din("moe_w_group", [2, D, 4]); moe_b_group = din("moe_b_group", [2, 4])
    moe_w_expert = din("moe_w_expert", [2, D, 16]); moe_b_expert = din("moe_b_expert", [2, 16])
    moe_w_gate = din("moe_w_gate", [2, 16, D, 512]); moe_w_up = din("moe_w_up", [2, 16, D, 512])
    moe_w_down = din("moe_w_down", [2, 16, 512, D])
    ln_g = din("ln_g", [2, 3, D]); ln_b = din("ln_b", [2, 3, D])
    cstf = din("cstf", [128, CF_N]); cstb = din("cstb", [128, CB_N], BF16)
    out = nc.dram_tensor("out", [NT, D], F32, kind="ExternalOutput").ap()
    dbg = {k: nc.dram_tensor("dbg_" + k, [NT, D], F32, kind="ExternalOutput").ap() for k in debug}

    Wb = {
        "a_w_in": dscr("wb_a_w_in", [D, 3072], BF16), "a_w_out": dscr("wb_a_w_out", [D, D], BF16),
        "b_w_in": dscr("wb_b_w_in", [D, 624], BF16), "b_w_uq": dscr("wb_b_w_uq", [256, 1024], BF16),
        "b_w_qidx": dscr("wb_b_w_qidx", [256, 1024], BF16), "b_w_uk": dscr("wb_b_w_uk", [8, 256, 96], BF16),
        "b_w_uv": dscr("wb_b_w_uv", [8, 256, 128], BF16), "b_w_out": dscr("wb_b_w_out", [D, D], BF16),
        "mem_w_kv": dscr("wb_mem_w_kv", [D, 2048], BF16), "xa_w_q": dscr("wb_xa_w_q", [2, D, D], BF16),
        "xa_w_out": dscr("wb_xa_w_out", [2, D, D], BF16),
        "moe_w_gate": dscr("wb_moe_w_gate", [2, 16, D, 512], BF16), "moe_w_up": dscr("wb_moe_w_up", [2, 16, D, 512], BF16),
        "moe_w_down": dscr("wb_moe_w_down", [2, 16, 512, D], BF16),
    }
    H = [dscr("H0", [NT, D], F32), dscr("H1", [NT, D], F32)]
    HT = dscr("HT", [128, 8, NT], BF16)
    QT = dscr("QT", [128, 8, NT], BF16)
    KT = dscr("KT", [128, 8, NT], BF16)
    V = dscr("V", [NT, D], BF16)
    OT = dscr("OT", [128, 8, NT], BF16)
    ROPE = dscr("ROPE", [NT, 128], F32)
    KVT = dscr("KVT", [128, 3, NT], BF16)
    CKV = dscr("CKV", [NT, 256], BF16)
    KIT = dscr("KIT", [128, NT], BF16)
    QFT = dscr("QFT", [128, 8, 3, NT], BF16)
    QIT = dscr("QIT", [128, NTL, 1024], BF16)
    WIX = dscr("WIX", [NT, 16], F32)
    NM = dscr("NM", [NT, NT], BF16)

    ARENA = 200 * 1024
    arena = st.enter_context(nc.sbuf_tensor("arena", [128, ARENA], U8))
    PS = st.enter_context(nc.psum_tensor("ps", [128, 4096], F32))
    state = {"off": 0, "mark": 0, "phase": "p0"}

    def sb(name, shape, dt):
        esz = 4 if dt in (F32, I32) else 2
        n = int(np.prod(shape[1:])) * esz
        n = (n + 63) // 64 * 64
        off = state["off"]
        assert off + n <= ARENA, "arena overflow in %s: %s" % (state["phase"], name)
        state["off"] = off + n
        v = arena[:, off:off + n].bitcast(dt)[:, 0:int(np.prod(shape[1:]))]
        if len(shape) == 3:
            v = v.rearrange("p (a b) -> p a b", a=shape[1])
        elif len(shape) == 4:
            v = v.rearrange("p (a b c) -> p a b c", a=shape[1], b=shape[2])
        return v

    def bank(i, n=1):
        return PS[:, i * 512:(i + n) * 512]

    def bank_bf(i):
        return PS[:, i * 512:(i + 1) * 512].bitcast(BF16)

    def phase(name):
        state["np"] = state.get("np", 0) + 1
        if upto >= 0 and state["np"] > upto:
            raise StopBuild()
        S.barrier()
        state["off"] = state["mark"]
        state["phase"] = name

    def mm(o, lhsT, rhs, start, stop, R, W):
        S.add("pe", lambda e: e.matmul(o, lhsT=lhsT, rhs=rhs, start=start, stop=stop), reads=R, writes=W)

    def tr(o, i, ident, R, W):
        S.add("pe", lambda e: e.transpose(out=o, in_=i, identity=ident), reads=R, writes=W)

    def act(o, i, func, R, W, bias=None, scale=None, accum=None, eng="act"):
        kw = {}
        if bias is not None:
            kw["bias"] = bias
        if scale is not None:
            kw["scale"] = scale
        if accum is not None:
            kw["accum_out"] = accum
        S.add("act", lambda e: e.activation(out=o, in_=i, func=func, **kw), reads=R, writes=W)

    def tt(o, a, b, op, R, W, eng="dve"):
        S.add(eng, lambda e: e.tensor_tensor(out=o, in0=a, in1=b, op=op), reads=R, writes=W)

    def ts(o, a, s1, s2, op0, op1, R, W, accum=None, eng="dve"):
        kw = {}
        if accum is not None:
            kw["accum_out"] = accum
        if op1 is None:
            S.add(eng, lambda e: e.tensor_scalar(out=o, in0=a, scalar1=s1, scalar2=None, op0=op0, **kw), reads=R, writes=W)
        else:
            S.add(eng, lambda e: e.tensor_scalar(out=o, in0=a, scalar1=s1, scalar2=s2, op0=op0, op1=op1, **kw), reads=R, writes=W)

    def stt(o, a, s, b, op0, op1, R, W):
        S.add("dve", lambda e: e.scalar_tensor_tensor(out=o, in0=a, scalar=s, in1=b, op0=op0, op1=op1), reads=R, writes=W)

    def cp(o, i, R, W, eng="dve"):
        if eng == "act":
            S.add("act", lambda e: e.activation(out=o, in_=i, func=AF.Copy), reads=R, writes=W)
        else:
            S.add(eng, lambda e: e.tensor_copy(out=o, in_=i), reads=R, writes=W)

    def red(o, i, op, R, W, negate=False):
        S.add("dve", lambda e: e.tensor_reduce(out=o, in_=i, axis=AX.X, op=op, negate=negate), reads=R, writes=W)

    def recip(o, i, R, W):
        S.add("dve", lambda e: e.reciprocal(out=o, in_=i), reads=R, writes=W)

    def memset(o, v, W, eng="pool", R=()):
        S.add(eng, lambda e: e.memset(o, v), reads=R, writes=W)

    def dma(eng, o, i, R, W, key=None, slow=False):
        if slow:
            S.add(eng, lambda e: e.dma_start(out=o, in_=i, allow_slow_non_contiguous=True), reads=R, writes=W, dma=True, semkey=key)
        else:
            S.add(eng, lambda e: e.dma_start(out=o, in_=i), reads=R, writes=W, dma=True, semkey=key)

    cf = sb("cf", [128, CF_N], F32)
    cb = sb("cb", [128, CB_N], BF16)
    epsc = sb("epsc", [128, 1], F32)
    memKT = sb("memKT", [128, 8, 256], BF16)
    memV = sb("memV", [128, 2, 1024], BF16)
    comb = sb("comb", [128, NTL, 16], F32)
    gbc = sb("gbc", [128, 1024], F32)
    bbc = sb("bbc", [128, 1024], F32)
    neglam = sb("neglam", [128, 1], F32)
    gsc = sb("gsc", [128, 1], F32)
    state["mark"] = state["off"]
    ident = cb[:, CB_ID:CB_ID + 128]
    ones_b = cb[:, CB_ONES:CB_ONES + 128]
    ident32 = cf[:, CF_ID:CF_ID + 128]
    ones32 = cf[:, CF_ONES:CF_ONES + 128]

    def sub(k):
        if upto == -k:
            raise StopBuild()

    def p0():
        dma("sp", cf, cstf, [], ["cf"])
        dma("sp", cb, cstb, [], ["cb"])
        memset(epsc, LN_EPS, ["epsc"])
        ci = [0]

        def cast(dst, src):
            dma("pool", dst, src, [], [("cast", ci[0])], key=("cast", ci[0] % 8))
            ci[0] += 1

        cast(Wb["a_w_in"], a_w_in); cast(Wb["a_w_out"], a_w_out); cast(Wb["mem_w_kv"], mem_w_kv)
        for i in range(2):
            cast(Wb["xa_w_q"][i], xa_w_q[i]); cast(Wb["xa_w_out"][i], xa_w_out[i])
        cast(Wb["b_w_in"], b_w_in); cast(Wb["b_w_uq"], b_w_uq); cast(Wb["b_w_qidx"], b_w_qidx)
        cast(Wb["b_w_uk"].rearrange("h r n -> (h r) n"), b_w_uk.rearrange("h r n -> (h r) n"))
        cast(Wb["b_w_uv"].rearrange("h r n -> (h r) n"), b_w_uv.rearrange("h r n -> (h r) n"))
        cast(Wb["b_w_out"], b_w_out)
        for i in range(2):
            for e in range(16):
                cast(Wb["moe_w_gate"][i, e], moe_w_gate[i, e]); cast(Wb["moe_w_up"][i, e], moe_w_up[i, e])
                cast(Wb["moe_w_down"][i, e], moe_w_down[i, e])

        sub(1)
        posi = sb("posi", [128, 128], I32)
        posf = sb("posf", [128, 128], F32)
        post = sb("post", [128, NTL], F32)
        ang = sb("ang", [128, NTL, 32], F32)
        kk = sb("kk", [128, NTL, 32], F32)
        sn = sb("sn", [128, NTL, 32], F32)
        cs = sb("cs", [128, NTL, 32], F32)
        tab = sb("tab", [128, NTL, 128], F32)
        dma("sp", posi[0:NTL, :], positions, [], ["posi"])
        cp(posf[0:NTL, :], posi[0:NTL, :], ["posi"], ["posf"])
        S.add("pe", lambda e: e.transpose(out=bank(0)[:, 0:NTL], in_=posf[0:NTL, :], identity=ident32[0:NTL, 0:NTL]),
              reads=["posf", "cf"], writes=["ps0"])
        cp(post, bank(0)[:, 0:NTL], ["ps0"], ["post"])
        invf = cf[:, CF_INVF:CF_INVF + 32]
        tt(ang, post.unsqueeze(2).to_broadcast([128, NTL, 32]), invf.unsqueeze(1).to_broadcast([128, NTL, 32]), ALU.mult,
           ["post", "cf"], ["ang"])
        MAGIC = 12582912.0
        TWO_PI = 2.0 * math.pi
        C1 = 6.28125
        C2 = float(np.float32(TWO_PI - C1))
        ts(kk, ang, 1.0 / TWO_PI, MAGIC, ALU.mult, ALU.add, ["ang"], ["kk"])
        ts(kk, kk, -MAGIC, None, ALU.add, None, ["kk"], ["kk"])
        stt(ang, kk, -C1, ang, ALU.mult, ALU.add, ["kk", "ang"], ["ang"])
        stt(ang, kk, -C2, ang, ALU.mult, ALU.add, ["kk", "ang"], ["ang"])
        ts(ang, ang, math.pi, -math.pi, ALU.min, ALU.max, ["ang"], ["ang"])
        act(sn, ang, AF.Sin, ["ang"], ["sn"])
        stt(kk, ang, -1.0, ang, ALU.mult, ALU.max, ["ang"], ["kk"])
        ts(kk, kk, -1.0, math.pi / 2, ALU.mult, ALU.add, ["kk"], ["kk"])
        act(cs, kk, AF.Sin, ["kk"], ["cs"])
        for (o0, f0, nf) in ((0, 0, 8), (32, 8, 16), (96, 24, 8)):
            cp(tab[:, :, o0:o0 + nf], cs[:, :, f0:f0 + nf], ["cs"], [("tab", o0, 0)])
            cp(tab[:, :, o0 + nf:o0 + 2 * nf], cs[:, :, f0:f0 + nf], ["cs"], [("tab", o0, 1)])
            ts(tab[:, :, o0 + 2 * nf:o0 + 3 * nf], sn[:, :, f0:f0 + nf], -1.0, None, ALU.mult, None, ["sn"], [("tab", o0, 2)])
            cp(tab[:, :, o0 + 3 * nf:o0 + 4 * nf], sn[:, :, f0:f0 + nf], ["sn"], [("tab", o0, 3)])
        dma("sp", ROPE.rearrange("(t p) c -> p t c", p=128), tab,
            [("tab", o0, k) for o0 in (0, 32, 96) for k in range(4)], ["ROPE"])

        sub(2)
        lam_init0 = 0.8 - 0.6 * math.exp(-0.3 * 0)
        lamb = sb("lamb", [128, 4, 64], F32)
        lamp = sb("lamp", [128, 2, 64], F32)
        lams = sb("lams", [128, 2], F32)
        dma("sp", lamb.rearrange("p a b -> p (a b)"), a_lambda.rearrange("a b -> (a b)").partition_broadcast(128), [], ["lamb"])
        tt(lamp[:, 0, :], lamb[:, 0, :], lamb[:, 1, :], ALU.mult, ["lamb"], [("lamp", 0)])
        tt(lamp[:, 1, :], lamb[:, 2, :], lamb[:, 3, :], ALU.mult, ["lamb"], [("lamp", 1)])
        red(lams, lamp, ALU.add, [("lamp", 0), ("lamp", 1)], ["lams"])
        act(lams, lams, AF.Exp, ["lams"], ["lams"])
        tt(neglam, lams[:, 1:2], lams[:, 0:1], ALU.subtract, ["lams"], ["neglam"])
        ts(neglam, neglam, -lam_init0, None, ALU.add, None, ["neglam"], ["neglam"])
        dma("sp", gsc, a_subln_g, [], ["gsc"])
        ts(gsc, gsc, 1.0 - lam_init0, None, ALU.mult, None, ["gsc"], ["gsc"])

        sub(3)
        S.barrier()
        state["phase"] = "p0b"
        wkv = sb("wkv", [128, 8, 2048], BF16)
        memf = sb("memf", [128, 2, 1024], F32)
        memb = sb("memb", [128, 2, 1024], BF16)
        memT = sb("memT", [128, 8, 256], BF16)
        dma("sp", wkv, Wb["mem_w_kv"].rearrange("(c p) n -> p c n", p=128), [], ["wkv"])
        dma("sp", memf, mem.rearrange("(t p) d -> p t d", p=128), [], ["memf"])
        cp(memb, memf, ["memf"], ["memb"], eng="act")
        for mt in range(2):
            ptv = bank_bf(mt).rearrange("p (c t) -> p c t", c=8)
            for c in range(8):
                tr(ptv[:, c, :], memb[:, mt, c * 128:(c + 1) * 128], ident, ["memb"], [("psb", mt)])
            cp(memT[:, :, mt * 128:(mt + 1) * 128], ptv, [("psb", mt)], [("memT", mt)])
        for j in range(8):
            pk = bank(2 + j % 2)[:, 0:256]
            for k in range(8):
                mm(pk, wkv[:, k, j * 128:(j + 1) * 128], memT[:, k, :], k == 0, k == 7, ["wkv", ("memT", 0), ("memT", 1)], [("pk", j % 2)])
            cp(memKT[:, j, :], pk, [("pk", j % 2)], [("memKT", j)], eng="act" if j % 2 else "dve")
        for mt in range(2):
            for hf in range(2):
                pv = bank(4 + (mt * 2 + hf) % 2)
                for k in range(8):
                    mm(pv, memT[:, k, mt * 128:(mt + 1) * 128], wkv[:, k, 1024 + hf * 512:1024 + (hf + 1) * 512], k == 0, k == 7,
                       ["wkv", ("memT", 0), ("memT", 1)], [("pv", (mt * 2 + hf) % 2)])
                cp(memV[:, mt, hf * 512:(hf + 1) * 512], pv, [("pv", (mt * 2 + hf) % 2)], [("memV", mt, hf)], eng="act" if hf else "dve")


    def load_ln(li, j):
        dma("sp", gbc, ln_g[li, j].partition_broadcast(128), [], ["gbc"])
        dma("sp", bbc, ln_b[li, j].partition_broadcast(128), [], ["bbc"])

    class Epi:
        def __init__(self, pt_bank, nbuf=2):
            self.nb = nbuf
            self.hsb = [sb("e_h%d" % i, [128, 1024], F32) for i in range(nbuf)]
            self.z = [sb("e_z%d" % i, [128, 1024], F32) for i in range(nbuf)]
            self.hb = [sb("e_hb%d" % i, [128, 1024], BF16) for i in range(nbuf)]
            self.hT = [sb("e_hT%d" % i, [128, 8, 128], BF16) for i in range(nbuf)]
            self.st = [sb("e_st%d" % i, [128, 2, 6], F32) for i in range(nbuf)]
            self.mv = [sb("e_mv%d" % i, [128, 2], F32) for i in range(nbuf)]
            self.rs = [sb("e_rs%d" % i, [128, 1], F32) for i in range(nbuf)]
            self.ptb = pt_bank
            self.n = 0

        def run(self, t, mix, mixR, h_src, h_dst, dbg_dst=None):
            b = self.n % self.nb
            self.n += 1
            hsb, z, hb, hT, stt_, mv, rs = self.hsb[b], self.z[b], self.hb[b], self.hT[b], self.st[b], self.mv[b], self.rs[b]
            k = "e%d" % b
            rows = slice(t * 128, (t + 1) * 128)
            dma("sp", hsb, h_src[rows, :], [("Hsrc", t)], [k + "h"])
            stt(z, hsb, ALPHA, mix, ALU.mult, ALU.add, [k + "h"] + mixR, [k + "z"])
            for c in range(2):
                S.add("dve", lambda e, c=c: e.bn_stats(out=stt_[:, c, :], in_=z[:, c * 512:(c + 1) * 512]), reads=[k + "z"], writes=[(k + "st", c)])
            S.add("dve", lambda e: e.bn_aggr(out=mv, in_=stt_.rearrange("p a b -> p (a b)")), reads=[(k + "st", 0), (k + "st", 1)], writes=[k + "mv"])
            act(rs, mv[:, 1:2], AF.Sqrt, [k + "mv"], [k + "rs"], bias=epsc)
            recip(rs, rs, [k + "rs"], [k + "rs"])
            ts(z, z, mv[:, 0:1], rs, ALU.subtract, ALU.mult, [k + "z", k + "mv", k + "rs"], [k + "z"])
            tt(z, z, gbc, ALU.mult, [k + "z", "gbc"], [k + "z"], eng="pool")
            tt(hsb, z, bbc, ALU.add, [k + "z", "bbc"], [k + "h"])
            dma(ST_ENG, h_dst[rows, :], hsb, [k + "h"], [("Hdst", t)], key=k + "hst")
            if dbg_dst is not None:
                dma(ST_ENG, dbg_dst[rows, :], hsb, [k + "h"], [("dbg", t)], key=k + "dbg")
            cp(hb, hsb, [k + "h"], [k + "hb"], eng="act")
            ptv = bank_bf(self.ptb).rearrange("p (c t) -> p c t", c=8)
            for c in range(8):
                tr(ptv[:, c, :], hb[:, c * 128:(c + 1) * 128], ident, [k + "hb"], ["e_pt"])
            cp(hT, ptv, ["e_pt"], [k + "hT"], eng="act")
            dma(ST_ENG, HT[:, :, rows], hT, [k + "hT"], [("HT", t)], key=k + "hTst")

    def t0():
        phase("t0")
        xs = [sb("xs%d" % i, [128, 1024], F32) for i in range(2)]
        xb = [sb("xb%d" % i, [128, 1024], BF16) for i in range(2)]
        xT = [sb("xT%d" % i, [128, 8, 128], BF16) for i in range(2)]
        for t in range(NTL):
            b = t % 2
            rows = slice(t * 128, (t + 1) * 128)
            dma("sp", xs[b], x[rows, :], [], [("xs", b)])
            cp(xb[b], xs[b], [("xs", b)], [("xb", b)], eng="act")
            ptv = bank_bf(b).rearrange("p (c t) -> p c t", c=8)
            for c in range(8):
                tr(ptv[:, c, :], xb[b][:, c * 128:(c + 1) * 128], ident, [("xb", b)], [("pt", b)])
            cp(xT[b], ptv, [("pt", b)], [("xT", b)])
            dma(ST_ENG, HT[:, :, rows], xT[b], [("xT", b)], [("HT", t)], key=("xTst", b))


    def rope(dst, src, nh, dh, rt, off, hf, tA, tB, R, W, key):
        cc = rt[:, off:off + 2 * hf].unsqueeze(1).to_broadcast([128, nh, 2 * hf])
        s1 = rt[:, off + 2 * hf:off + 3 * hf].unsqueeze(1).to_broadcast([128, nh, hf])
        s2 = rt[:, off + 3 * hf:off + 4 * hf].unsqueeze(1).to_broadcast([128, nh, hf])
        tt(tA[:, 0:nh, 0:2 * hf], src[:, :, 0:2 * hf], cc, ALU.mult, R, [key + "A"])
        tt(tB[:, 0:nh, 0:hf], src[:, :, hf:2 * hf], s1, ALU.mult, R, [key + "B0"])
        tt(tB[:, 0:nh, hf:2 * hf], src[:, :, 0:hf], s2, ALU.mult, R, [key + "B1"])
        tt(dst[:, :, 0:2 * hf], tA[:, 0:nh, 0:2 * hf], tB[:, 0:nh, 0:2 * hf], ALU.add, [key + "A", key + "B0", key + "B1"], W)
        if dh > 2 * hf:
            cp(dst[:, :, 2 * hf:dh], src[:, :, 2 * hf:dh], R, W, eng="act")

    def diff_attention(h_src, h_dst, dbg_dst):
        phase("a1")
        wA = sb("wA", [128, 8, 3072], BF16)
        hts = [sb("hts%d" % i, [128, 8, 128], BF16) for i in range(2)]
        rts = [sb("rts%d" % i, [128, 128], F32) for i in range(2)]
        tok = [sb("tok%d" % i, [128, 16, 64], BF16) for i in range(2)]
        vtok = [sb("vtok%d" % i, [128, 1024], BF16) for i in range(2)]
        tA = sb("tA", [128, 16, 32], F32)
        tB = sb("tB", [128, 16, 32], F32)
        oT = [sb("oT%d" % i, [128, 8, 128], BF16) for i in range(2)]
        dma("sp", wA, Wb["a_w_in"].rearrange("(c p) n -> p c n", p=128), [], ["wA"])
        n = 0
        for t in range(NTL):
            b = t % 2
            rows = slice(t * 128, (t + 1) * 128)
            dma("sp", hts[b], HT[:, :, rows], [], [("hts", b)])
            dma("sp", rts[b], ROPE[rows, :], [], [("rts", b)])
            for which in range(3):
                pb = 2 * (n % 2)
                pa = bank(pb, 2)
                for hf in range(2):
                    for k in range(8):
                        mm(pa[:, hf * 512:(hf + 1) * 512], hts[b][:, k, :], wA[:, k, which * 1024 + hf * 512: which * 1024 + (hf + 1) * 512],
                           k == 0, k == 7, [("hts", b), "wA"], [("pa", pb, hf)])
                paR = [("pa", pb, 0), ("pa", pb, 1)]
                if which < 2:
                    tb_ = tok[n % 2]
                    rope(tb_, pa.rearrange("p (h d) -> p h d", h=16), 16, 64, rts[b], 0, 8, tA, tB, paR + [("rts", b)], [("tok", n % 2)], "r")
                    ptb = 4 + n % 2
                    ptv = bank_bf(ptb).rearrange("p (c t) -> p c t", c=8)
                    tf = tb_.rearrange("p h d -> p (h d)")
                    for c in range(8):
                        tr(ptv[:, c, :], tf[:, c * 128:(c + 1) * 128], ident, [("tok", n % 2)], [("pt", ptb)])
                    ob = oT[n % 2]
                    if which == 0:
                        act(ob, ptv, AF.Copy, [("pt", ptb)], [("oT", n % 2)], scale=0.125)
                    else:
                        cp(ob, ptv, [("pt", ptb)], [("oT", n % 2)])
                    dma(ST_ENG, (QT if which == 0 else KT)[:, :, rows], ob, [("oT", n % 2)], [("QK", which, t)], key=("oTst", n % 2))
                else:
                    cp(vtok[b], pa, paR, [("vtok", b)], eng="act")
                    dma(ST_ENG, V[rows, :], vtok[b], [("vtok", b)], [("V", t)], key=("vst", b))
                n += 1

        phase("a2")
        KTh = sb("KTh", [128, NT], BF16)
        QTh = sb("QTh", [128, NT], BF16)
        Vh = sb("Vh", [128, NTL, 128], BF16)
        NPT = 4
        pT = [sb("pT%d" % i, [128, 512], BF16) for i in range(NPT)]
        r0 = sb("r0", [128, 512], F32); r1 = sb("r1", [128, 512], F32)
        a0 = sb("a0", [128, 512], F32); a1 = sb("a1", [128, 512], F32)
        od = sb("od", [128, 512], F32); sq = sb("sq", [128, 512], F32); rstd = sb("rstd", [128, 512], F32)
        on = [sb("on%d" % i, [128, 512], BF16) for i in range(2)]
        cnt_u = [0]
        for h in range(8):
            dma("sp", KTh, KT[:, h, :], [], ["KTh"])
            dma("sp", QTh, QT[:, h, :], [], ["QTh"])
            dma("sp", Vh, V[:, h * 128:(h + 1) * 128].rearrange("(t p) e -> p t e", p=128), [], ["Vh"])
            for qb in range(NQB):
                nj = 4 * qb + 4
                units = [(j, c) for j in range(nj) for c in range(2)]
                base = cnt_u[0]

                def qk(i, qb=qb, base=base, units=units):
                    j, c = units[i]
                    g = base + i
                    c0 = 0 if j < 4 * qb else 128 * (j - 4 * qb)
                    sbk = g % 4
                    ps_ = bank(sbk)
                    pr = slice(c * 64, (c + 1) * 64)
                    mm(ps_[:, c0:512], KTh[pr, j * 128:(j + 1) * 128], QTh[pr, qb * 512 + c0:(qb + 1) * 512], True, True,
                       ["KTh", "QTh"], [("ps", sbk)])
                    pt_ = pT[g % NPT]
                    pk = ("pT", g % NPT)
                    act(pt_[:, c0:512], ps_[:, c0:512], AF.Exp, [("ps", sbk)], [pk])
                    if j >= 4 * qb:
                        memset(pt_[64:128, c0:c0 + 64], 0.0, [pk])

                def pv(i, qb=qb, base=base, units=units, nj=nj):
                    j, c = units[i]
                    g = base + i
                    c0 = 0 if j < 4 * qb else 128 * (j - 4 * qb)
                    pt_ = pT[g % NPT]
                    pk = ("pT", g % NPT)
                    mm(bank(4 + c)[:, c0:512], Vh[:, j, :], pt_[:, c0:512], j == 0, j == nj - 1, ["Vh", pk], [("po", c)])
                    mm(bank(6 + c)[:, c0:512], ones_b, pt_[:, c0:512], j == 0, j == nj - 1, [pk], [("pl", c)])

                qk(0)
                qk(1)
                for i in range(len(units)):
                    if i + 2 < len(units):
                        qk(i + 2)
                    pv(i)
                cnt_u[0] += len(units)
                recip(r0, bank(6), [("pl", 0)], ["r0"])
                recip(r1, bank(7), [("pl", 1)], ["r1"])
                tt(a0, bank(4), r0, ALU.mult, [("po", 0), "r0"], ["a0"])
                tt(a1, bank(5), r1, ALU.mult, [("po", 1), "r1"], ["a1"])
                stt(od, a1, neglam, a0, ALU.mult, ALU.add, ["a0", "a1"], ["od"])
                act(sq, od, AF.Square, ["od"], ["sq"])
                sbk = cnt_u[0] % 4
                cnt_u[0] += 1
                mm(bank(sbk), ones32, sq, True, True, ["sq"], [("ps", sbk)])
                act(rstd, bank(sbk), AF.Sqrt, [("ps", sbk)], ["rstd"], bias=epsc, scale=1.0 / 128.0)
                recip(rstd, rstd, ["rstd"], ["rstd"])
                ob = on[(h * NQB + qb) % 2]
                stt(ob, od, gsc, rstd, ALU.mult, ALU.mult, ["od", "rstd"], [("on", (h * NQB + qb) % 2)])
                dma(ST_ENG, OT[:, h, qb * 512:(qb + 1) * 512], ob, [("on", (h * NQB + qb) % 2)], [("OT", h, qb)], key=("onst", (h * NQB + qb) % 2))

        out_proj(Wb["a_w_out"], h_src, h_dst, 0, 0, dbg_dst)

    def out_proj(wdram, h_src, h_dst, li, lj, dbg_dst):
        phase("a3")
        wO = sb("wO", [128, 8, 1024], BF16)
        ots = [sb("ots%d" % i, [128, 8, 128], BF16) for i in range(2)]
        ep = Epi(6)
        dma("sp", wO, wdram.rearrange("(c p) n -> p c n", p=128), [], ["wO"])
        load_ln(li, lj)
        for t in range(NTL):
            b = t % 2
            rows = slice(t * 128, (t + 1) * 128)
            dma("sp", ots[b], OT[:, :, rows], [], [("ots", b)])
            pm = bank(2 * b, 2)
            for hf in range(2):
                for c in range(8):
                    mm(pm[:, hf * 512:(hf + 1) * 512], ots[b][:, c, :], wO[:, c, hf * 512:(hf + 1) * 512], c == 0, c == 7,
                       [("ots", b), "wO"], [("pm", b, hf)])
            ep.run(t, pm, [("pm", b, 0), ("pm", b, 1)], h_src, h_dst, dbg_dst)

    def cross_attention(li, h_src, h_dst, dbg_dst):
        phase("x%d" % li)
        wq = sb("wq", [128, 8, 1024], BF16)
        wo = sb("wo", [128, 8, 1024], BF16)
        hts = [sb("hts%d" % i, [128, 8, 128], BF16) for i in range(2)]
        qT = sb("qT", [128, 8, 128], BF16)
        nmx = sb("nmx", [128, 4], F32)
        lsum = sb("lsum", [128, 4], F32)
        P = sb("P", [128, 4, 256], BF16)
        Pn = sb("Pn", [128, 4, 256], BF16)
        PT = sb("PT", [128, 8, 128], BF16)
        oT = sb("oT", [128, 8, 128], BF16)
        ep = Epi(7)
        dma("sp", wq, Wb["xa_w_q"][li].rearrange("(c p) n -> p c n", p=128), [], ["wq"])
        dma("sp", wo, Wb["xa_w_out"][li].rearrange("(c p) n -> p c n", p=128), [], ["wo"])
        load_ln(li, 1)
        pA = bank(0, 2).rearrange("p (c t) -> p c t", c=8)
        psc = bank(2, 2).rearrange("p (h m) -> p h m", h=4)
        ptp = bank_bf(4).rearrange("p (c t) -> p c t", c=8)
        pmix = bank(5, 2)
        for t in range(NTL):
            b = t % 2
            rows = slice(t * 128, (t + 1) * 128)
            dma("sp", hts[b], HT[:, :, rows], [], [("hts", b)])
            for j in range(8):
                for k in range(8):
                    mm(pA[:, j, :], wq[:, k, j * 128:(j + 1) * 128], hts[b][:, k, :], k == 0, k == 7, ["wq", ("hts", b)], [("pA", j // 4)])
            act(qT, pA, AF.Copy, [("pA", 0), ("pA", 1)], ["qT"], scale=1.0 / 16.0)
            for hh in range(4):
                for dc in range(2):
                    mm(psc[:, hh, :], qT[:, 2 * hh + dc, :], memKT[:, 2 * hh + dc, :], dc == 0, dc == 1, ["qT"], [("psc", hh // 2)])
            red(nmx, psc, ALU.max, [("psc", 0), ("psc", 1)], ["nmx"], negate=True)
            for hh in range(4):
                act(P[:, hh, :], psc[:, hh, :], AF.Exp, [("psc", hh // 2), "nmx"], [("P", hh)], bias=nmx[:, hh:hh + 1], accum=lsum[:, hh:hh + 1])
            recip(lsum, lsum, [("P", hh) for hh in range(4)], ["lsum"])
            tt(Pn, P, lsum.unsqueeze(2).to_broadcast([128, 4, 256]), ALU.mult, [("P", hh) for hh in range(4)] + ["lsum"], ["Pn"])
            for hh in range(4):
                for mc in range(2):
                    tr(ptp[:, hh * 2 + mc, :], Pn[:, hh, mc * 128:(mc + 1) * 128], ident, ["Pn"], ["ptp"])
            cp(PT, ptp, ["ptp"], ["PT"])
            for hh in range(4):
                for dch in range(2):
                    for mc in range(2):
                        mm(pA[:, hh * 2 + dch, :], memV[:, mc, hh * 256 + dch * 128: hh * 256 + (dch + 1) * 128], PT[:, hh * 2 + mc, :],
                           mc == 0, mc == 1, ["PT"], [("pA", (hh * 2 + dch) // 4)])
            cp(oT, pA, [("pA", 0), ("pA", 1)], ["oT"], eng="act")
            for hf in range(2):
                for c in range(8):
                    mm(pmix[:, hf * 512:(hf + 1) * 512], oT[:, c, :], wo[:, c, hf * 512:(hf + 1) * 512], c == 0, c == 7, ["oT", "wo"], [("pmix", hf)])
            ep.run(t, pmix, [("pmix", 0), ("pmix", 1)], h_src, h_dst, dbg_dst)

    def moe(li, h_src, h_dst, dbg_dst):
        phase("m1_%d" % li)
        wr = sb("wr", [128, 8, 20], F32)
        whi = sb("whi", [128, 8, 20], BF16)
        wlo = sb("wlo", [128, 8, 20], BF16)
        rb = sb("rb", [128, 20], F32)
        LG = sb("LG", [128, NTL, 20], F32)
        hs = [sb("hs%d" % i, [128, 1024], F32) for i in range(2)]
        hhi = [sb("hhi%d" % i, [128, 1024], BF16) for i in range(2)]
        hlo = [sb("hlo%d" % i, [128, 1024], BF16) for i in range(2)]
        hiT = [sb("hiT%d" % i, [128, 8, 128], BF16) for i in range(2)]
        loT = [sb("loT%d" % i, [128, 8, 128], BF16) for i in range(2)]
        dma("sp", wr[:, :, 0:4], moe_w_group[li].rearrange("(c p) n -> p c n", p=128), [], [("wr", 0)], slow=True)
        dma("sp", wr[:, :, 4:20], moe_w_expert[li].rearrange("(c p) n -> p c n", p=128), [], [("wr", 1)], slow=True)
        dma("sp", rb[:, 0:4], moe_b_group[li].partition_broadcast(128), [], [("rb", 0)])
        dma("sp", rb[:, 4:20], moe_b_expert[li].partition_broadcast(128), [], [("rb", 1)])
        cp(whi, wr, [("wr", 0), ("wr", 1)], ["whi"])
        tt(wlo, wr, whi, ALU.subtract, [("wr", 0), ("wr", 1), "whi"], ["wlo"])
        for t in range(NTL):
            b = t % 2
            rows = slice(t * 128, (t + 1) * 128)
            dma("sp", hs[b], h_src[rows, :], [], [("hs", b)])
            cp(hhi[b], hs[b], [("hs", b)], [("hhi", b)], eng="act")
            tt(hlo[b], hs[b], hhi[b], ALU.subtract, [("hs", b), ("hhi", b)], [("hlo", b)])
            p1 = bank_bf(2 * b).rearrange("p (c t) -> p c t", c=8)
            p2 = bank_bf(2 * b + 1).rearrange("p (c t) -> p c t", c=8)
            for c in range(8):
                tr(p1[:, c, :], hhi[b][:, c * 128:(c + 1) * 128], ident, [("hhi", b)], [("p1", b)])
            for c in range(8):
                tr(p2[:, c, :], hlo[b][:, c * 128:(c + 1) * 128], ident, [("hlo", b)], [("p2", b)])
            cp(hiT[b], p1, [("p1", b)], [("hiT", b)], eng="act")
            cp(loT[b], p2, [("p2", b)], [("loT", b)])
            pl = bank(4 + b)[:, 0:20]
            n = 0
            for (aT, w_, ka, kw_) in ((hiT[b], whi, ("hiT", b), "whi"), (hiT[b], wlo, ("hiT", b), "wlo"), (loT[b], whi, ("loT", b), "whi")):
                for k in range(8):
                    mm(pl, aT[:, k, :], w_[:, k, :], n == 0, n == 23, [ka, kw_], [("pl", b)])
                    n += 1
            tt(LG[:, t, :], pl, rb, ALU.add, [("pl", b), ("rb", 0), ("rb", 1)], [("LG", t)])
        T = NTL
        G = LG[:, :, 0:4]
        E = LG[:, :, 4:20].rearrange("p t (g j) -> p t g j", g=4)
        LGR = [("LG", t) for t in range(NTL)]
        gmax = sb("gmax", [128, T], F32); goh = sb("goh", [128, T, 4], F32); ge = sb("ge", [128, T, 4], F32)
        gsum = sb("gsum", [128, T], F32); tmp4 = sb("tmp4", [128, T, 4, 4], F32); esel = sb("esel", [128, T, 4], F32)
        m1 = sb("m1", [128, T], F32); oh1 = sb("oh1", [128, T, 4], F32); e2 = sb("e2", [128, T, 4], F32)
        m2 = sb("m2", [128, T], F32); oh2 = sb("oh2", [128, T, 4], F32); dd = sb("dd", [128, T], F32)
        w1 = sb("w1", [128, T], F32); w2 = sb("w2", [128, T], F32); cin = sb("cin", [128, T, 4], F32); cin2 = sb("cin2", [128, T, 4], F32)

        def bc3(a):
            return a.unsqueeze(2).to_broadcast([128, T, 4])

        red(gmax, G, ALU.max, LGR, ["gmax"])
        tt(goh, G, bc3(gmax), ALU.is_equal, LGR + ["gmax"], ["goh"])
        tt(ge, G, bc3(gmax), ALU.subtract, LGR + ["gmax"], ["ge"])
        act(ge, ge, AF.Exp, ["ge"], ["ge"])
        red(gsum, ge, ALU.add, ["ge"], ["gsum"])
        recip(gsum, gsum, ["gsum"], ["gsum"])
        tt(tmp4, E, goh.unsqueeze(3).to_broadcast([128, T, 4, 4]), ALU.mult, LGR + ["goh"], ["tmp4"])
        red(esel, tmp4.rearrange("p t g j -> p t j g"), ALU.add, ["tmp4"], ["esel"])
        red(m1, esel, ALU.max, ["esel"], ["m1"])
        tt(oh1, esel, bc3(m1), ALU.is_equal, ["esel", "m1"], ["oh1"])
        stt(e2, oh1, -1e30, esel, ALU.mult, ALU.add, ["oh1", "esel"], ["e2"])
        red(m2, e2, ALU.max, ["e2"], ["m2"])
        tt(oh2, e2, bc3(m2), ALU.is_equal, ["e2", "m2"], ["oh2"])
        tt(dd, m2, m1, ALU.subtract, ["m1", "m2"], ["dd"])
        act(dd, dd, AF.Exp, ["dd"], ["dd"])
        ts(w1, dd, 1.0, None, ALU.add, None, ["dd"], ["w1"])
        recip(w1, w1, ["w1"], ["w1"])
        tt(w2, dd, w1, ALU.mult, ["dd", "w1"], ["w2"])
        tt(w1, w1, gsum, ALU.mult, ["w1", "gsum"], ["w1"])
        tt(w2, w2, gsum, ALU.mult, ["w2", "gsum"], ["w2"])
        tt(cin, oh1, bc3(w1), ALU.mult, ["oh1", "w1"], ["cin"])
        tt(cin2, oh2, bc3(w2), ALU.mult, ["oh2", "w2"], ["cin2"])
        tt(cin, cin, cin2, ALU.add, ["cin", "cin2"], ["cin"])
        tt(comb.rearrange("p t (g j) -> p t g j", g=4), goh.unsqueeze(3).to_broadcast([128, T, 4, 4]),
           cin.unsqueeze(2).to_broadcast([128, T, 4, 4]), ALU.mult, ["goh", "cin"], ["comb"])

        phase("m2_%d" % li)
        TB = 1024
        hTb = sb("hTb", [128, 8, TB], BF16)
        yacc = sb("yacc", [128, 8, 1024], F32)
        wg = [sb("wg%d" % i, [128, 8, 512], BF16) for i in range(2)]
        wu = [sb("wu%d" % i, [128, 8, 512], BF16) for i in range(2)]
        wd = [sb("wd%d" % i, [128, 4, 1024], BF16) for i in range(2)]
        sg = [sb("sg%d" % i, [128, 512], F32) for i in range(2)]
        hid = [sb("hid%d" % i, [128, 4, 512], BF16) for i in range(2)]
        ep = Epi(6)
        load_ln(li, 2)
        nh = 0
        ny = 0
        for blk in range(NT // TB):
            dma("sp", hTb, HT[:, :, blk * TB:(blk + 1) * TB], [], ["hTb"])
            memset(yacc, 0.0, ["yacc"] + [("yacc", tl, dh) for tl in range(8) for dh in range(2)])
            for e in range(16):
                wbuf = e % 2
                dma("sp", wg[wbuf], Wb["moe_w_gate"][li, e].rearrange("(c p) n -> p c n", p=128), [], [("wg", wbuf)])
                dma("sp", wu[wbuf], Wb["moe_w_up"][li, e].rearrange("(c p) n -> p c n", p=128), [], [("wu", wbuf)])
                dma("sp", wd[wbuf], Wb["moe_w_down"][li, e].rearrange("(c p) n -> p c n", p=128), [], [("wd", wbuf)])
                for half in range(TB // 512):
                    hb_ = hid[nh % 2]
                    hk = ("hid", nh % 2)
                    for fc in range(4):
                        pg = bank(0 + (fc % 2))
                        pu = bank(2 + (fc % 2))
                        for k in range(8):
                            mm(pg, wg[wbuf][:, k, fc * 128:(fc + 1) * 128], hTb[:, k, half * 512:(half + 1) * 512], k == 0, k == 7,
                               [("wg", wbuf), "hTb"], [("pg", fc % 2)])
                        for k in range(8):
                            mm(pu, wu[wbuf][:, k, fc * 128:(fc + 1) * 128], hTb[:, k, half * 512:(half + 1) * 512], k == 0, k == 7,
                               [("wu", wbuf), "hTb"], [("pu", fc % 2)])
                        act(sg[fc % 2], pg, AF.Silu, [("pg", fc % 2)], [("sg", fc % 2)])
                        tt(hb_[:, fc, :], sg[fc % 2], pu, ALU.mult, [("sg", fc % 2), ("pu", fc % 2)], [hk + (fc,)])
                    hR = [hk + (fc,) for fc in range(4)]
                    for tt_ in range(4):
                        tl = half * 4 + tt_
                        tg = blk * 8 + tl
                        for dh in range(2):
                            py = bank(4 + ny % 2)
                            for fc in range(4):
                                mm(py, hb_[:, fc, tt_ * 128:(tt_ + 1) * 128], wd[wbuf][:, fc, dh * 512:(dh + 1) * 512], fc == 0, fc == 3,
                                   hR + [("wd", wbuf)], [("py", ny % 2)])
                            ya = yacc[:, tl, dh * 512:(dh + 1) * 512]
                            stt(ya, py, comb[:, tg, e:e + 1], ya, ALU.mult, ALU.add, [("py", ny % 2), "yacc", ("yacc", tl, dh)], [("yacc", tl, dh)])
                            ny += 1
                    nh += 1
            for tl in range(8):
                ep.run(blk * 8 + tl, yacc[:, tl, :], [("yacc", tl, 0), ("yacc", tl, 1)], h_src, h_dst, dbg_dst)

    def dsa(h_src, h_dst, dbg_dst):
        phase("d1")
        wI = sb("wI", [128, 8, 624], BF16)
        wUQ = sb("wUQ", [128, 2, 1024], BF16)
        wQI = sb("wQI", [128, 2, 1024], BF16)
        wUKT = sb("wUKT", [128, 8, 256], BF16)
        gq = sb("gq", [128, 256], F32)
        gkv = sb("gkv", [128, 256], F32)
        hts = [sb("hts%d" % i, [128, 8, 128], BF16) for i in range(2)]
        rts = [sb("rts%d" % i, [128, 128], F32) for i in range(2)]
        sqj = sb("sqj", [128, 256], F32)
        ssq = sb("ssq", [128, 2], F32)
        cqn = sb("cqn", [128, 256], BF16)
        ckn = sb("ckn", [128, 256], BF16)
        cqT = sb("cqT", [128, 2, 128], BF16)
        kvTs = sb("kvTs", [128, 3, 128], BF16)
        krp = sb("krp", [128, 1, 128], BF16)
        kix = sb("kix", [128, 2, 64], BF16)
        kiT = sb("kiT", [128, 128], BF16)
        wix = sb("wix", [128, 16], F32)
        tA = sb("tA", [128, 16, 32], F32)
        tB = sb("tB", [128, 16, 32], F32)
        qtk = sb("qtk", [128, 8, 128], BF16)
        qiT = sb("qiT", [128, 8, 8, 16], BF16)
        qT = sb("qT", [128, 8, 128], BF16)
        qfT = sb("qfT", [128, 8, 3, 128], BF16)
        qiks = [sb("qik%d" % i, [128, 16, 64], BF16) for i in range(2)]
        dma("sp", wI, Wb["b_w_in"].rearrange("(c p) n -> p c n", p=128), [], ["wI"])
        dma("sp", wUQ, Wb["b_w_uq"].rearrange("(c p) n -> p c n", p=128), [], ["wUQ"])
        dma("sp", wQI, Wb["b_w_qidx"].rearrange("(c p) n -> p c n", p=128), [], ["wQI"])
        wukn = sb("wukn", [128, 8, 2, 128], BF16)
        memset(wukn, 0.0, ["wukn"])
        for rc in range(2):
            dma("sp", wukn[:, :, rc, 32:128], Wb["b_w_uk"][:, rc * 128:(rc + 1) * 128, :].rearrange("h p n -> p h n"), [], ["wukn", ("wukn", rc)], key=("wukn", rc))
        for hh in range(8):
            ptw = bank_bf(hh % 2).rearrange("p (c t) -> p c t", c=8)
            for rc in range(2):
                tr(ptw[:, rc, :], wukn[:, hh, rc, :], ident, ["wukn", ("wukn", 0), ("wukn", 1)], ["p%d" % (hh % 2)])
            cp(wUKT[:, hh, :].rearrange("p (c t) -> p c t", c=2), ptw[:, 0:2, :], ["p%d" % (hh % 2)], [("wUKT", hh)])
        wUKR = [("wUKT", hh) for hh in range(8)]
        dma("sp", gq, b_q_norm_g.partition_broadcast(128), [], ["gq"])
        dma("sp", gkv, b_kv_norm_g.partition_broadcast(128), [], ["gkv"])
        memset(kvTs, 0.0, ["kvTs"])
        memset(krp, 0.0, ["krp"])
        memset(qfT, 0.0, ["qfT"])
        sub(11)
        for t in range(NTL):
            b = t % 2
            rows = slice(t * 128, (t + 1) * 128)
            dma("sp", hts[b], HT[:, :, rows], [], [("hts", b)])
            dma("sp", rts[b], ROPE[rows, :], [], [("rts", b)])
            p0 = bank(0)
            p1 = bank(1)[:, 0:112]
            for k in range(8):
                mm(p0, hts[b][:, k, :], wI[:, k, 0:512], k == 0, k == 7, [("hts", b), "wI"], ["p0"])
            for k in range(8):
                mm(p1, hts[b][:, k, :], wI[:, k, 512:624], k == 0, k == 7, [("hts", b), "wI"], ["p1"])
            for i, (gg, dst, nm_) in enumerate(((gq, cqn, "cqn"), (gkv, ckn, "ckn"))):
                act(sqj, p0[:, i * 256:(i + 1) * 256], AF.Square, ["p0"], ["sqj"], accum=ssq[:, i:i + 1])
                act(ssq[:, i:i + 1], ssq[:, i:i + 1], AF.Sqrt, ["sqj"], [("ssq", i)], bias=epsc, scale=1.0 / 256.0)
                recip(ssq[:, i:i + 1], ssq[:, i:i + 1], [("ssq", i)], [("ssq", i)])
                stt(dst, p0[:, i * 256:(i + 1) * 256], ssq[:, i:i + 1], gg, ALU.mult, ALU.mult, ["p0", ("ssq", i), "gq", "gkv"], [nm_])
            dma(ST_ENG, CKV[rows, :], ckn, ["ckn"], [("CKV", t)], key="ckvst")
            sub(12)
            rope(krp[:, :, 0:32], p1[:, 0:32].rearrange("p (h d) -> p h d", h=1), 1, 32, rts[b], 32, 16, tA, tB, ["p1", ("rts", b)], ["krp"], "rk")
            rope(kix[:, 0:1, :], p1[:, 32:96].rearrange("p (h d) -> p h d", h=1), 1, 64, rts[b], 96, 8, tA, tB, ["p1", ("rts", b)], [("kix", 0)], "ri")
            cp(kix[:, 1:2, :], kix[:, 0:1, :], [("kix", 0)], [("kix", 1)])
            ts(wix, p1[:, 96:112], 1.0 / 32.0, None, ALU.mult, None, ["p1"], ["wix"])
            dma(ST_ENG, WIX[rows, :], wix, ["wix"], [("WIX", t)], key="wixst")
            sub(13)
            pt = bank_bf(2).rearrange("p (c t) -> p c t", c=8)
            for c in range(2):
                tr(pt[:, c, :], cqn[:, c * 128:(c + 1) * 128], ident, ["cqn"], ["pt2"])
            sub(1310)
            for c in range(2):
                tr(pt[:, 2 + c, :], ckn[:, c * 128:(c + 1) * 128], ident, ["ckn", ("CKV", t)], ["pt2"])
            sub(1311)
            tr(pt[:, 4, :], krp.rearrange("p h d -> p (h d)"), ident, ["krp"], ["pt2"])
            tr(pt[:, 5, :], kix.rearrange("p a d -> p (a d)"), ident, [("kix", 0), ("kix", 1)], ["pt2"])
            sub(131)
            cp(cqT, pt[:, 0:2, :], ["pt2"], ["cqT"])
            cp(kvTs[:, 0:2, :], pt[:, 2:4, :], ["pt2"], [("kvTs", 0)], eng="act")
            cp(kvTs[:, 2, :], pt[:, 4, :], ["pt2"], [("kvTs", 1)])
            cp(kiT, pt[:, 5, :], ["pt2"], ["kiT"], eng="act")
            sub(132)
            dma(ST_ENG, KVT[:, :, rows], kvTs, ["kvTs", ("kvTs", 0), ("kvTs", 1)], [("KVT", t)], key="kvtst")
            sub(133)
            dma(ST_ENG, KIT[:, rows], kiT, ["kiT"], [("KIT", t)], key="kitst")
            sub(14)
            pq = bank(3, 2)
            for hf in range(2):
                for c in range(2):
                    mm(pq[:, hf * 512:(hf + 1) * 512], cqT[:, c, :], wUQ[:, c, hf * 512:(hf + 1) * 512], c == 0, c == 1, ["cqT", "wUQ"], [("pq", hf)])
            rope(qtk, pq.rearrange("p (h d) -> p h d", h=8), 8, 128, rts[b], 32, 16, tA, tB, [("pq", 0), ("pq", 1), ("rts", b)], ["qtk"], "rq")
            sub(15)
            pqi = bank(5, 2)
            for hf in range(2):
                for c in range(2):
                    mm(pqi[:, hf * 512:(hf + 1) * 512], cqT[:, c, :], wQI[:, c, hf * 512:(hf + 1) * 512], c == 0, c == 1, ["cqT", "wQI"], [("pqi", hf)])
            qik = qiks[b]
            rope(qik, pqi.rearrange("p (h d) -> p h d", h=16), 16, 64, rts[b], 96, 8, tA, tB, [("pqi", 0), ("pqi", 1), ("rts", b)], [("qik", b)], "rqi")
            sub(16)
            pt7 = bank_bf(7).rearrange("p (c t) -> p c t", c=8)
            qf = qtk.rearrange("p h d -> p (h d)")
            for hh in range(8):
                tr(pt7[:, hh, :], qf[:, hh * 128:(hh + 1) * 128], ident, ["qtk"], ["pt7"])
            cp(qT, pt7, ["pt7"], ["qT"])
            qif = qik.rearrange("p h d -> p (h d)")
            for c in range(8):
                tr(pt7[:, c, :], qif[:, c * 128:(c + 1) * 128], ident, [("qik", b)], ["pt7"])
            cp(qiT.rearrange("p g j q -> p j g q"), pt7.rearrange("p j (g q) -> p j g q", g=8), ["pt7"], ["qiT"])
            dma(ST_ENG, QIT[:, t, :], qiT.rearrange("p g j q -> p (g j q)"), ["qiT"], [("QIT", t)], key="qitst")
            sub(17)
            sc = 1.0 / math.sqrt(128.0)
            pl_ = bank(0, 2).rearrange("p (c t) -> p c t", c=8)
            for half in range(2):
                for hh4 in range(4):
                    hh = half * 4 + hh4
                    for rc in range(2):
                        mm(pl_[:, hh4 * 2 + rc, :], wUKT[:, hh, rc * 128:(rc + 1) * 128], qT[:, hh, :], True, True,
                           wUKR + ["qT"], ["p0" if (hh4 * 2 + rc) < 4 else "p1"])
                for hh4 in range(4):
                    hh = half * 4 + hh4
                    act(qfT[:, hh, 0:2, :], pl_[:, hh4 * 2:hh4 * 2 + 2, :], AF.Copy, ["p0", "p1"], [("qfT", hh)], scale=sc)
            act(qfT[0:32, :, 2, :], qT[0:32, :, :], AF.Copy, ["qT"], [("qfTr")], scale=sc)
            dma(ST_ENG, QFT[:, :, :, rows], qfT, ["qfT", "qfTr"] + [("qfT", hh) for hh in range(8)], [("QFT", t)], key="qftst")

        phase("d2")
        KIs = sb("KIs", [128, NT], BF16)
        qis = [sb("qis%d" % i, [128, 1024], BF16) for i in range(2)]
        wxs = [sb("wxs%d" % i, [128, 16], F32) for i in range(2)]
        wsel = sb("wsel", [128, 8, 16], F32)
        wc = sb("wc", [128, 8, 2], F32)
        Wblk = [sb("Wblk%d" % i, [128, 16, 128], BF16) for i in range(2)]
        NRB = 4
        Rb = [sb("Rb%d" % i, [128, 512], BF16) for i in range(NRB)]
        sc_ = [sb("sc%d" % i, [128, NT], F32) for i in range(2)]
        junk = sb("junk", [128, NT], BF16)
        nmk = [sb("nmk%d" % i, [128, NT], BF16) for i in range(2)]
        lo = [sb("lo%d" % i, [128, 1], F32) for i in range(2)]
        hi = [sb("hi%d" % i, [128, 1], F32) for i in range(2)]
        stp = [sb("stp%d" % i, [128, 20], F32) for i in range(2)]
        mid = [sb("mid%d" % i, [128, 1], F32) for i in range(2)]
        cnt = [sb("cnt%d" % i, [128, 1], F32) for i in range(2)]
        geb = [sb("geb%d" % i, [128, 1], F32) for i in range(2)]
        NIT = 18
        hmask = cf[:, CF_HM:CF_HM + 16]
        dma("sp", KIs, KIT, [], ["KIs"])
        nlg = [0]

        def prep(t):
            b = t % 2
            rows = slice(t * 128, (t + 1) * 128)
            dma("sp", qis[b], QIT[:, t, :], [], [("qis", b)])
            dma("sp", wxs[b], WIX[rows, :], [], [("wxs", b)])
            pw = bank(6)[:, 0:128].rearrange("p (g h) -> p g h", g=8)
            for g in range(8):
                mm(pw[:, g, :], cf[:, CF_SEL + g * 128:CF_SEL + (g + 1) * 128], wxs[b], True, True, [("wxs", b)], ["pw"])
            tt(wsel, pw, hmask.unsqueeze(1).to_broadcast([128, 8, 16]), ALU.mult, ["pw"], ["wsel"])
            red(wc, wsel.rearrange("p g (j r) -> p g r j", r=2), ALU.add, ["wsel"], ["wc"])
            for g in range(8):
                for par in range(2):
                    ts(Wblk[b][:, g * 2 + par, :], cb[:, CB_E + g * 128:CB_E + (g + 1) * 128], wc[:, g, par:par + 1], None, ALU.mult, None,
                       ["wc"], [("Wblk", b, g * 2 + par)], eng="pool")

        def main(t):
            b = t % 2
            nk = 128 * (t + 1)
            WR = [("Wblk", b, i) for i in range(16)]
            nkb = (nk + 511) // 512
            steps = [(kb, i) for kb in range(nkb) for i in range(16)]
            base = nlg[0]

            def geom(kb):
                kw = min(512, nk - kb * 512)
                return kw, slice(kb * 512, kb * 512 + kw)

            def lg(s):
                kb, i = steps[s]
                kw, ks = geom(kb)
                n = base + s
                g, par = i // 2, i % 2
                plg = bank(n % 4)
                pr = slice(par * 64, (par + 1) * 64)
                mm(plg[:, 0:kw], qis[b][pr, g * 128:(g + 1) * 128], KIs[pr, ks], True, True, [("qis", b), "KIs"], [("plg", n % 4)])
                act(Rb[n % NRB][:, 0:kw], plg[:, 0:kw], AF.Relu, [("plg", n % 4)], [("Rb", n % NRB)])

            def scm(s):
                kb, i = steps[s]
                kw, ks = geom(kb)
                n = base + s
                pscore = bank(4 + kb % 2)
                mm(pscore[:, 0:kw], Wblk[b][:, i, :], Rb[n % NRB][:, 0:kw], i == 0, i == 15, WR + [("Rb", n % NRB)], [("pscore", kb % 2)])
                if i == 15:
                    cp(sc_[b][:, ks], pscore[:, 0:kw], [("pscore", kb % 2)], [("sc", b, kb)], eng="act")

            lg(0)
            lg(1)
            for s in range(len(steps)):
                if s + 2 < len(steps):
                    lg(s + 2)
                scm(s)
            nlg[0] += len(steps)

        def bisect(t):
            b = t % 2
            rows = slice(t * 128, (t + 1) * 128)
            nk = 128 * (t + 1)
            nkb = (nk + 511) // 512
            scR = [("sc", b, kb) for kb in range(nkb)]
            scK = ("scall", b)
            memset(sc_[b][0:64, t * 128 + 64:(t + 1) * 128], -1e30, [scK], R=scR)
            sv = sc_[b][:, 0:nk]
            if t >= 2:
                red(lo[b], sc_[b][:, 0:nk - 64], ALU.min, scR + [scK], [("lo", b)])
                red(hi[b], sv, ALU.max, scR + [scK], [("hi", b)])
                tt(mid[b], hi[b], lo[b], ALU.subtract, [("lo", b), ("hi", b)], [("mid", b)])
                for i in range(NIT):
                    ts(stp[b][:, i:i + 1], mid[b], 2.0 ** -(i + 1), None, ALU.mult, None, [("mid", b)], [("stp", b, i)], eng="pool")
                for i in range(NIT):
                    tt(mid[b], lo[b], stp[b][:, i:i + 1], ALU.add, [("lo", b), ("stp", b, i)], [("mid", b)])
                    ts(junk[:, 0:nk], sv, mid[b], 0.0, ALU.is_ge, ALU.add, scR + [scK, ("mid", b)], ["junk", ("cnt", b)], accum=cnt[b])
                    ts(geb[b], cnt[b], 255.5, stp[b][:, i:i + 1], ALU.is_ge, ALU.mult, [("cnt", b), ("stp", b, i)], [("geb", b)])
                    tt(lo[b], lo[b], geb[b], ALU.add, [("lo", b), ("geb", b)], [("lo", b)])
            else:
                memset(lo[b], -1e29, [("lo", b)], eng="dve")
            ts(nmk[b][:, 0:nk], sv, lo[b], NEG, ALU.is_lt, ALU.mult, scR + [scK, ("lo", b)], [("nmk", b)])
            dma(ST_ENG, NM[rows, 0:nk], nmk[b][:, 0:nk], [("nmk", b)], [("NM", t)], key=("nmst", b))

        prep(0)
        for t in range(NTL):
            main(t)
            if t + 1 < NTL:
                prep(t + 1)
            bisect(t)

        phase("d3")
        KVs = sb("KVs", [128, 3, NT], BF16)
        CKs = sb("CKs", [128, NTL, 256], BF16)
        wUV = sb("wUV", [128, 8, 2, 128], BF16)
        qfs = [sb("qfs%d" % i, [128, 8, 3, 512], BF16) for i in range(2)]
        NNM = 6
        nms = [sb("nms%d" % i, [128, 4, 128], BF16) for i in range(NNM)]
        NPT = 4
        pT = [sb("pT%d" % i, [128, 512], BF16) for i in range(NPT)]
        rl = sb("rl", [128, 512], F32)
        olat = sb("olat", [128, 2, 512], BF16)
        on = [sb("on%d" % i, [128, 512], BF16) for i in range(2)]
        dma("sp", KVs, KVT, [], ["KVs"])
        dma("sp", CKs, CKV.rearrange("(t p) r -> p t r", p=128), [], ["CKs"])
        for hh in range(8):
            dma("sp", wUV[:, hh, :, :], Wb["b_w_uv"][hh].rearrange("(c p) v -> p c v", p=128), [], [("wUV", hh)])
        gcnt = [0]
        SB3 = (0, 1, 7)
        for qb in range(NQB):
            qbuf = qb % 2
            dma("sp", qfs[qbuf], QFT[:, :, :, qb * 512:(qb + 1) * 512], [], [("qfs", qbuf)])
            nj = 4 * qb + 4
            for hh in range(8):
                base = gcnt[0]

                def qk(j, qb=qb, hh=hh, base=base, qbuf=qbuf):
                    g = base + j
                    c0 = 0 if j < 4 * qb else 128 * (j - 4 * qb)
                    s0 = c0 // 128
                    nb_ = nms[g % NNM]
                    nk_ = ("nms", g % NNM)
                    dma("sp", nb_[:, s0:4, :], NM[qb * 512 + c0:(qb + 1) * 512, j * 128:(j + 1) * 128].rearrange("(s p) k -> p s k", p=128),
                        [], [nk_])
                    bk = SB3[g % 3]
                    ps_ = bank(bk)
                    pk_ = ("ps", bk)
                    mm(ps_[:, c0:512], KVs[:, 0, j * 128:(j + 1) * 128], qfs[qbuf][:, hh, 0, c0:512], True, False, ["KVs", ("qfs", qbuf)], [pk_])
                    mm(ps_[:, c0:512], KVs[:, 1, j * 128:(j + 1) * 128], qfs[qbuf][:, hh, 1, c0:512], False, False, ["KVs", ("qfs", qbuf)], [pk_])
                    mm(ps_[:, c0:512], KVs[:, 2, j * 128:(j + 1) * 128], qfs[qbuf][:, hh, 2, c0:512], False, False, ["KVs", ("qfs", qbuf)], [pk_])
                    for s in range(s0, 4):
                        mm(ps_[:, s * 128:(s + 1) * 128], nb_[:, s, :], ident, False, s == 3, [nk_], [pk_])
                    act(pT[g % NPT][:, c0:512], ps_[:, c0:512], AF.Exp, [pk_], [("pT", g % NPT)])

                def pv(j, qb=qb, hh=hh, base=base, nj=nj):
                    g = base + j
                    c0 = 0 if j < 4 * qb else 128 * (j - 4 * qb)
                    pt_ = pT[g % NPT]
                    pk = ("pT", g % NPT)
                    for rc in range(2):
                        mm(bank(2 + rc)[:, c0:512], CKs[:, j, rc * 128:(rc + 1) * 128], pt_[:, c0:512], j == 0, j == nj - 1, ["CKs", pk], [("po", rc)])
                    mm(bank(4)[:, c0:512], ones_b, pt_[:, c0:512], j == 0, j == nj - 1, [pk], ["pl"])

                qk(0)
                if nj > 1:
                    qk(1)
                for j in range(nj):
                    if j + 2 < nj:
                        qk(j + 2)
                    pv(j)
                gcnt[0] += nj
                recip(rl, bank(4), ["pl"], ["rl"])
                for rc in range(2):
                    tt(olat[:, rc, :], bank(2 + rc), rl, ALU.mult, [("po", rc), "rl"], [("olat", rc)])
                pvb = bank(5 + hh % 2)
                for rc in range(2):
                    mm(pvb, wUV[:, hh, rc, :], olat[:, rc, :], rc == 0, rc == 1, [("wUV", hh), ("olat", 0), ("olat", 1)], [("pv", hh % 2)])
                ob = on[hh % 2]
                cp(ob, pvb, [("pv", hh % 2)], [("on", hh % 2)], eng="act")
                dma(ST_ENG, OT[:, hh, qb * 512:(qb + 1) * 512], ob, [("on", hh % 2)], [("OT", hh, qb)], key=("onst", hh % 2))

        out_proj(Wb["b_w_out"], h_src, h_dst, 1, 0, dbg_dst)

    try:
        p0()
        t0()
        diff_attention(x, H[0], dbg.get("h0_0"))
        cross_attention(0, H[0], H[1], dbg.get("h0_1"))
        moe(0, H[1], H[0], dbg.get("h0_2"))
        dsa(H[0], H[1], dbg.get("h1_0"))
        cross_attention(1, H[1], H[0], dbg.get("h1_1"))
        moe(1, H[0], out, None)
    except StopBuild:
        pass
    S.barrier()
    S.emit()
    st.close()
    return nc, S


_CACHE = {}


def make_in_maps(inputs, NT, ncores):
    cf, cb = _consts()
    maps = []
    f = lambda a: np.ascontiguousarray(a, dtype=np.float32)
    for b in range(ncores):
        m = {
            "x": f(inputs["x"][b, :NT]), "mem": f(inputs["mem"][b]),
            "positions": np.ascontiguousarray(inputs["positions"][b, :NT].reshape(NT // 128, 128).astype(np.int32)),
            "a_w_in": f(inputs["a_w_in"][0]), "a_lambda": f(inputs["a_lambda"][0]),
            "a_subln_g": f(inputs["a_subln_g"][0].reshape(128, 1)), "a_w_out": f(inputs["a_w_out"][0]),
            "b_w_in": f(inputs["b_w_in"][0]), "b_q_norm_g": f(inputs["b_q_norm_g"][0]), "b_kv_norm_g": f(inputs["b_kv_norm_g"][0]),
            "b_w_uq": f(inputs["b_w_uq"][0]), "b_w_qidx": f(inputs["b_w_qidx"][0]), "b_w_uk": f(inputs["b_w_uk"][0]),
            "b_w_uv": f(inputs["b_w_uv"][0]), "b_w_out": f(inputs["b_w_out"][0]),
            "mem_w_kv": f(inputs["mem_w_kv"]), "xa_w_q": f(inputs["xa_w_q"]), "xa_w_out": f(inputs["xa_w_out"]),
            "moe_w_group": f(inputs["moe_w_group"]), "moe_b_group": f(inputs["moe_b_group"]),
            "moe_w_expert": f(inputs["moe_w_expert"]), "moe_b_expert": f(inputs["moe_b_expert"]),
            "moe_w_gate": f(inputs["moe_w_gate"]), "moe_w_up": f(inputs["moe_w_up"]), "moe_w_down": f(inputs["moe_w_down"]),
            "ln_g": f(inputs["ln_g"]), "ln_b": f(inputs["ln_b"]),
            "cstf": cf, "cstb": cb,
        }
        maps.append(m)
    return maps


def kernel(**inputs):
    NT = inputs["x"].shape[1]
    nb = inputs["x"].shape[0]
    if NT not in _CACHE:
        _CACHE[NT] = build(NT)[0]
    nc = _CACHE[NT]
    maps = make_in_maps(inputs, NT, nb)
    res = run_bass_kernel_spmd(nc, maps, core_ids=list(range(nb)))
    return np.stack([np.asarray(r["out"], dtype=np.float32) for r in res.results], axis=0)
```

```python
import math
import contextlib
import numpy as np
import ml_dtypes
import concourse.bass as bass
import concourse.mybir as mybir
from concourse.bass_utils import run_bass_kernel_spmd

F32 = mybir.dt.float32
BF16 = mybir.dt.bfloat16
I32 = mybir.dt.int32
U8 = mybir.dt.uint8
AF = mybir.ActivationFunctionType
ALU = mybir.AluOpType
AX = mybir.AxisListType

D = 1024
DEPTH = 2
ALPHA = (2.0 * DEPTH) ** 0.25
LN_EPS = 1e-5
ROPE_THETA = 500000.0
NEG = -30000.0
ENGS = ("pe", "act", "dve", "pool", "sp")
NPOOL = 88
ST_ENG = "sp"


PSUM_NAMES = {"ps0", "psb", "pk", "pv", "pt", "pa", "ps", "po", "pl", "pm", "pA", "psc", "ptp", "pmix", "p1", "p2", "pg", "pu",
              "py", "e_pt", "p0", "pt2", "pq", "pqi", "pt7", "pw", "pscore", "plg"}


def is_psum(key):
    base = key if isinstance(key, str) else key[0]
    return base in PSUM_NAMES


class Op:
    __slots__ = ("eng", "fn", "is_dma", "deps", "sem", "val", "signal", "waits", "semidx")

    def __init__(self, eng, fn, is_dma, semidx):
        self.eng = eng
        self.fn = fn
        self.is_dma = is_dma
        self.deps = []
        self.sem = None
        self.val = 0
        self.signal = False
        self.waits = []
        self.semidx = semidx


class Sched:
    def __init__(self, nc):
        self.nc = nc
        self.ops = []
        self.last_write = {}
        self.reads_since = {}
        self.keymap = {}
        self.dmas_since = []
        self.last_on = {}

    def add(self, eng, fn, reads=(), writes=(), dma=False, semkey=None):
        semidx = None
        if dma:
            if semkey is None:
                semkey = writes[0]
            if semkey not in self.keymap:
                assert len(self.keymap) < NPOOL, "too many dma sem keys in phase"
                self.keymap[semkey] = len(self.keymap)
            semidx = self.keymap[semkey]
        op = Op(eng, fn, dma, semidx)
        reads_eff = [r for r in reads if not is_psum(r)]
        writes_eff = list(writes) + [r for r in reads if is_psum(r)]
        deps = {}
        for r in reads_eff:
            for w in self.last_write.get(r, {}).values():
                deps[id(w)] = (w, "raw")
        for r in writes_eff:
            for w in self.last_write.get(r, {}).values():
                if id(w) not in deps:
                    deps[id(w)] = (w, "waw")
            for rd in self.reads_since.get(r, ()):
                if id(rd) not in deps:
                    deps[id(rd)] = (rd, "war")
        for d, kind in deps.values():
            if (not d.is_dma) and (not dma) and d.eng == eng:
                if eng == "pe":
                    continue
                if kind != "raw" and eng != "pool":
                    continue
            op.deps.append(d)
        for r in reads_eff:
            self.reads_since.setdefault(r, []).append(op)
        for r in writes_eff:
            self.last_write.setdefault(r, {})["dma" if dma else eng] = op
            self.reads_since[r] = []
        self.ops.append(op)
        if dma:
            self.dmas_since.append(op)
        else:
            self.last_on[eng] = op
        return op

    def barrier(self):
        lasts = [o for o in self.last_on.values() if o.fn is not None] + list(self.dmas_since)
        for e in ENGS:
            op = Op(e, None, False, None)
            op.deps = list(lasts)
            self.ops.append(op)
        self.last_write = {}
        self.reads_since = {}
        self.keymap = {}
        self.dmas_since = []
        self.last_on = {}

    def emit(self):
        nc = self.nc
        for op in self.ops:
            for d in op.deps:
                d.signal = True
        stack = contextlib.ExitStack()
        eng_sem = {e: stack.enter_context(nc.semaphore("s_" + e)) for e in ENGS}
        npool = max([o.semidx for o in self.ops if o.is_dma] + [0]) + 1
        pool = [stack.enter_context(nc.semaphore("d_%d" % i)) for i in range(npool)]
        counts = {}
        for op in self.ops:
            if op.is_dma:
                k = ("d", op.semidx)
                op.sem = pool[op.semidx]
                counts[k] = counts.get(k, 0) + 16
                op.val = counts[k]
                op.signal = True
            else:
                op.sem = eng_sem[op.eng]
                if op.signal and op.fn is not None:
                    counts[op.eng] = counts.get(op.eng, 0) + 1
                op.val = counts.get(op.eng, 0)
        waited = {e: {} for e in ENGS}
        per_eng = {e: [] for e in ENGS}
        nw = 0
        for op in self.ops:
            w = waited[op.eng]
            need = {}
            for d in op.deps:
                key = id(d.sem)
                if w.get(key, 0) >= d.val:
                    continue
                if key not in need or need[key][1] < d.val:
                    need[key] = (d.sem, d.val)
            for key, (sem, val) in need.items():
                w[key] = val
                op.waits.append((sem, val))
            nw += len(op.waits)
            per_eng[op.eng].append(op)
        self.stats = {e: len(per_eng[e]) for e in ENGS}
        self.stats["waits"] = nw
        self.stats["maxsem"] = max(counts.values()) if counts else 0

        def run(eng_obj, lst):
            for op in lst:
                for sem, val in op.waits:
                    eng_obj.wait_ge(sem, val)
                if op.fn is None:
                    continue
                ins = op.fn(eng_obj)
                if op.signal:
                    ins.then_inc(op.sem, 16 if op.is_dma else 1)

        with nc.Block() as block:
            @block.tensor
            def _(e):
                run(e, per_eng["pe"])

            @block.scalar
            def _(e):
                run(e, per_eng["act"])

            @block.vector
            def _(e):
                run(e, per_eng["dve"])

            @block.gpsimd
            def _(e):
                run(e, per_eng["pool"])

            @block.sync
            def _(e):
                run(e, per_eng["sp"])
        stack.close()


def _inv_freq(rot):
    return (np.float32(ROPE_THETA) ** (-np.arange(0, rot, 2, dtype=np.float32) / np.float32(rot))).astype(np.float32)


CF_ID = 0
CF_ONES = 128
CF_INVF = 256
CF_SEL = 288
CF_HM = 288 + 1024
CF_N = CF_HM + 16
CB_ID = 0
CB_ONES = 128
CB_E = 256
CB_N = 256 + 1024


def _consts():
    cf = np.zeros((128, CF_N), np.float32)
    cf[:, CF_ID:CF_ID + 128] = np.eye(128, dtype=np.float32)
    cf[:, CF_ONES:CF_ONES + 128] = 1.0
    cf[:, CF_INVF:CF_INVF + 32] = np.concatenate([_inv_freq(16), _inv_freq(32), _inv_freq(16)])[None, :]
    cb = np.zeros((128, CB_N), np.float32)
    cb[:, CB_ID:CB_ID + 128] = np.eye(128, dtype=np.float32)
    cb[:, CB_ONES:CB_ONES + 128] = 1.0
    p = np.arange(128)
    j, q = p // 16, p % 16
    for g in range(8):
        cf[16 * g + q, CF_SEL + g * 128 + p] = 1.0
        cb[p, CB_E + g * 128 + 16 * g + q] = 1.0
    for h in range(16):
        cf[:, CF_HM + h] = (h // 2 == j).astype(np.float32)
    return cf, cb.astype(ml_dtypes.bfloat16)


class B:
    pass


class StopBuild(Exception):
    pass


def build(NT, debug=(), upto=99):
    NTL = NT // 128
    NQB = NT // 512
    nc = bass.Bass("TRN2", target_bir_lowering=False)
    S = Sched(nc)
    st = contextlib.ExitStack()

    def din(name, shape, dt=F32):
        return nc.dram_tensor(name, list(shape), dt, kind="ExternalInput").ap()

    def dscr(name, shape, dt):
        return nc.dram_tensor(name, list(shape), dt, kind="Internal").ap()

    x = din("x", [NT, D])
    mem = din("mem", [256, D])
    positions = din("positions", [NTL, 128], I32)
    a_w_in = din("a_w_in", [D, 3072]); a_lambda = din("a_lambda", [4, 64]); a_subln_g = din("a_subln_g", [128, 1])
    a_w_out = din("a_w_out", [D, D])
    b_w_in = din("b_w_in", [D, 624]); b_q_norm_g = din("b_q_norm_g", [256]); b_kv_norm_g = din("b_kv_norm_g", [256])
    b_w_uq = din("b_w_uq", [256, 1024]); b_w_qidx = din("b_w_qidx", [256, 1024])
    b_w_uk = din("b_w_uk", [8, 256, 96]); b_w_uv = din("b_w_uv", [8, 256, 128]); b_w_out = din("b_w_out", [D, D])
    mem_w_kv = din("mem_w_kv", [D, 2048]); xa_w_q = din("xa_w_q", [2, D, D]); xa_w_out = din("xa_w_out", [2, D, D])
    moe_w_group = din("moe_w_group", [2, D, 4]); moe_b_group = din("moe_b_group", [2, 4])
    moe_w_expert = din("moe_w_expert", [2, D, 16]); moe_b_expert = din("moe_b_expert", [2, 16])
    moe_w_gate = din("moe_w_gate", [2, 16, D, 512]); moe_w_up = din("moe_w_up", [2, 16, D, 512])
    moe_w_down = din("moe_w_down", [2, 16, 512, D])
    ln_g = din("ln_g", [2, 3, D]); ln_b = din("ln_b", [2, 3, D])
    cstf = din("cstf", [128, CF_N]); cstb = din("cstb", [128, CB_N], BF16)
    out = nc.dram_tensor("out", [NT, D], F32, kind="ExternalOutput").ap()
    dbg = {k: nc.dram_tensor("dbg_" + k, [NT, D], F32, kind="ExternalOutput").ap() for k in debug}

    Wb = {
        "a_w_in": dscr("wb_a_w_in", [D, 3072], BF16), "a_w_out": dscr("wb_a_w_out", [D, D], BF16),
        "b_w_in": dscr("wb_b_w_in", [D, 624], BF16), "b_w_uq": dscr("wb_b_w_uq", [256, 1024], BF16),
        "b_w_qidx": dscr("wb_b_w_qidx", [256, 1024], BF16), "b_w_uk": dscr("wb_b_w_uk", [8, 256, 96], BF16),
        "b_w_uv": dscr("wb_b_w_uv", [8, 256, 128], BF16), "b_w_out": dscr("wb_b_w_out", [D, D], BF16),
        "mem_w_kv": dscr("wb_mem_w_kv", [D, 2048], BF16), "xa_w_q": dscr("wb_xa_w_q", [2, D, D], BF16),
        "xa_w_out": dscr("wb_xa_w_out", [2, D, D], BF16),
        "moe_w_gate": dscr("wb_moe_w_gate", [2, 16, D, 512], BF16), "moe_w_up": dscr("wb_moe_w_up", [2, 16, D, 512], BF16),
        "moe_w_down": dscr("wb_moe_w_down", [2, 16, 512, D], BF16),
    }
    H = [dscr("H0", [NT, D], F32), dscr("H1", [NT, D], F32)]
    HT = dscr("HT", [128, 8, NT], BF16)
    QT = dscr("QT", [128, 8, NT], BF16)
    KT = dscr("KT", [128, 8, NT], BF16)
    V = dscr("V", [NT, D], BF16)
    OT = dscr("OT", [128, 8, NT], BF16)
    ROPE = dscr("ROPE", [NT, 128], F32)
    KVT = dscr("KVT", [128, 3, NT], BF16)
    CKV = dscr("CKV", [NT, 256], BF16)
    KIT = dscr("KIT", [128, NT], BF16)
    QFT = dscr("QFT", [128, 8, 3, NT], BF16)
    QIT = dscr("QIT", [128, NTL, 1024], BF16)
    WIX = dscr("WIX", [NT, 16], F32)
    NM = dscr("NM", [NT, NT], BF16)

    ARENA = 200 * 1024
    arena = st.enter_context(nc.sbuf_tensor("arena", [128, ARENA], U8))
    PS = st.enter_context(nc.psum_tensor("ps", [128, 4096], F32))
    state = {"off": 0, "mark": 0, "phase": "p0"}

    def sb(name, shape, dt):
        esz = 4 if dt in (F32, I32) else 2
        n = int(np.prod(shape[1:])) * esz
        n = (n + 63) // 64 * 64
        off = state["off"]
        assert off + n <= ARENA, "arena overflow in %s: %s" % (state["phase"], name)
        state["off"] = off + n
        v = arena[:, off:off + n].bitcast(dt)[:, 0:int(np.prod(shape[1:]))]
        if len(shape) == 3:
            v = v.rearrange("p (a b) -> p a b", a=shape[1])
        elif len(shape) == 4:
            v = v.rearrange("p (a b c) -> p a b c", a=shape[1], b=shape[2])
        return v

    def bank(i, n=1):
        return PS[:, i * 512:(i + n) * 512]

    def bank_bf(i):
        return PS[:, i * 512:(i + 1) * 512].bitcast(BF16)

    def phase(name):
        state["np"] = state.get("np", 0) + 1
        if upto >= 0 and state["np"] > upto:
            raise StopBuild()
        S.barrier()
        state["off"] = state["mark"]
        state["phase"] = name

    def mm(o, lhsT, rhs, start, stop, R, W):
        S.add("pe", lambda e: e.matmul(o, lhsT=lhsT, rhs=rhs, start=start, stop=stop), reads=R, writes=W)

    def tr(o, i, ident, R, W):
        S.add("pe", lambda e: e.transpose(out=o, in_=i, identity=ident), reads=R, writes=W)

    def act(o, i, func, R, W, bias=None, scale=None, accum=None, eng="act"):
        kw = {}
        if bias is not None:
            kw["bias"] = bias
        if scale is not None:
            kw["scale"] = scale
        if accum is not None:
            kw["accum_out"] = accum
        S.add("act", lambda e: e.activation(out=o, in_=i, func=func, **kw), reads=R, writes=W)

    def tt(o, a, b, op, R, W, eng="dve"):
        S.add(eng, lambda e: e.tensor_tensor(out=o, in0=a, in1=b, op=op), reads=R, writes=W)

    def ts(o, a, s1, s2, op0, op1, R, W, accum=None, eng="dve"):
        kw = {}
        if accum is not None:
            kw["accum_out"] = accum
        if op1 is None:
            S.add(eng, lambda e: e.tensor_scalar(out=o, in0=a, scalar1=s1, scalar2=None, op0=op0, **kw), reads=R, writes=W)
        else:
            S.add(eng, lambda e: e.tensor_scalar(out=o, in0=a, scalar1=s1, scalar2=s2, op0=op0, op1=op1, **kw), reads=R, writes=W)

    def stt(o, a, s, b, op0, op1, R, W):
        S.add("dve", lambda e: e.scalar_tensor_tensor(out=o, in0=a, scalar=s, in1=b, op0=op0, op1=op1), reads=R, writes=W)

    def cp(o, i, R, W, eng="dve"):
        if eng == "act":
            S.add("act", lambda e: e.activation(out=o, in_=i, func=AF.Copy), reads=R, writes=W)
        else:
            S.add(eng, lambda e: e.tensor_copy(out=o, in_=i), reads=R, writes=W)

    def red(o, i, op, R, W, negate=False):
        S.add("dve", lambda e: e.tensor_reduce(out=o, in_=i, axis=AX.X, op=op, negate=negate), reads=R, writes=W)

    def recip(o, i, R, W):
        S.add("dve", lambda e: e.reciprocal(out=o, in_=i), reads=R, writes=W)

    def memset(o, v, W, eng="pool", R=()):
        S.add(eng, lambda e: e.memset(o, v), reads=R, writes=W)

    def dma(eng, o, i, R, W, key=None, slow=False):
        if slow:
            S.add(eng, lambda e: e.dma_start(out=o, in_=i, allow_slow_non_contiguous=True), reads=R, writes=W, dma=True, semkey=key)
        else:
            S.add(eng, lambda e: e.dma_start(out=o, in_=i), reads=R, writes=W, dma=True, semkey=key)

    cf = sb("cf", [128, CF_N], F32)
    cb = sb("cb", [128, CB_N], BF16)
    epsc = sb("epsc", [128, 1], F32)
    memKT = sb("memKT", [128, 8, 256], BF16)
    memV = sb("memV", [128, 2, 1024], BF16)
    comb = sb("comb", [128, NTL, 16], F32)
    gbc = sb("gbc", [128, 1024], F32)
    bbc = sb("bbc", [128, 1024], F32)
    neglam = sb("neglam", [128, 1], F32)
    gsc = sb("gsc", [128, 1], F32)
    state["mark"] = state["off"]
    ident = cb[:, CB_ID:CB_ID + 128]
    ones_b = cb[:, CB_ONES:CB_ONES + 128]
    ident32 = cf[:, CF_ID:CF_ID + 128]
    ones32 = cf[:, CF_ONES:CF_ONES + 128]

    def sub(k):
        if upto == -k:
            raise StopBuild()

    def p0():
        dma("sp", cf, cstf, [], ["cf"])
        dma("sp", cb, cstb, [], ["cb"])
        memset(epsc, LN_EPS, ["epsc"])
        ci = [0]

        def cast(dst, src):
            dma("pool", dst, src, [], [("cast", ci[0])], key=("cast", ci[0] % 8))
            ci[0] += 1

        cast(Wb["a_w_in"], a_w_in); cast(Wb["a_w_out"], a_w_out); cast(Wb["mem_w_kv"], mem_w_kv)
        for i in range(2):
            cast(Wb["xa_w_q"][i], xa_w_q[i]); cast(Wb["xa_w_out"][i], xa_w_out[i])
        cast(Wb["b_w_in"], b_w_in); cast(Wb["b_w_uq"], b_w_uq); cast(Wb["b_w_qidx"], b_w_qidx)
        cast(Wb["b_w_uk"].rearrange("h r n -> (h r) n"), b_w_uk.rearrange("h r n -> (h r) n"))
        cast(Wb["b_w_uv"].rearrange("h r n -> (h r) n"), b_w_uv.rearrange("h r n -> (h r) n"))
        cast(Wb["b_w_out"], b_w_out)
        for i in range(2):
            for e in range(16):
                cast(Wb["moe_w_gate"][i, e], moe_w_gate[i, e]); cast(Wb["moe_w_up"][i, e], moe_w_up[i, e])
                cast(Wb["moe_w_down"][i, e], moe_w_down[i, e])

        sub(1)
        posi = sb("posi", [128, 128], I32)
        posf = sb("posf", [128, 128], F32)
        post = sb("post", [128, NTL], F32)
        ang = sb("ang", [128, NTL, 32], F32)
        kk = sb("kk", [128, NTL, 32], F32)
        sn = sb("sn", [128, NTL, 32], F32)
        cs = sb("cs", [128, NTL, 32], F32)
        tab = sb("tab", [128, NTL, 128], F32)
        dma("sp", posi[0:NTL, :], positions, [], ["posi"])
        cp(posf[0:NTL, :], posi[0:NTL, :], ["posi"], ["posf"])
        S.add("pe", lambda e: e.transpose(out=bank(0)[:, 0:NTL], in_=posf[0:NTL, :], identity=ident32[0:NTL, 0:NTL]),
              reads=["posf", "cf"], writes=["ps0"])
        cp(post, bank(0)[:, 0:NTL], ["ps0"], ["post"])
        invf = cf[:, CF_INVF:CF_INVF + 32]
        tt(ang, post.unsqueeze(2).to_broadcast([128, NTL, 32]), invf.unsqueeze(1).to_broadcast([128, NTL, 32]), ALU.mult,
           ["post", "cf"], ["ang"])
        MAGIC = 12582912.0
        TWO_PI = 2.0 * math.pi
        C1 = 6.28125
        C2 = float(np.float32(TWO_PI - C1))
        ts(kk, ang, 1.0 / TWO_PI, MAGIC, ALU.mult, ALU.add, ["ang"], ["kk"])
        ts(kk, kk, -MAGIC, None, ALU.add, None, ["kk"], ["kk"])
        stt(ang, kk, -C1, ang, ALU.mult, ALU.add, ["kk", "ang"], ["ang"])
        stt(ang, kk, -C2, ang, ALU.mult, ALU.add, ["kk", "ang"], ["ang"])
        ts(ang, ang, math.pi, -math.pi, ALU.min, ALU.max, ["ang"], ["ang"])
        act(sn, ang, AF.Sin, ["ang"], ["sn"])
        stt(kk, ang, -1.0, ang, ALU.mult, ALU.max, ["ang"], ["kk"])
        ts(kk, kk, -1.0, math.pi / 2, ALU.mult, ALU.add, ["kk"], ["kk"])
        act(cs, kk, AF.Sin, ["kk"], ["cs"])
        for (o0, f0, nf) in ((0, 0, 8), (32, 8, 16), (96, 24, 8)):
            cp(tab[:, :, o0:o0 + nf], cs[:, :, f0:f0 + nf], ["cs"], [("tab", o0, 0)])
            cp(tab[:, :, o0 + nf:o0 + 2 * nf], cs[:, :, f0:f0 + nf], ["cs"], [("tab", o0, 1)])
            ts(tab[:, :, o0 + 2 * nf:o0 + 3 * nf], sn[:, :, f0:f0 + nf], -1.0, None, ALU.mult, None, ["sn"], [("tab", o0, 2)])
            cp(tab[:, :, o0 + 3 * nf:o0 + 4 * nf], sn[:, :, f0:f0 + nf], ["sn"], [("tab", o0, 3)])
        dma("sp", ROPE.rearrange("(t p) c -> p t c", p=128), tab,
            [("tab", o0, k) for o0 in (0, 32, 96) for k in range(4)], ["ROPE"])

        sub(2)
        lam_init0 = 0.8 - 0.6 * math.exp(-0.3 * 0)
        lamb = sb("lamb", [128, 4, 64], F32)
        lamp = sb("lamp", [128, 2, 64], F32)
        lams = sb("lams", [128, 2], F32)
        dma("sp", lamb.rearrange("p a b -> p (a b)"), a_lambda.rearrange("a b -> (a b)").partition_broadcast(128), [], ["lamb"])
        tt(lamp[:, 0, :], lamb[:, 0, :], lamb[:, 1, :], ALU.mult, ["lamb"], [("lamp", 0)])
        tt(lamp[:, 1, :], lamb[:, 2, :], lamb[:, 3, :], ALU.mult, ["lamb"], [("lamp", 1)])
        red(lams, lamp, ALU.add, [("lamp", 0), ("lamp", 1)], ["lams"])
        act(lams, lams, AF.Exp, ["lams"], ["lams"])
        tt(neglam, lams[:, 1:2], lams[:, 0:1], ALU.subtract, ["lams"], ["neglam"])
        ts(neglam, neglam, -lam_init0, None, ALU.add, None, ["neglam"], ["neglam"])
        dma("sp", gsc, a_subln_g, [], ["gsc"])
        ts(gsc, gsc, 1.0 - lam_init0, None, ALU.mult, None, ["gsc"], ["gsc"])

        sub(3)
        S.barrier()
        state["phase"] = "p0b"
        wkv = sb("wkv", [128, 8, 2048], BF16)
        memf = sb("memf", [128, 2, 1024], F32)
        memb = sb("memb", [128, 2, 1024], BF16)
        memT = sb("memT", [128, 8, 256], BF16)
        dma("sp", wkv, Wb["mem_w_kv"].rearrange("(c p) n -> p c n", p=128), [], ["wkv"])
        dma("sp", memf, mem.rearrange("(t p) d -> p t d", p=128), [], ["memf"])
        cp(memb, memf, ["memf"], ["memb"], eng="act")
        for mt in range(2):
            ptv = bank_bf(mt).rearrange("p (c t) -> p c t", c=8)
            for c in range(8):
                tr(ptv[:, c, :], memb[:, mt, c * 128:(c + 1) * 128], ident, ["memb"], [("psb", mt)])
            cp(memT[:, :, mt * 128:(mt + 1) * 128], ptv, [("psb", mt)], [("memT", mt)])
        for j in range(8):
            pk = bank(2 + j % 2)[:, 0:256]
            for k in range(8):
                mm(pk, wkv[:, k, j * 128:(j + 1) * 128], memT[:, k, :], k == 0, k == 7, ["wkv", ("memT", 0), ("memT", 1)], [("pk", j % 2)])
            cp(memKT[:, j, :], pk, [("pk", j % 2)], [("memKT", j)], eng="act" if j % 2 else "dve")
        for mt in range(2):
            for hf in range(2):
                pv = bank(4 + (mt * 2 + hf) % 2)
                for k in range(8):
                    mm(pv, memT[:, k, mt * 128:(mt + 1) * 128], wkv[:, k, 1024 + hf * 512:1024 + (hf + 1) * 512], k == 0, k == 7,
                       ["wkv", ("memT", 0), ("memT", 1)], [("pv", (mt * 2 + hf) % 2)])
                cp(memV[:, mt, hf * 512:(hf + 1) * 512], pv, [("pv", (mt * 2 + hf) % 2)], [("memV", mt, hf)], eng="act" if hf else "dve")


    def load_ln(li, j):
        dma("sp", gbc, ln_g[li, j].partition_broadcast(128), [], ["gbc"])
        dma("sp", bbc, ln_b[li, j].partition_broadcast(128), [], ["bbc"])

    class Epi:
        def __init__(self, pt_bank, nbuf=2):
            self.nb = nbuf
            self.hsb = [sb("e_h%d" % i, [128, 1024], F32) for i in range(nbuf)]
            self.z = [sb("e_z%d" % i, [128, 1024], F32) for i in range(nbuf)]
            self.hb = [sb("e_hb%d" % i, [128, 1024], BF16) for i in range(nbuf)]
            self.hT = [sb("e_hT%d" % i, [128, 8, 128], BF16) for i in range(nbuf)]
            self.st = [sb("e_st%d" % i, [128, 2, 6], F32) for i in range(nbuf)]
            self.mv = [sb("e_mv%d" % i, [128, 2], F32) for i in range(nbuf)]
            self.rs = [sb("e_rs%d" % i, [128, 1], F32) for i in range(nbuf)]
            self.ptb = pt_bank
            self.n = 0

        def run(self, t, mix, mixR, h_src, h_dst, dbg_dst=None):
            b = self.n % self.nb
            self.n += 1
            hsb, z, hb, hT, stt_, mv, rs = self.hsb[b], self.z[b], self.hb[b], self.hT[b], self.st[b], self.mv[b], self.rs[b]
            k = "e%d" % b
            rows = slice(t * 128, (t + 1) * 128)
            dma("sp", hsb, h_src[rows, :], [("Hsrc", t)], [k + "h"])
            stt(z, hsb, ALPHA, mix, ALU.mult, ALU.add, [k + "h"] + mixR, [k + "z"])
            for c in range(2):
                S.add("dve", lambda e, c=c: e.bn_stats(out=stt_[:, c, :], in_=z[:, c * 512:(c + 1) * 512]), reads=[k + "z"], writes=[(k + "st", c)])
            S.add("dve", lambda e: e.bn_aggr(out=mv, in_=stt_.rearrange("p a b -> p (a b)")), reads=[(k + "st", 0), (k + "st", 1)], writes=[k + "mv"])
            act(rs, mv[:, 1:2], AF.Sqrt, [k + "mv"], [k + "rs"], bias=epsc)
            recip(rs, rs, [k + "rs"], [k + "rs"])
            ts(z, z, mv[:, 0:1], rs, ALU.subtract, ALU.mult, [k + "z", k + "mv", k + "rs"], [k + "z"])
            tt(z, z, gbc, ALU.mult, [k + "z", "gbc"], [k + "z"], eng="pool")
            tt(hsb, z, bbc, ALU.add, [k + "z", "bbc"], [k + "h"])
            dma(ST_ENG, h_dst[rows, :], hsb, [k + "h"], [("Hdst", t)], key=k + "hst")
            if dbg_dst is not None:
                dma(ST_ENG, dbg_dst[rows, :], hsb, [k + "h"], [("dbg", t)], key=k + "dbg")
            cp(hb, hsb, [k + "h"], [k + "hb"], eng="act")
            ptv = bank_bf(self.ptb).rearrange("p (c t) -> p c t", c=8)
            for c in range(8):
                tr(ptv[:, c, :], hb[:, c * 128:(c + 1) * 128], ident, [k + "hb"], ["e_pt"])
            cp(hT, ptv, ["e_pt"], [k + "hT"], eng="act")
            dma(ST_ENG, HT[:, :, rows], hT, [k + "hT"], [("HT", t)], key=k + "hTst")

    def t0():
        phase("t0")
        xs = [sb("xs%d" % i, [128, 1024], F32) for i in range(2)]
        xb = [sb("xb%d" % i, [128, 1024], BF16) for i in range(2)]
        xT = [sb("xT%d" % i, [128, 8, 128], BF16) for i in range(2)]
        for t in range(NTL):
            b = t % 2
            rows = slice(t * 128, (t + 1) * 128)
            dma("sp", xs[b], x[rows, :], [], [("xs", b)])
            cp(xb[b], xs[b], [("xs", b)], [("xb", b)], eng="act")
            ptv = bank_bf(b).rearrange("p (c t) -> p c t", c=8)
            for c in range(8):
                tr(ptv[:, c, :], xb[b][:, c * 128:(c + 1) * 128], ident, [("xb", b)], [("pt", b)])
            cp(xT[b], ptv, [("pt", b)], [("xT", b)])
            dma(ST_ENG, HT[:, :, rows], xT[b], [("xT", b)], [("HT", t)], key=("xTst", b))


    def rope(dst, src, nh, dh, rt, off, hf, tA, tB, R, W, key):
        cc = rt[:, off:off + 2 * hf].unsqueeze(1).to_broadcast([128, nh, 2 * hf])
        s1 = rt[:, off + 2 * hf:off + 3 * hf].unsqueeze(1).to_broadcast([128, nh, hf])
        s2 = rt[:, off + 3 * hf:off + 4 * hf].unsqueeze(1).to_broadcast([128, nh, hf])
        tt(tA[:, 0:nh, 0:2 * hf], src[:, :, 0:2 * hf], cc, ALU.mult, R, [key + "A"])
        tt(tB[:, 0:nh, 0:hf], src[:, :, hf:2 * hf], s1, ALU.mult, R, [key + "B0"])
        tt(tB[:, 0:nh, hf:2 * hf], src[:, :, 0:hf], s2, ALU.mult, R, [key + "B1"])
        tt(dst[:, :, 0:2 * hf], tA[:, 0:nh, 0:2 * hf], tB[:, 0:nh, 0:2 * hf], ALU.add, [key + "A", key + "B0", key + "B1"], W)
        if dh > 2 * hf:
            cp(dst[:, :, 2 * hf:dh], src[:, :, 2 * hf:dh], R, W, eng="act")

    def diff_attention(h_src, h_dst, dbg_dst):
        phase("a1")
        wA = sb("wA", [128, 8, 3072], BF16)
        hts = [sb("hts%d" % i, [128, 8, 128], BF16) for i in range(2)]
        rts = [sb("rts%d" % i, [128, 128], F32) for i in range(2)]
        tok = [sb("tok%d" % i, [128, 16, 64], BF16) for i in range(2)]
        vtok = [sb("vtok%d" % i, [128, 1024], BF16) for i in range(2)]
        tA = sb("tA", [128, 16, 32], F32)
        tB = sb("tB", [128, 16, 32], F32)
        oT = [sb("oT%d" % i, [128, 8, 128], BF16) for i in range(2)]
        dma("sp", wA, Wb["a_w_in"].rearrange("(c p) n -> p c n", p=128), [], ["wA"])
        n = 0
        for t in range(NTL):
            b = t % 2
            rows = slice(t * 128, (t + 1) * 128)
            dma("sp", hts[b], HT[:, :, rows], [], [("hts", b)])
            dma("sp", rts[b], ROPE[rows, :], [], [("rts", b)])
            for which in range(3):
                pb = 2 * (n % 2)
                pa = bank(pb, 2)
                for hf in range(2):
                    for k in range(8):
                        mm(pa[:, hf * 512:(hf + 1) * 512], hts[b][:, k, :], wA[:, k, which * 1024 + hf * 512: which * 1024 + (hf + 1) * 512],
                           k == 0, k == 7, [("hts", b), "wA"], [("pa", pb, hf)])
                paR = [("pa", pb, 0), ("pa", pb, 1)]
                if which < 2:
                    tb_ = tok[n % 2]
                    rope(tb_, pa.rearrange("p (h d) -> p h d", h=16), 16, 64, rts[b], 0, 8, tA, tB, paR + [("rts", b)], [("tok", n % 2)], "r")
                    ptb = 4 + n % 2
                    ptv = bank_bf(ptb).rearrange("p (c t) -> p c t", c=8)
                    tf = tb_.rearrange("p h d -> p (h d)")
                    for c in range(8):
                        tr(ptv[:, c, :], tf[:, c * 128:(c + 1) * 128], ident, [("tok", n % 2)], [("pt", ptb)])
                    ob = oT[n % 2]
                    if which == 0:
                        act(ob, ptv, AF.Copy, [("pt", ptb)], [("oT", n % 2)], scale=0.125)
                    else:
                        cp(ob, ptv, [("pt", ptb)], [("oT", n % 2)])
                    dma(ST_ENG, (QT if which == 0 else KT)[:, :, rows], ob, [("oT", n % 2)], [("QK", which, t)], key=("oTst", n % 2))
                else:
                    cp(vtok[b], pa, paR, [("vtok", b)], eng="act")
                    dma(ST_ENG, V[rows, :], vtok[b], [("vtok", b)], [("V", t)], key=("vst", b))
                n += 1

        phase("a2")
        KTh = sb("KTh", [128, NT], BF16)
        QTh = [sb("QTh%d" % c, [128, NT], BF16) for c in range(2)]
        Vh = sb("Vh", [128, NTL, 128], BF16)
        memset(QTh[0][64:128, :], 0.0, [("QThz", 0)])
        memset(QTh[1][0:64, :], 0.0, [("QThz", 1)])
        NPT = 4
        pT = [sb("pT%d" % i, [128, 512], BF16) for i in range(NPT)]
        r0 = sb("r0", [128, 512], F32); r1 = sb("r1", [128, 512], F32)
        a0 = sb("a0", [128, 512], F32); a1 = sb("a1", [128, 512], F32)
        od = sb("od", [128, 512], F32); sq = sb("sq", [128, 512], F32); rstd = sb("rstd", [128, 512], F32)
        on = [sb("on%d" % i, [128, 512], BF16) for i in range(2)]
        cnt_u = [0]
        for h in range(8):
            dma("sp", KTh, KT[:, h, :], [], ["KTh"])
            dma("sp", QTh[0][0:64, :], QT[0:64, h, :], [], [("QTh", 0)])
            dma("sp", QTh[1][64:128, :], QT[64:128, h, :], [], [("QTh", 1)])
            dma("sp", Vh, V[:, h * 128:(h + 1) * 128].rearrange("(t p) e -> p t e", p=128), [], ["Vh"])
            for qb in range(NQB):
                nj = 4 * qb + 4
                units = [(j, c) for j in range(nj) for c in range(2)]
                base = cnt_u[0]

                def qk(i, qb=qb, base=base, units=units):
                    j, c = units[i]
                    g = base + i
                    c0 = 0 if j < 4 * qb else 128 * (j - 4 * qb)
                    sbk = g % 4
                    ps_ = bank(sbk)
                    mm(ps_[:, c0:512], KTh[:, j * 128:(j + 1) * 128], QTh[c][:, qb * 512 + c0:(qb + 1) * 512], True, True,
                       ["KTh", ("QTh", c), ("QThz", c)], [("ps", sbk)])
                    pt_ = pT[g % NPT]
                    pk = ("pT", g % NPT)
                    act(pt_[:, c0:512], ps_[:, c0:512], AF.Exp, [("ps", sbk)], [pk])
                    if j >= 4 * qb:
                        memset(pt_[64:128, c0:c0 + 64], 0.0, [pk])

                def pv(i, qb=qb, base=base, units=units, nj=nj):
                    j, c = units[i]
                    g = base + i
                    c0 = 0 if j < 4 * qb else 128 * (j - 4 * qb)
                    pt_ = pT[g % NPT]
                    pk = ("pT", g % NPT)
                    mm(bank(4 + c)[:, c0:512], Vh[:, j, :], pt_[:, c0:512], j == 0, j == nj - 1, ["Vh", pk], [("po", c)])
                    mm(bank(6 + c)[:, c0:512], ones_b, pt_[:, c0:512], j == 0, j == nj - 1, [pk], [("pl", c)])

                qk(0)
                qk(1)
                for i in range(len(units)):
                    if i + 2 < len(units):
                        qk(i + 2)
                    pv(i)
                cnt_u[0] += len(units)
                recip(r0, bank(6), [("pl", 0)], ["r0"])
                recip(r1, bank(7), [("pl", 1)], ["r1"])
                tt(a0, bank(4), r0, ALU.mult, [("po", 0), "r0"], ["a0"])
                tt(a1, bank(5), r1, ALU.mult, [("po", 1), "r1"], ["a1"])
                stt(od, a1, neglam, a0, ALU.mult, ALU.add, ["a0", "a1"], ["od"])
                act(sq, od, AF.Square, ["od"], ["sq"])
                sbk = cnt_u[0] % 4
                cnt_u[0] += 1
                mm(bank(sbk), ones32, sq, True, True, ["sq"], [("ps", sbk)])
                act(rstd, bank(sbk), AF.Sqrt, [("ps", sbk)], ["rstd"], bias=epsc, scale=1.0 / 128.0)
                recip(rstd, rstd, ["rstd"], ["rstd"])
                ob = on[(h * NQB + qb) % 2]
                stt(ob, od, gsc, rstd, ALU.mult, ALU.mult, ["od", "rstd"], [("on", (h * NQB + qb) % 2)])
                dma(ST_ENG, OT[:, h, qb * 512:(qb + 1) * 512], ob, [("on", (h * NQB + qb) % 2)], [("OT", h, qb)], key=("onst", (h * NQB + qb) % 2))

        out_proj(Wb["a_w_out"], h_src, h_dst, 0, 0, dbg_dst)

    def out_proj(wdram, h_src, h_dst, li, lj, dbg_dst):
        phase("a3")
        wO = sb("wO", [128, 8, 1024], BF16)
        ots = [sb("ots%d" % i, [128, 8, 128], BF16) for i in range(2)]
        ep = Epi(6)
        dma("sp", wO, wdram.rearrange("(c p) n -> p c n", p=128), [], ["wO"])
        load_ln(li, lj)
        for t in range(NTL):
            b = t % 2
            rows = slice(t * 128, (t + 1) * 128)
            dma("sp", ots[b], OT[:, :, rows], [], [("ots", b)])
            pm = bank(2 * b, 2)
            for hf in range(2):
                for c in range(8):
                    mm(pm[:, hf * 512:(hf + 1) * 512], ots[b][:, c, :], wO[:, c, hf * 512:(hf + 1) * 512], c == 0, c == 7,
                       [("ots", b), "wO"], [("pm", b, hf)])
            ep.run(t, pm, [("pm", b, 0), ("pm", b, 1)], h_src, h_dst, dbg_dst)

    def cross_attention(li, h_src, h_dst, dbg_dst):
        phase("x%d" % li)
        wq = sb("wq", [128, 8, 1024], BF16)
        wo = sb("wo", [128, 8, 1024], BF16)
        hts = [sb("hts%d" % i, [128, 8, 128], BF16) for i in range(2)]
        qT = sb("qT", [128, 8, 128], BF16)
        nmx = sb("nmx", [128, 4], F32)
        lsum = sb("lsum", [128, 4], F32)
        P = sb("P", [128, 4, 256], BF16)
        Pn = sb("Pn", [128, 4, 256], BF16)
        PT = sb("PT", [128, 8, 128], BF16)
        oT = sb("oT", [128, 8, 128], BF16)
        ep = Epi(7)
        dma("sp", wq, Wb["xa_w_q"][li].rearrange("(c p) n -> p c n", p=128), [], ["wq"])
        dma("sp", wo, Wb["xa_w_out"][li].rearrange("(c p) n -> p c n", p=128), [], ["wo"])
        load_ln(li, 1)
        pA = bank(0, 2).rearrange("p (c t) -> p c t", c=8)
        psc = bank(2, 2).rearrange("p (h m) -> p h m", h=4)
        ptp = bank_bf(4).rearrange("p (c t) -> p c t", c=8)
        pmix = bank(5, 2)
        for t in range(NTL):
            b = t % 2
            rows = slice(t * 128, (t + 1) * 128)
            dma("sp", hts[b], HT[:, :, rows], [], [("hts", b)])
            for j in range(8):
                for k in range(8):
                    mm(pA[:, j, :], wq[:, k, j * 128:(j + 1) * 128], hts[b][:, k, :], k == 0, k == 7, ["wq", ("hts", b)], [("pA", j // 4)])
            act(qT, pA, AF.Copy, [("pA", 0), ("pA", 1)], ["qT"], scale=1.0 / 16.0)
            for hh in range(4):
                for dc in range(2):
                    mm(psc[:, hh, :], qT[:, 2 * hh + dc, :], memKT[:, 2 * hh + dc, :], dc == 0, dc == 1, ["qT"], [("psc", hh // 2)])
            red(nmx, psc, ALU.max, [("psc", 0), ("psc", 1)], ["nmx"], negate=True)
            for hh in range(4):
                act(P[:, hh, :], psc[:, hh, :], AF.Exp, [("psc", hh // 2), "nmx"], [("P", hh)], bias=nmx[:, hh:hh + 1], accum=lsum[:, hh:hh + 1])
            recip(lsum, lsum, [("P", hh) for hh in range(4)], ["lsum"])
            tt(Pn, P, lsum.unsqueeze(2).to_broadcast([128, 4, 256]), ALU.mult, [("P", hh) for hh in range(4)] + ["lsum"], ["Pn"])
            for hh in range(4):
                for mc in range(2):
                    tr(ptp[:, hh * 2 + mc, :], Pn[:, hh, mc * 128:(mc + 1) * 128], ident, ["Pn"], ["ptp"])
            cp(PT, ptp, ["ptp"], ["PT"])
            for hh in range(4):
                for dch in range(2):
                    for mc in range(2):
                        mm(pA[:, hh * 2 + dch, :], memV[:, mc, hh * 256 + dch * 128: hh * 256 + (dch + 1) * 128], PT[:, hh * 2 + mc, :],
                           mc == 0, mc == 1, ["PT"], [("pA", (hh * 2 + dch) // 4)])
            cp(oT, pA, [("pA", 0), ("pA", 1)], ["oT"], eng="act")
            for hf in range(2):
                for c in range(8):
                    mm(pmix[:, hf * 512:(hf + 1) * 512], oT[:, c, :], wo[:, c, hf * 512:(hf + 1) * 512], c == 0, c == 7, ["oT", "wo"], [("pmix", hf)])
            ep.run(t, pmix, [("pmix", 0), ("pmix", 1)], h_src, h_dst, dbg_dst)

    def moe(li, h_src, h_dst, dbg_dst):
        phase("m1_%d" % li)
        wr = sb("wr", [128, 8, 20], F32)
        whi = sb("whi", [128, 8, 20], BF16)
        wlo = sb("wlo", [128, 8, 20], BF16)
        rb = sb("rb", [128, 20], F32)
        LG = sb("LG", [128, NTL, 20], F32)
        hs = [sb("hs%d" % i, [128, 1024], F32) for i in range(2)]
        hhi = [sb("hhi%d" % i, [128, 1024], BF16) for i in range(2)]
        hlo = [sb("hlo%d" % i, [128, 1024], BF16) for i in range(2)]
        hiT = [sb("hiT%d" % i, [128, 8, 128], BF16) for i in range(2)]
        loT = [sb("loT%d" % i, [128, 8, 128], BF16) for i in range(2)]
        dma("sp", wr[:, :, 0:4], moe_w_group[li].rearrange("(c p) n -> p c n", p=128), [], [("wr", 0)], slow=True)
        dma("sp", wr[:, :, 4:20], moe_w_expert[li].rearrange("(c p) n -> p c n", p=128), [], [("wr", 1)], slow=True)
        dma("sp", rb[:, 0:4], moe_b_group[li].partition_broadcast(128), [], [("rb", 0)])
        dma("sp", rb[:, 4:20], moe_b_expert[li].partition_broadcast(128), [], [("rb", 1)])
        cp(whi, wr, [("wr", 0), ("wr", 1)], ["whi"])
        tt(wlo, wr, whi, ALU.subtract, [("wr", 0), ("wr", 1), "whi"], ["wlo"])
        for t in range(NTL):
            b = t % 2
            rows = slice(t * 128, (t + 1) * 128)
            dma("sp", hs[b], h_src[rows, :], [], [("hs", b)])
            cp(hhi[b], hs[b], [("hs", b)], [("hhi", b)], eng="act")
            tt(hlo[b], hs[b], hhi[b], ALU.subtract, [("hs", b), ("hhi", b)], [("hlo", b)])
            p1 = bank_bf(2 * b).rearrange("p (c t) -> p c t", c=8)
            p2 = bank_bf(2 * b + 1).rearrange("p (c t) -> p c t", c=8)
            for c in range(8):
                tr(p1[:, c, :], hhi[b][:, c * 128:(c + 1) * 128], ident, [("hhi", b)], [("p1", b)])
            for c in range(8):
                tr(p2[:, c, :], hlo[b][:, c * 128:(c + 1) * 128], ident, [("hlo", b)], [("p2", b)])
            cp(hiT[b], p1, [("p1", b)], [("hiT", b)], eng="act")
            cp(loT[b], p2, [("p2", b)], [("loT", b)])
            pl = bank(4 + b)[:, 0:20]
            n = 0
            for (aT, w_, ka, kw_) in ((hiT[b], whi, ("hiT", b), "whi"), (hiT[b], wlo, ("hiT", b), "wlo"), (loT[b], whi, ("loT", b), "whi")):
                for k in range(8):
                    mm(pl, aT[:, k, :], w_[:, k, :], n == 0, n == 23, [ka, kw_], [("pl", b)])
                    n += 1
            tt(LG[:, t, :], pl, rb, ALU.add, [("pl", b), ("rb", 0), ("rb", 1)], [("LG", t)])
        T = NTL
        G = LG[:, :, 0:4]
        E = LG[:, :, 4:20].rearrange("p t (g j) -> p t g j", g=4)
        LGR = [("LG", t) for t in range(NTL)]
        gmax = sb("gmax", [128, T], F32); goh = sb("goh", [128, T, 4], F32); ge = sb("ge", [128, T, 4], F32)
        gsum = sb("gsum", [128, T], F32); tmp4 = sb("tmp4", [128, T, 4, 4], F32); esel = sb("esel", [128, T, 4], F32)
        m1 = sb("m1", [128, T], F32); oh1 = sb("oh1", [128, T, 4], F32); e2 = sb("e2", [128, T, 4], F32)
        m2 = sb("m2", [128, T], F32); oh2 = sb("oh2", [128, T, 4], F32); dd = sb("dd", [128, T], F32)
        w1 = sb("w1", [128, T], F32); w2 = sb("w2", [128, T], F32); cin = sb("cin", [128, T, 4], F32); cin2 = sb("cin2", [128, T, 4], F32)

        def bc3(a):
            return a.unsqueeze(2).to_broadcast([128, T, 4])

        red(gmax, G, ALU.max, LGR, ["gmax"])
        tt(goh, G, bc3(gmax), ALU.is_equal, LGR + ["gmax"], ["goh"])
        tt(ge, G, bc3(gmax), ALU.subtract, LGR + ["gmax"], ["ge"])
        act(ge, ge, AF.Exp, ["ge"], ["ge"])
        red(gsum, ge, ALU.add, ["ge"], ["gsum"])
        recip(gsum, gsum, ["gsum"], ["gsum"])
        tt(tmp4, E, goh.unsqueeze(3).to_broadcast([128, T, 4, 4]), ALU.mult, LGR + ["goh"], ["tmp4"])
        red(esel, tmp4.rearrange("p t g j -> p t j g"), ALU.add, ["tmp4"], ["esel"])
        red(m1, esel, ALU.max, ["esel"], ["m1"])
        tt(oh1, esel, bc3(m1), ALU.is_equal, ["esel", "m1"], ["oh1"])
        stt(e2, oh1, -1e30, esel, ALU.mult, ALU.add, ["oh1", "esel"], ["e2"])
        red(m2, e2, ALU.max, ["e2"], ["m2"])
        tt(oh2, e2, bc3(m2), ALU.is_equal, ["e2", "m2"], ["oh2"])
        tt(dd, m2, m1, ALU.subtract, ["m1", "m2"], ["dd"])
        act(dd, dd, AF.Exp, ["dd"], ["dd"])
        ts(w1, dd, 1.0, None, ALU.add, None, ["dd"], ["w1"])
        recip(w1, w1, ["w1"], ["w1"])
        tt(w2, dd, w1, ALU.mult, ["dd", "w1"], ["w2"])
        tt(w1, w1, gsum, ALU.mult, ["w1", "gsum"], ["w1"])
        tt(w2, w2, gsum, ALU.mult, ["w2", "gsum"], ["w2"])
        tt(cin, oh1, bc3(w1), ALU.mult, ["oh1", "w1"], ["cin"])
        tt(cin2, oh2, bc3(w2), ALU.mult, ["oh2", "w2"], ["cin2"])
        tt(cin, cin, cin2, ALU.add, ["cin", "cin2"], ["cin"])
        tt(comb.rearrange("p t (g j) -> p t g j", g=4), goh.unsqueeze(3).to_broadcast([128, T, 4, 4]),
           cin.unsqueeze(2).to_broadcast([128, T, 4, 4]), ALU.mult, ["goh", "cin"], ["comb"])

        phase("m2_%d" % li)
        TB = 1024
        hTb = sb("hTb", [128, 8, TB], BF16)
        yacc = sb("yacc", [128, 8, 1024], F32)
        wg = [sb("wg%d" % i, [128, 8, 512], BF16) for i in range(2)]
        wu = [sb("wu%d" % i, [128, 8, 512], BF16) for i in range(2)]
        wd = [sb("wd%d" % i, [128, 4, 1024], BF16) for i in range(2)]
        sg = [sb("sg%d" % i, [128, 512], F32) for i in range(2)]
        hid = [sb("hid%d" % i, [128, 4, 512], BF16) for i in range(2)]
        ep = Epi(6)
        load_ln(li, 2)
        nh = 0
        ny = 0
        for blk in range(NT // TB):
            dma("sp", hTb, HT[:, :, blk * TB:(blk + 1) * TB], [], ["hTb"])
            memset(yacc, 0.0, ["yacc"] + [("yacc", tl, dh) for tl in range(8) for dh in range(2)])
            for e in range(16):
                wbuf = e % 2
                dma("sp", wg[wbuf], Wb["moe_w_gate"][li, e].rearrange("(c p) n -> p c n", p=128), [], [("wg", wbuf)])
                dma("sp", wu[wbuf], Wb["moe_w_up"][li, e].rearrange("(c p) n -> p c n", p=128), [], [("wu", wbuf)])
                dma("sp", wd[wbuf], Wb["moe_w_down"][li, e].rearrange("(c p) n -> p c n", p=128), [], [("wd", wbuf)])
                for half in range(TB // 512):
                    hb_ = hid[nh % 2]
                    hk = ("hid", nh % 2)
                    for fc in range(4):
                        pg = bank(0 + (fc % 2))
                        pu = bank(2 + (fc % 2))
                        for k in range(8):
                            mm(pg, wg[wbuf][:, k, fc * 128:(fc + 1) * 128], hTb[:, k, half * 512:(half + 1) * 512], k == 0, k == 7,
                               [("wg", wbuf), "hTb"], [("pg", fc % 2)])
                        for k in range(8):
                            mm(pu, wu[wbuf][:, k, fc * 128:(fc + 1) * 128], hTb[:, k, half * 512:(half + 1) * 512], k == 0, k == 7,
                               [("wu", wbuf), "hTb"], [("pu", fc % 2)])
                        act(sg[fc % 2], pg, AF.Silu, [("pg", fc % 2)], [("sg", fc % 2)])
                        tt(hb_[:, fc, :], sg[fc % 2], pu, ALU.mult, [("sg", fc % 2), ("pu", fc % 2)], [hk + (fc,)])
                    hR = [hk + (fc,) for fc in range(4)]
                    for tt_ in range(4):
                        tl = half * 4 + tt_
                        tg = blk * 8 + tl
                        for dh in range(2):
                            py = bank(4 + ny % 2)
                            for fc in range(4):
                                mm(py, hb_[:, fc, tt_ * 128:(tt_ + 1) * 128], wd[wbuf][:, fc, dh * 512:(dh + 1) * 512], fc == 0, fc == 3,
                                   hR + [("wd", wbuf)], [("py", ny % 2)])
                            ya = yacc[:, tl, dh * 512:(dh + 1) * 512]
                            stt(ya, py, comb[:, tg, e:e + 1], ya, ALU.mult, ALU.add, [("py", ny % 2), "yacc", ("yacc", tl, dh)], [("yacc", tl, dh)])
                            ny += 1
                    nh += 1
            for tl in range(8):
                ep.run(blk * 8 + tl, yacc[:, tl, :], [("yacc", tl, 0), ("yacc", tl, 1)], h_src, h_dst, dbg_dst)

    def dsa(h_src, h_dst, dbg_dst):
        phase("d1")
        wI = sb("wI", [128, 8, 624], BF16)
        wUQ = sb("wUQ", [128, 2, 1024], BF16)
        wQI = sb("wQI", [128, 2, 1024], BF16)
        wUKT = sb("wUKT", [128, 8, 256], BF16)
        gq = sb("gq", [128, 256], F32)
        gkv = sb("gkv", [128, 256], F32)
        hts = [sb("hts%d" % i, [128, 8, 128], BF16) for i in range(2)]
        rts = [sb("rts%d" % i, [128, 128], F32) for i in range(2)]
        sqj = sb("sqj", [128, 256], F32)
        ssq = sb("ssq", [128, 2], F32)
        cqn = sb("cqn", [128, 256], BF16)
        ckn = sb("ckn", [128, 256], BF16)
        cqT = sb("cqT", [128, 2, 128], BF16)
        kvTs = sb("kvTs", [128, 3, 128], BF16)
        krp = sb("krp", [128, 1, 128], BF16)
        kix = sb("kix", [128, 2, 64], BF16)
        kiT = sb("kiT", [128, 128], BF16)
        wix = sb("wix", [128, 16], F32)
        tA = sb("tA", [128, 16, 32], F32)
        tB = sb("tB", [128, 16, 32], F32)
        qtk = sb("qtk", [128, 8, 128], BF16)
        qiT = sb("qiT", [128, 8, 8, 16], BF16)
        qT = sb("qT", [128, 8, 128], BF16)
        qfT = sb("qfT", [128, 8, 3, 128], BF16)
        qiks = [sb("qik%d" % i, [128, 16, 64], BF16) for i in range(2)]
        dma("sp", wI, Wb["b_w_in"].rearrange("(c p) n -> p c n", p=128), [], ["wI"])
        dma("sp", wUQ, Wb["b_w_uq"].rearrange("(c p) n -> p c n", p=128), [], ["wUQ"])
        dma("sp", wQI, Wb["b_w_qidx"].rearrange("(c p) n -> p c n", p=128), [], ["wQI"])
        wukn = sb("wukn", [128, 8, 2, 128], BF16)
        memset(wukn, 0.0, ["wukn"])
        for rc in range(2):
            dma("sp", wukn[:, :, rc, 32:128], Wb["b_w_uk"][:, rc * 128:(rc + 1) * 128, :].rearrange("h p n -> p h n"), [], ["wukn", ("wukn", rc)], key=("wukn", rc))
        for hh in range(8):
            ptw = bank_bf(hh % 2).rearrange("p (c t) -> p c t", c=8)
            for rc in range(2):
                tr(ptw[:, rc, :], wukn[:, hh, rc, :], ident, ["wukn", ("wukn", 0), ("wukn", 1)], ["p%d" % (hh % 2)])
            cp(wUKT[:, hh, :].rearrange("p (c t) -> p c t", c=2), ptw[:, 0:2, :], ["p%d" % (hh % 2)], [("wUKT", hh)])
        wUKR = [("wUKT", hh) for hh in range(8)]
        dma("sp", gq, b_q_norm_g.partition_broadcast(128), [], ["gq"])
        dma("sp", gkv, b_kv_norm_g.partition_broadcast(128), [], ["gkv"])
        memset(kvTs, 0.0, ["kvTs"])
        memset(krp, 0.0, ["krp"])
        memset(qfT, 0.0, ["qfT"])
        sub(11)
        for t in range(NTL):
            b = t % 2
            rows = slice(t * 128, (t + 1) * 128)
            dma("sp", hts[b], HT[:, :, rows], [], [("hts", b)])
            dma("sp", rts[b], ROPE[rows, :], [], [("rts", b)])
            p0 = bank(0)
            p1 = bank(1)[:, 0:112]
            for k in range(8):
                mm(p0, hts[b][:, k, :], wI[:, k, 0:512], k == 0, k == 7, [("hts", b), "wI"], ["p0"])
            for k in range(8):
                mm(p1, hts[b][:, k, :], wI[:, k, 512:624], k == 0, k == 7, [("hts", b), "wI"], ["p1"])
            for i, (gg, dst, nm_) in enumerate(((gq, cqn, "cqn"), (gkv, ckn, "ckn"))):
                act(sqj, p0[:, i * 256:(i + 1) * 256], AF.Square, ["p0"], ["sqj"], accum=ssq[:, i:i + 1])
                act(ssq[:, i:i + 1], ssq[:, i:i + 1], AF.Sqrt, ["sqj"], [("ssq", i)], bias=epsc, scale=1.0 / 256.0)
                recip(ssq[:, i:i + 1], ssq[:, i:i + 1], [("ssq", i)], [("ssq", i)])
                stt(dst, p0[:, i * 256:(i + 1) * 256], ssq[:, i:i + 1], gg, ALU.mult, ALU.mult, ["p0", ("ssq", i), "gq", "gkv"], [nm_])
            dma(ST_ENG, CKV[rows, :], ckn, ["ckn"], [("CKV", t)], key="ckvst")
            sub(12)
            rope(krp[:, :, 0:32], p1[:, 0:32].rearrange("p (h d) -> p h d", h=1), 1, 32, rts[b], 32, 16, tA, tB, ["p1", ("rts", b)], ["krp"], "rk")
            rope(kix[:, 0:1, :], p1[:, 32:96].rearrange("p (h d) -> p h d", h=1), 1, 64, rts[b], 96, 8, tA, tB, ["p1", ("rts", b)], [("kix", 0)], "ri")
            cp(kix[:, 1:2, :], kix[:, 0:1, :], [("kix", 0)], [("kix", 1)])
            ts(wix, p1[:, 96:112], 1.0 / 32.0, None, ALU.mult, None, ["p1"], ["wix"])
            dma(ST_ENG, WIX[rows, :], wix, ["wix"], [("WIX", t)], key="wixst")
            sub(13)
            pt = bank_bf(2).rearrange("p (c t) -> p c t", c=8)
            for c in range(2):
                tr(pt[:, c, :], cqn[:, c * 128:(c + 1) * 128], ident, ["cqn"], ["pt2"])
            sub(1310)
            for c in range(2):
                tr(pt[:, 2 + c, :], ckn[:, c * 128:(c + 1) * 128], ident, ["ckn", ("CKV", t)], ["pt2"])
            sub(1311)
            tr(pt[:, 4, :], krp.rearrange("p h d -> p (h d)"), ident, ["krp"], ["pt2"])
            tr(pt[:, 5, :], kix.rearrange("p a d -> p (a d)"), ident, [("kix", 0), ("kix", 1)], ["pt2"])
            sub(131)
            cp(cqT, pt[:, 0:2, :], ["pt2"], ["cqT"])
            cp(kvTs[:, 0:2, :], pt[:, 2:4, :], ["pt2"], [("kvTs", 0)], eng="act")
            cp(kvTs[:, 2, :], pt[:, 4, :], ["pt2"], [("kvTs", 1)])
            cp(kiT, pt[:, 5, :], ["pt2"], ["kiT"], eng="act")
            sub(132)
            dma(ST_ENG, KVT[:, :, rows], kvTs, ["kvTs", ("kvTs", 0), ("kvTs", 1)], [("KVT", t)], key="kvtst")
            sub(133)
            dma(ST_ENG, KIT[:, rows], kiT, ["kiT"], [("KIT", t)], key="kitst")
            sub(14)
            pq = bank(3, 2)
            for hf in range(2):
                for c in range(2):
                    mm(pq[:, hf * 512:(hf + 1) * 512], cqT[:, c, :], wUQ[:, c, hf * 512:(hf + 1) * 512], c == 0, c == 1, ["cqT", "wUQ"], [("pq", hf)])
            rope(qtk, pq.rearrange("p (h d) -> p h d", h=8), 8, 128, rts[b], 32, 16, tA, tB, [("pq", 0), ("pq", 1), ("rts", b)], ["qtk"], "rq")
            sub(15)
            pqi = bank(5, 2)
            for hf in range(2):
                for c in range(2):
                    mm(pqi[:, hf * 512:(hf + 1) * 512], cqT[:, c, :], wQI[:, c, hf * 512:(hf + 1) * 512], c == 0, c == 1, ["cqT", "wQI"], [("pqi", hf)])
            qik = qiks[b]
            rope(qik, pqi.rearrange("p (h d) -> p h d", h=16), 16, 64, rts[b], 96, 8, tA, tB, [("pqi", 0), ("pqi", 1), ("rts", b)], [("qik", b)], "rqi")
            sub(16)
            pt7 = bank_bf(7).rearrange("p (c t) -> p c t", c=8)
            qf = qtk.rearrange("p h d -> p (h d)")
            for hh in range(8):
                tr(pt7[:, hh, :], qf[:, hh * 128:(hh + 1) * 128], ident, ["qtk"], ["pt7"])
            cp(qT, pt7, ["pt7"], ["qT"])
            qif = qik.rearrange("p h d -> p (h d)")
            for c in range(8):
                tr(pt7[:, c, :], qif[:, c * 128:(c + 1) * 128], ident, [("qik", b)], ["pt7"])
            cp(qiT.rearrange("p g j q -> p j g q"), pt7.rearrange("p j (g q) -> p j g q", g=8), ["pt7"], ["qiT"])
            dma(ST_ENG, QIT[:, t, :], qiT.rearrange("p g j q -> p (g j q)"), ["qiT"], [("QIT", t)], key="qitst")
            sub(17)
            sc = 1.0 / math.sqrt(128.0)
            pl_ = bank(0, 2).rearrange("p (c t) -> p c t", c=8)
            for half in range(2):
                for hh4 in range(4):
                    hh = half * 4 + hh4
                    for rc in range(2):
                        mm(pl_[:, hh4 * 2 + rc, :], wUKT[:, hh, rc * 128:(rc + 1) * 128], qT[:, hh, :], True, True,
                           wUKR + ["qT"], ["p0" if (hh4 * 2 + rc) < 4 else "p1"])
                for hh4 in range(4):
                    hh = half * 4 + hh4
                    act(qfT[:, hh, 0:2, :], pl_[:, hh4 * 2:hh4 * 2 + 2, :], AF.Copy, ["p0", "p1"], [("qfT", hh)], scale=sc)
            act(qfT[0:32, :, 2, :], qT[0:32, :, :], AF.Copy, ["qT"], [("qfTr")], scale=sc)
            dma(ST_ENG, QFT[:, :, :, rows], qfT, ["qfT", "qfTr"] + [("qfT", hh) for hh in range(8)], [("QFT", t)], key="qftst")

        phase("d2")
        KIs = sb("KIs", [128, NT], BF16)
        qis = [[sb("qis%d_%d" % (i, par), [128, 1024], BF16) for par in range(2)] for i in range(2)]
        for i in range(2):
            memset(qis[i][0][64:128, :], 0.0, [("qisz", i, 0)])
            memset(qis[i][1][0:64, :], 0.0, [("qisz", i, 1)])
        wxs = [sb("wxs%d" % i, [128, 16], F32) for i in range(2)]
        wsel = sb("wsel", [128, 8, 16], F32)
        wc = sb("wc", [128, 8, 2], F32)
        Wblk = [sb("Wblk%d" % i, [128, 16, 128], BF16) for i in range(2)]
        NRB = 4
        Rb = [sb("Rb%d" % i, [128, 512], BF16) for i in range(NRB)]
        sc_ = [sb("sc%d" % i, [128, NT], F32) for i in range(2)]
        junk = sb("junk", [128, NT], BF16)
        nmk = [sb("nmk%d" % i, [128, NT], BF16) for i in range(2)]
        lo = [sb("lo%d" % i, [128, 1], F32) for i in range(2)]
        hi = [sb("hi%d" % i, [128, 1], F32) for i in range(2)]
        stp = [sb("stp%d" % i, [128, 20], F32) for i in range(2)]
        mid = [sb("mid%d" % i, [128, 1], F32) for i in range(2)]
        cnt = [sb("cnt%d" % i, [128, 1], F32) for i in range(2)]
        geb = [sb("geb%d" % i, [128, 1], F32) for i in range(2)]
        NIT = 18
        hmask = cf[:, CF_HM:CF_HM + 16]
        dma("sp", KIs, KIT, [], ["KIs"])
        nlg = [0]

        def prep(t):
            b = t % 2
            rows = slice(t * 128, (t + 1) * 128)
            dma("sp", qis[b][0][0:64, :], QIT[0:64, t, :], [], [("qis", b, 0)])
            dma("sp", qis[b][1][64:128, :], QIT[64:128, t, :], [], [("qis", b, 1)])
            dma("sp", wxs[b], WIX[rows, :], [], [("wxs", b)])
            pw = bank(6)[:, 0:128].rearrange("p (g h) -> p g h", g=8)
            for g in range(8):
                mm(pw[:, g, :], cf[:, CF_SEL + g * 128:CF_SEL + (g + 1) * 128], wxs[b], True, True, [("wxs", b)], ["pw"])
            tt(wsel, pw, hmask.unsqueeze(1).to_broadcast([128, 8, 16]), ALU.mult, ["pw"], ["wsel"])
            red(wc, wsel.rearrange("p g (j r) -> p g r j", r=2), ALU.add, ["wsel"], ["wc"])
            for g in range(8):
                for par in range(2):
                    ts(Wblk[b][:, g * 2 + par, :], cb[:, CB_E + g * 128:CB_E + (g + 1) * 128], wc[:, g, par:par + 1], None, ALU.mult, None,
                       ["wc"], [("Wblk", b, g * 2 + par)], eng="pool")

        def main(t):
            b = t % 2
            nk = 128 * (t + 1)
            WR = [("Wblk", b, i) for i in range(16)]
            nkb = (nk + 511) // 512
            steps = [(kb, i) for kb in range(nkb) for i in range(16)]
            base = nlg[0]

            def geom(kb):
                kw = min(512, nk - kb * 512)
                return kw, slice(kb * 512, kb * 512 + kw)

            def lg(s):
                kb, i = steps[s]
                kw, ks = geom(kb)
                n = base + s
                g, par = i // 2, i % 2
                plg = bank(n % 4)
                mm(plg[:, 0:kw], qis[b][par][:, g * 128:(g + 1) * 128], KIs[:, ks], True, True, [("qis", b, par), ("qisz", b, par), "KIs"], [("plg", n % 4)])
                act(Rb[n % NRB][:, 0:kw], plg[:, 0:kw], AF.Relu, [("plg", n % 4)], [("Rb", n % NRB)])

            def scm(s):
                kb, i = steps[s]
                kw, ks = geom(kb)
                n = base + s
                pscore = bank(4 + kb % 2)
                mm(pscore[:, 0:kw], Wblk[b][:, i, :], Rb[n % NRB][:, 0:kw], i == 0, i == 15, WR + [("Rb", n % NRB)], [("pscore", kb % 2)])
                if i == 15:
                    cp(sc_[b][:, ks], pscore[:, 0:kw], [("pscore", kb % 2)], [("sc", b, kb)], eng="act")

            lg(0)
            lg(1)
            for s in range(len(steps)):
                if s + 2 < len(steps):
                    lg(s + 2)
                scm(s)
            nlg[0] += len(steps)

        def bisect(t):
            b = t % 2
            rows = slice(t * 128, (t + 1) * 128)
            nk = 128 * (t + 1)
            nkb = (nk + 511) // 512
            scR = [("sc", b, kb) for kb in range(nkb)]
            scK = ("scall", b)
            memset(sc_[b][0:64, t * 128 + 64:(t + 1) * 128], -1e30, [scK], R=scR)
            sv = sc_[b][:, 0:nk]
            if t >= 2:
                red(lo[b], sc_[b][:, 0:nk - 64], ALU.min, scR + [scK], [("lo", b)])
                red(hi[b], sv, ALU.max, scR + [scK], [("hi", b)])
                tt(mid[b], hi[b], lo[b], ALU.subtract, [("lo", b), ("hi", b)], [("mid", b)])
                for i in range(NIT):
                    ts(stp[b][:, i:i + 1], mid[b], 2.0 ** -(i + 1), None, ALU.mult, None, [("mid", b)], [("stp", b, i)], eng="pool")
                for i in range(NIT):
                    tt(mid[b], lo[b], stp[b][:, i:i + 1], ALU.add, [("lo", b), ("stp", b, i)], [("mid", b)])
                    ts(junk[:, 0:nk], sv, mid[b], 0.0, ALU.is_ge, ALU.add, scR + [scK, ("mid", b)], ["junk", ("cnt", b)], accum=cnt[b])
                    ts(geb[b], cnt[b], 255.5, stp[b][:, i:i + 1], ALU.is_ge, ALU.mult, [("cnt", b), ("stp", b, i)], [("geb", b)])
                    tt(lo[b], lo[b], geb[b], ALU.add, [("lo", b), ("geb", b)], [("lo", b)])
            else:
                memset(lo[b], -1e29, [("lo", b)], eng="dve")
            ts(nmk[b][:, 0:nk], sv, lo[b], NEG, ALU.is_lt, ALU.mult, scR + [scK, ("lo", b)], [("nmk", b)])
            dma(ST_ENG, NM[rows, 0:nk], nmk[b][:, 0:nk], [("nmk", b)], [("NM", t)], key=("nmst", b))

        prep(0)
        for t in range(NTL):
            main(t)
            if t + 1 < NTL:
                prep(t + 1)
            bisect(t)

        phase("d3")
        KVs = sb("KVs", [128, 3, NT], BF16)
        CKs = sb("CKs", [128, NTL, 256], BF16)
        wUV = sb("wUV", [128, 8, 2, 128], BF16)
        qfs = [sb("qfs%d" % i, [128, 8, 3, 512], BF16) for i in range(2)]
        NNM = 6
        nms = [sb("nms%d" % i, [128, 4, 128], BF16) for i in range(NNM)]
        NPT = 4
        pT = [sb("pT%d" % i, [128, 512], BF16) for i in range(NPT)]
        rl = sb("rl", [128, 512], F32)
        olat = sb("olat", [128, 2, 512], BF16)
        on = [sb("on%d" % i, [128, 512], BF16) for i in range(2)]
        dma("sp", KVs, KVT, [], ["KVs"])
        dma("sp", CKs, CKV.rearrange("(t p) r -> p t r", p=128), [], ["CKs"])
        for hh in range(8):
            dma("sp", wUV[:, hh, :, :], Wb["b_w_uv"][hh].rearrange("(c p) v -> p c v", p=128), [], [("wUV", hh)])
        gcnt = [0]
        SB3 = (0, 1, 7)
        for qb in range(NQB):
            qbuf = qb % 2
            dma("sp", qfs[qbuf], QFT[:, :, :, qb * 512:(qb + 1) * 512], [], [("qfs", qbuf)])
            nj = 4 * qb + 4
            for hh in range(8):
                base = gcnt[0]

                def qk(j, qb=qb, hh=hh, base=base, qbuf=qbuf):
                    g = base + j
                    c0 = 0 if j < 4 * qb else 128 * (j - 4 * qb)
                    s0 = c0 // 128
                    nb_ = nms[g % NNM]
                    nk_ = ("nms", g % NNM)
                    dma("sp", nb_[:, s0:4, :], NM[qb * 512 + c0:(qb + 1) * 512, j * 128:(j + 1) * 128].rearrange("(s p) k -> p s k", p=128),
                        [], [nk_])
                    bk = SB3[g % 3]
                    ps_ = bank(bk)
                    pk_ = ("ps", bk)
                    mm(ps_[:, c0:512], KVs[:, 0, j * 128:(j + 1) * 128], qfs[qbuf][:, hh, 0, c0:512], True, False, ["KVs", ("qfs", qbuf)], [pk_])
                    mm(ps_[:, c0:512], KVs[:, 1, j * 128:(j + 1) * 128], qfs[qbuf][:, hh, 1, c0:512], False, False, ["KVs", ("qfs", qbuf)], [pk_])
                    mm(ps_[:, c0:512], KVs[:, 2, j * 128:(j + 1) * 128], qfs[qbuf][:, hh, 2, c0:512], False, False, ["KVs", ("qfs", qbuf)], [pk_])
                    for s in range(s0, 4):
                        mm(ps_[:, s * 128:(s + 1) * 128], nb_[:, s, :], ident, False, s == 3, [nk_], [pk_])
                    act(pT[g % NPT][:, c0:512], ps_[:, c0:512], AF.Exp, [pk_], [("pT", g % NPT)])

                def pv(j, qb=qb, hh=hh, base=base, nj=nj):
                    g = base + j
                    c0 = 0 if j < 4 * qb else 128 * (j - 4 * qb)
                    pt_ = pT[g % NPT]
                    pk = ("pT", g % NPT)
                    for rc in range(2):
                        mm(bank(2 + rc)[:, c0:512], CKs[:, j, rc * 128:(rc + 1) * 128], pt_[:, c0:512], j == 0, j == nj - 1, ["CKs", pk], [("po", rc)])
                    mm(bank(4)[:, c0:512], ones_b, pt_[:, c0:512], j == 0, j == nj - 1, [pk], ["pl"])

                qk(0)
                if nj > 1:
                    qk(1)
                for j in range(nj):
                    if j + 2 < nj:
                        qk(j + 2)
                    pv(j)
                gcnt[0] += nj
                recip(rl, bank(4), ["pl"], ["rl"])
                for rc in range(2):
                    tt(olat[:, rc, :], bank(2 + rc), rl, ALU.mult, [("po", rc), "rl"], [("olat", rc)])
                pvb = bank(5 + hh % 2)
                for rc in range(2):
                    mm(pvb, wUV[:, hh, rc, :], olat[:, rc, :], rc == 0, rc == 1, [("wUV", hh), ("olat", 0), ("olat", 1)], [("pv", hh % 2)])
                ob = on[hh % 2]
                cp(ob, pvb, [("pv", hh % 2)], [("on", hh % 2)], eng="act")
                dma(ST_ENG, OT[:, hh, qb * 512:(qb + 1) * 512], ob, [("on", hh % 2)], [("OT", hh, qb)], key=("onst", hh % 2))

        out_proj(Wb["b_w_out"], h_src, h_dst, 1, 0, dbg_dst)

    try:
        p0()
        t0()
        diff_attention(x, H[0], dbg.get("h0_0"))
        cross_attention(0, H[0], H[1], dbg.get("h0_1"))
        moe(0, H[1], H[0], dbg.get("h0_2"))
        dsa(H[0], H[1], dbg.get("h1_0"))
        cross_attention(1, H[1], H[0], dbg.get("h1_1"))
        moe(1, H[0], out, None)
    except StopBuild:
        pass
    S.barrier()
    S.emit()
    st.close()
    return nc, S


_CACHE = {}


def make_in_maps(inputs, NT, ncores):
    cf, cb = _consts()
    maps = []
    f = lambda a: np.ascontiguousarray(a, dtype=np.float32)
    for b in range(ncores):
        m = {
            "x": f(inputs["x"][b, :NT]), "mem": f(inputs["mem"][b]),
            "positions": np.ascontiguousarray(inputs["positions"][b, :NT].reshape(NT // 128, 128).astype(np.int32)),
            "a_w_in": f(inputs["a_w_in"][0]), "a_lambda": f(inputs["a_lambda"][0]),
            "a_subln_g": f(inputs["a_subln_g"][0].reshape(128, 1)), "a_w_out": f(inputs["a_w_out"][0]),
            "b_w_in": f(inputs["b_w_in"][0]), "b_q_norm_g": f(inputs["b_q_norm_g"][0]), "b_kv_norm_g": f(inputs["b_kv_norm_g"][0]),
            "b_w_uq": f(inputs["b_w_uq"][0]), "b_w_qidx": f(inputs["b_w_qidx"][0]), "b_w_uk": f(inputs["b_w_uk"][0]),
            "b_w_uv": f(inputs["b_w_uv"][0]), "b_w_out": f(inputs["b_w_out"][0]),
            "mem_w_kv": f(inputs["mem_w_kv"]), "xa_w_q": f(inputs["xa_w_q"]), "xa_w_out": f(inputs["xa_w_out"]),
            "moe_w_group": f(inputs["moe_w_group"]), "moe_b_group": f(inputs["moe_b_group"]),
            "moe_w_expert": f(inputs["moe_w_expert"]), "moe_b_expert": f(inputs["moe_b_expert"]),
            "moe_w_gate": f(inputs["moe_w_gate"]), "moe_w_up": f(inputs["moe_w_up"]), "moe_w_down": f(inputs["moe_w_down"]),
            "ln_g": f(inputs["ln_g"]), "ln_b": f(inputs["ln_b"]),
            "cstf": cf, "cstb": cb,
        }
        maps.append(m)
    return maps


def kernel(**inputs):
    NT = inputs["x"].shape[1]
    nb = inputs["x"].shape[0]
    if NT not in _CACHE:
        _CACHE[NT] = build(NT)[0]
    nc = _CACHE[NT]
    maps = make_in_maps(inputs, NT, nb)
    res = run_bass_kernel_spmd(nc, maps, core_ids=list(range(nb)))
    return np.stack([np.asarray(r["out"], dtype=np.float32) for r in res.results], axis=0)
```

```python
import math
import contextlib
import numpy as np
import ml_dtypes
import concourse.bass as bass
import concourse.mybir as mybir
from concourse.bass_utils import run_bass_kernel_spmd

F32 = mybir.dt.float32
BF16 = mybir.dt.bfloat16
I32 = mybir.dt.int32
U8 = mybir.dt.uint8
AF = mybir.ActivationFunctionType
ALU = mybir.AluOpType
AX = mybir.AxisListType

D = 1024
DEPTH = 2
ALPHA = (2.0 * DEPTH) ** 0.25
LN_EPS = 1e-5
ROPE_THETA = 500000.0
NEG = -30000.0
ENGS = ("pe", "act", "dve", "pool", "sp")
NPOOL = 88
ST_ENG = "sp"


PSUM_NAMES = {"ps0", "psb", "pk", "pv", "pt", "pa", "ps", "po", "pl", "pm", "pA", "psc", "ptp", "pmix", "p1", "p2", "pg", "pu",
              "py", "e_pt", "p0", "pt2", "pq", "pqi", "pt7", "pw", "pscore", "plg"}


def is_psum(key):
    base = key if isinstance(key, str) else key[0]
    return base in PSUM_NAMES


class Op:
    __slots__ = ("eng", "fn", "is_dma", "deps", "sem", "val", "signal", "waits", "semidx")

    def __init__(self, eng, fn, is_dma, semidx):
        self.eng = eng
        self.fn = fn
        self.is_dma = is_dma
        self.deps = []
        self.sem = None
        self.val = 0
        self.signal = False
        self.waits = []
        self.semidx = semidx


class Sched:
    def __init__(self, nc):
        self.nc = nc
        self.ops = []
        self.last_write = {}
        self.reads_since = {}
        self.keymap = {}
        self.dmas_since = []
        self.last_on = {}

    def add(self, eng, fn, reads=(), writes=(), dma=False, semkey=None):
        semidx = None
        if dma:
            if semkey is None:
                semkey = writes[0]
            if semkey not in self.keymap:
                assert len(self.keymap) < NPOOL, "too many dma sem keys in phase"
                self.keymap[semkey] = len(self.keymap)
            semidx = self.keymap[semkey]
        op = Op(eng, fn, dma, semidx)
        reads_eff = [r for r in reads if not is_psum(r)]
        writes_eff = list(writes) + [r for r in reads if is_psum(r)]
        deps = {}
        for r in reads_eff:
            for w in self.last_write.get(r, {}).values():
                deps[id(w)] = (w, "raw")
        for r in writes_eff:
            for w in self.last_write.get(r, {}).values():
                if id(w) not in deps:
                    deps[id(w)] = (w, "waw")
            for rd in self.reads_since.get(r, ()):
                if id(rd) not in deps:
                    deps[id(rd)] = (rd, "war")
        for d, kind in deps.values():
            if (not d.is_dma) and (not dma) and d.eng == eng:
                if eng == "pe":
                    continue
                if kind != "raw" and eng != "pool":
                    continue
            op.deps.append(d)
        for r in reads_eff:
            self.reads_since.setdefault(r, []).append(op)
        for r in writes_eff:
            self.last_write.setdefault(r, {})["dma" if dma else eng] = op
            self.reads_since[r] = []
        self.ops.append(op)
        if dma:
            self.dmas_since.append(op)
        else:
            self.last_on[eng] = op
        return op

    def barrier(self):
        lasts = [o for o in self.last_on.values() if o.fn is not None] + list(self.dmas_since)
        for e in ENGS:
            op = Op(e, None, False, None)
            op.deps = list(lasts)
            self.ops.append(op)
        self.last_write = {}
        self.reads_since = {}
        self.keymap = {}
        self.dmas_since = []
        self.last_on = {}

    def emit(self):
        nc = self.nc
        for op in self.ops:
            for d in op.deps:
                d.signal = True
        stack = contextlib.ExitStack()
        eng_sem = {e: stack.enter_context(nc.semaphore("s_" + e)) for e in ENGS}
        npool = max([o.semidx for o in self.ops if o.is_dma] + [0]) + 1
        pool = [stack.enter_context(nc.semaphore("d_%d" % i)) for i in range(npool)]
        counts = {}
        for op in self.ops:
            if op.is_dma:
                k = ("d", op.semidx)
                op.sem = pool[op.semidx]
                counts[k] = counts.get(k, 0) + 16
                op.val = counts[k]
                op.signal = True
            else:
                op.sem = eng_sem[op.eng]
                if op.signal and op.fn is not None:
                    counts[op.eng] = counts.get(op.eng, 0) + 1
                op.val = counts.get(op.eng, 0)
        waited = {e: {} for e in ENGS}
        per_eng = {e: [] for e in ENGS}
        nw = 0
        for op in self.ops:
            w = waited[op.eng]
            need = {}
            for d in op.deps:
                key = id(d.sem)
                if w.get(key, 0) >= d.val:
                    continue
                if key not in need or need[key][1] < d.val:
                    need[key] = (d.sem, d.val)
            for key, (sem, val) in need.items():
                w[key] = val
                op.waits.append((sem, val))
            nw += len(op.waits)
            per_eng[op.eng].append(op)
        self.stats = {e: len(per_eng[e]) for e in ENGS}
        self.stats["waits"] = nw
        self.stats["maxsem"] = max(counts.values()) if counts else 0

        def run(eng_obj, lst):
            for op in lst:
                for sem, val in op.waits:
                    eng_obj.wait_ge(sem, val)
                if op.fn is None:
                    continue
                ins = op.fn(eng_obj)
                if op.signal:
                    ins.then_inc(op.sem, 16 if op.is_dma else 1)

        with nc.Block() as block:
            @block.tensor
            def _(e):
                run(e, per_eng["pe"])

            @block.scalar
            def _(e):
                run(e, per_eng["act"])

            @block.vector
            def _(e):
                run(e, per_eng["dve"])

            @block.gpsimd
            def _(e):
                run(e, per_eng["pool"])

            @block.sync
            def _(e):
                run(e, per_eng["sp"])
        stack.close()


def _inv_freq(rot):
    return (np.float32(ROPE_THETA) ** (-np.arange(0, rot, 2, dtype=np.float32) / np.float32(rot))).astype(np.float32)


CF_ID = 0
CF_ONES = 128
CF_INVF = 256
CF_SEL = 288
CF_HM = 288 + 1024
CF_N = CF_HM + 16
CB_ID = 0
CB_ONES = 128
CB_E = 256
CB_N = 256 + 1024


def _consts():
    cf = np.zeros((128, CF_N), np.float32)
    cf[:, CF_ID:CF_ID + 128] = np.eye(128, dtype=np.float32)
    cf[:, CF_ONES:CF_ONES + 128] = 1.0
    cf[:, CF_INVF:CF_INVF + 32] = np.concatenate([_inv_freq(16), _inv_freq(32), _inv_freq(16)])[None, :]
    cb = np.zeros((128, CB_N), np.float32)
    cb[:, CB_ID:CB_ID + 128] = np.eye(128, dtype=np.float32)
    cb[:, CB_ONES:CB_ONES + 128] = 1.0
    p = np.arange(128)
    j, q = p // 16, p % 16
    for g in range(8):
        cf[16 * g + q, CF_SEL + g * 128 + p] = 1.0
        cb[p, CB_E + g * 128 + 16 * g + q] = 1.0
    for h in range(16):
        cf[:, CF_HM + h] = (h // 2 == j).astype(np.float32)
    return cf, cb.astype(ml_dtypes.bfloat16)


class B:
    pass


class StopBuild(Exception):
    pass


def build(NT, debug=(), upto=99):
    NTL = NT // 128
    NQB = NT // 512
    nc = bass.Bass("TRN2", target_bir_lowering=False)
    S = Sched(nc)
    st = contextlib.ExitStack()

    def din(name, shape, dt=F32):
        return nc.dram_tensor(name, list(shape), dt, kind="ExternalInput").ap()

    def dscr(name, shape, dt):
        return nc.dram_tensor(name, list(shape), dt, kind="Internal").ap()

    x = din("x", [NT, D])
    mem = din("mem", [256, D])
    positions = din("positions", [NTL, 128], I32)
    a_w_in = din("a_w_in", [D, 3072]); a_lambda = din("a_lambda", [4, 64]); a_subln_g = din("a_subln_g", [128, 1])
    a_w_out = din("a_w_out", [D, D])
    b_w_in = din("b_w_in", [D, 624]); b_q_norm_g = din("b_q_norm_g", [256]); b_kv_norm_g = din("b_kv_norm_g", [256])
    b_w_uq = din("b_w_uq", [256, 1024]); b_w_qidx = din("b_w_qidx", [256, 1024])
    b_w_uk = din("b_w_uk", [8, 256, 96]); b_w_uv = din("b_w_uv", [8, 256, 128]); b_w_out = din("b_w_out", [D, D])
    mem_w_kv = din("mem_w_kv", [D, 2048]); xa_w_q = din("xa_w_q", [2, D, D]); xa_w_out = din("xa_w_out", [2, D, D])
    moe_w_group = din("moe_w_group", [2, D, 4]); moe_b_group = din("moe_b_group", [2, 4])
    moe_w_expert = din("moe_w_expert", [2, D, 16]); moe_b_expert = din("moe_b_expert", [2, 16])
    moe_w_gate = din("moe_w_gate", [2, 16, D, 512]); moe_w_up = din("moe_w_up", [2, 16, D, 512])
    moe_w_down = din("moe_w_down", [2, 16, 512, D])
    ln_g = din("ln_g", [2, 3, D]); ln_b = din("ln_b", [2, 3, D])
    cstf = din("cstf", [128, CF_N]); cstb = din("cstb", [128, CB_N], BF16)
    out = nc.dram_tensor("out", [NT, D], F32, kind="ExternalOutput").ap()
    dbg = {k: nc.dram_tensor("dbg_" + k, [NT, D], F32, kind="ExternalOutput").ap() for k in debug}

    Wb = {
        "a_w_in": dscr("wb_a_w_in", [D, 3072], BF16), "a_w_out": dscr("wb_a_w_out", [D, D], BF16),
        "b_w_in": dscr("wb_b_w_in", [D, 624], BF16), "b_w_uq": dscr("wb_b_w_uq", [256, 1024], BF16),
        "b_w_qidx": dscr("wb_b_w_qidx", [256, 1024], BF16), "b_w_uk": dscr("wb_b_w_uk", [8, 256, 96], BF16),
        "b_w_uv": dscr("wb_b_w_uv", [8, 256, 128], BF16), "b_w_out": dscr("wb_b_w_out", [D, D], BF16),
        "mem_w_kv": dscr("wb_mem_w_kv", [D, 2048], BF16), "xa_w_q": dscr("wb_xa_w_q", [2, D, D], BF16),
        "xa_w_out": dscr("wb_xa_w_out", [2, D, D], BF16),
        "moe_w_gate": dscr("wb_moe_w_gate", [2, 16, D, 512], BF16), "moe_w_up": dscr("wb_moe_w_up", [2, 16, D, 512], BF16),
        "moe_w_down": dscr("wb_moe_w_down", [2, 16, 512, D], BF16),
    }
    H = [dscr("H0", [NT, D], F32), dscr("H1", [NT, D], F32)]
    HT = dscr("HT", [128, 8, NT], BF16)
    QT = dscr("QT", [128, 8, NT], BF16)
    KT = dscr("KT", [128, 8, NT], BF16)
    V = dscr("V", [NT, D], BF16)
    OT = dscr("OT", [128, 8, NT], BF16)
    ROPE = dscr("ROPE", [NT, 128], F32)
    KVT = dscr("KVT", [128, 3, NT], BF16)
    CKV = dscr("CKV", [NT, 256], BF16)
    KIT = dscr("KIT", [128, NT], BF16)
    QFT = dscr("QFT", [128, 8, 3, NT], BF16)
    QIT = dscr("QIT", [128, NTL, 1024], BF16)
    WIX = dscr("WIX", [NT, 16], F32)
    NM = dscr("NM", [NT, NT], BF16)

    ARENA = 200 * 1024
    arena = st.enter_context(nc.sbuf_tensor("arena", [128, ARENA], U8))
    PS = st.enter_context(nc.psum_tensor("ps", [128, 4096], F32))
    state = {"off": 0, "mark": 0, "phase": "p0"}

    def sb(name, shape, dt):
        esz = 4 if dt in (F32, I32) else 2
        n = int(np.prod(shape[1:])) * esz
        n = (n + 63) // 64 * 64
        off = state["off"]
        assert off + n <= ARENA, "arena overflow in %s: %s" % (state["phase"], name)
        state["off"] = off + n
        v = arena[:, off:off + n].bitcast(dt)[:, 0:int(np.prod(shape[1:]))]
        if len(shape) == 3:
            v = v.rearrange("p (a b) -> p a b", a=shape[1])
        elif len(shape) == 4:
            v = v.rearrange("p (a b c) -> p a b c", a=shape[1], b=shape[2])
        return v

    def bank(i, n=1):
        return PS[:, i * 512:(i + n) * 512]

    def bank_bf(i):
        return PS[:, i * 512:(i + 1) * 512].bitcast(BF16)

    def phase(name):
        state["np"] = state.get("np", 0) + 1
        if upto >= 0 and state["np"] > upto:
            raise StopBuild()
        S.barrier()
        state["off"] = state["mark"]
        state["phase"] = name

    def mm(o, lhsT, rhs, start, stop, R, W):
        S.add("pe", lambda e: e.matmul(o, lhsT=lhsT, rhs=rhs, start=start, stop=stop), reads=R, writes=W)

    def tr(o, i, ident, R, W):
        S.add("pe", lambda e: e.transpose(out=o, in_=i, identity=ident), reads=R, writes=W)

    def act(o, i, func, R, W, bias=None, scale=None, accum=None, eng="act"):
        kw = {}
        if bias is not None:
            kw["bias"] = bias
        if scale is not None:
            kw["scale"] = scale
        if accum is not None:
            kw["accum_out"] = accum
        S.add("act", lambda e: e.activation(out=o, in_=i, func=func, **kw), reads=R, writes=W)

    def tt(o, a, b, op, R, W, eng="dve"):
        S.add(eng, lambda e: e.tensor_tensor(out=o, in0=a, in1=b, op=op), reads=R, writes=W)

    def ts(o, a, s1, s2, op0, op1, R, W, accum=None, eng="dve"):
        kw = {}
        if accum is not None:
            kw["accum_out"] = accum
        if op1 is None:
            S.add(eng, lambda e: e.tensor_scalar(out=o, in0=a, scalar1=s1, scalar2=None, op0=op0, **kw), reads=R, writes=W)
        else:
            S.add(eng, lambda e: e.tensor_scalar(out=o, in0=a, scalar1=s1, scalar2=s2, op0=op0, op1=op1, **kw), reads=R, writes=W)

    def stt(o, a, s, b, op0, op1, R, W):
        S.add("dve", lambda e: e.scalar_tensor_tensor(out=o, in0=a, scalar=s, in1=b, op0=op0, op1=op1), reads=R, writes=W)

    def cp(o, i, R, W, eng="dve"):
        if eng == "act":
            S.add("act", lambda e: e.activation(out=o, in_=i, func=AF.Copy), reads=R, writes=W)
        else:
            S.add(eng, lambda e: e.tensor_copy(out=o, in_=i), reads=R, writes=W)

    def red(o, i, op, R, W, negate=False):
        S.add("dve", lambda e: e.tensor_reduce(out=o, in_=i, axis=AX.X, op=op, negate=negate), reads=R, writes=W)

    def recip(o, i, R, W):
        S.add("dve", lambda e: e.reciprocal(out=o, in_=i), reads=R, writes=W)

    def memset(o, v, W, eng="pool", R=()):
        S.add(eng, lambda e: e.memset(o, v), reads=R, writes=W)

    def dma(eng, o, i, R, W, key=None, slow=False):
        if slow:
            S.add(eng, lambda e: e.dma_start(out=o, in_=i, allow_slow_non_contiguous=True), reads=R, writes=W, dma=True, semkey=key)
        else:
            S.add(eng, lambda e: e.dma_start(out=o, in_=i), reads=R, writes=W, dma=True, semkey=key)

    cf = sb("cf", [128, CF_N], F32)
    cb = sb("cb", [128, CB_N], BF16)
    epsc = sb("epsc", [128, 1], F32)
    memKT = sb("memKT", [128, 8, 256], BF16)
    memV = sb("memV", [128, 2, 1024], BF16)
    comb = sb("comb", [128, NTL, 16], F32)
    gbc = sb("gbc", [128, 1024], F32)
    bbc = sb("bbc", [128, 1024], F32)
    neglam = sb("neglam", [128, 1], F32)
    gsc = sb("gsc", [128, 1], F32)
    state["mark"] = state["off"]
    ident = cb[:, CB_ID:CB_ID + 128]
    ones_b = cb[:, CB_ONES:CB_ONES + 128]
    ident32 = cf[:, CF_ID:CF_ID + 128]
    ones32 = cf[:, CF_ONES:CF_ONES + 128]

    def sub(k):
        if upto == -k:
            raise StopBuild()

    def p0():
        dma("sp", cf, cstf, [], ["cf"])
        dma("sp", cb, cstb, [], ["cb"])
        memset(epsc, LN_EPS, ["epsc"])
        ci = [0]

        def cast(dst, src):
            dma("pool", dst, src, [], [("cast", ci[0])], key=("cast", ci[0] % 8))
            ci[0] += 1

        cast(Wb["a_w_in"], a_w_in); cast(Wb["a_w_out"], a_w_out); cast(Wb["mem_w_kv"], mem_w_kv)
        for i in range(2):
            cast(Wb["xa_w_q"][i], xa_w_q[i]); cast(Wb["xa_w_out"][i], xa_w_out[i])
        cast(Wb["b_w_in"], b_w_in); cast(Wb["b_w_uq"], b_w_uq); cast(Wb["b_w_qidx"], b_w_qidx)
        cast(Wb["b_w_uk"].rearrange("h r n -> (h r) n"), b_w_uk.rearrange("h r n -> (h r) n"))
        cast(Wb["b_w_uv"].rearrange("h r n -> (h r) n"), b_w_uv.rearrange("h r n -> (h r) n"))
        cast(Wb["b_w_out"], b_w_out)
        for i in range(2):
            for e in range(16):
                cast(Wb["moe_w_gate"][i, e], moe_w_gate[i, e]); cast(Wb["moe_w_up"][i, e], moe_w_up[i, e])
                cast(Wb["moe_w_down"][i, e], moe_w_down[i, e])

        sub(1)
        posi = sb("posi", [128, 128], I32)
        posf = sb("posf", [128, 128], F32)
        post = sb("post", [128, NTL], F32)
        ang = sb("ang", [128, NTL, 32], F32)
        kk = sb("kk", [128, NTL, 32], F32)
        sn = sb("sn", [128, NTL, 32], F32)
        cs = sb("cs", [128, NTL, 32], F32)
        tab = sb("tab", [128, NTL, 128], F32)
        dma("sp", posi[0:NTL, :], positions, [], ["posi"])
        cp(posf[0:NTL, :], posi[0:NTL, :], ["posi"], ["posf"])
        S.add("pe", lambda e: e.transpose(out=bank(0)[:, 0:NTL], in_=posf[0:NTL, :], identity=ident32[0:NTL, 0:NTL]),
              reads=["posf", "cf"], writes=["ps0"])
        cp(post, bank(0)[:, 0:NTL], ["ps0"], ["post"])
        invf = cf[:, CF_INVF:CF_INVF + 32]
        tt(ang, post.unsqueeze(2).to_broadcast([128, NTL, 32]), invf.unsqueeze(1).to_broadcast([128, NTL, 32]), ALU.mult,
           ["post", "cf"], ["ang"])
        MAGIC = 12582912.0
        TWO_PI = 2.0 * math.pi
        C1 = 6.28125
        C2 = float(np.float32(TWO_PI - C1))
        ts(kk, ang, 1.0 / TWO_PI, MAGIC, ALU.mult, ALU.add, ["ang"], ["kk"])
        ts(kk, kk, -MAGIC, None, ALU.add, None, ["kk"], ["kk"])
        stt(ang, kk, -C1, ang, ALU.mult, ALU.add, ["kk", "ang"], ["ang"])
        stt(ang, kk, -C2, ang, ALU.mult, ALU.add, ["kk", "ang"], ["ang"])
        ts(ang, ang, math.pi, -math.pi, ALU.min, ALU.max, ["ang"], ["ang"])
        act(sn, ang, AF.Sin, ["ang"], ["sn"])
        stt(kk, ang, -1.0, ang, ALU.mult, ALU.max, ["ang"], ["kk"])
        ts(kk, kk, -1.0, math.pi / 2, ALU.mult, ALU.add, ["kk"], ["kk"])
        act(cs, kk, AF.Sin, ["kk"], ["cs"])
        for (o0, f0, nf) in ((0, 0, 8), (32, 8, 16), (96, 24, 8)):
            cp(tab[:, :, o0:o0 + nf], cs[:, :, f0:f0 + nf], ["cs"], [("tab", o0, 0)])
            cp(tab[:, :, o0 + nf:o0 + 2 * nf], cs[:, :, f0:f0 + nf], ["cs"], [("tab", o0, 1)])
            ts(tab[:, :, o0 + 2 * nf:o0 + 3 * nf], sn[:, :, f0:f0 + nf], -1.0, None, ALU.mult, None, ["sn"], [("tab", o0, 2)])
            cp(tab[:, :, o0 + 3 * nf:o0 + 4 * nf], sn[:, :, f0:f0 + nf], ["sn"], [("tab", o0, 3)])
        dma("sp", ROPE.rearrange("(t p) c -> p t c", p=128), tab,
            [("tab", o0, k) for o0 in (0, 32, 96) for k in range(4)], ["ROPE"])

        sub(2)
        lam_init0 = 0.8 - 0.6 * math.exp(-0.3 * 0)
        lamb = sb("lamb", [128, 4, 64], F32)
        lamp = sb("lamp", [128, 2, 64], F32)
        lams = sb("lams", [128, 2], F32)
        dma("sp", lamb.rearrange("p a b -> p (a b)"), a_lambda.rearrange("a b -> (a b)").partition_broadcast(128), [], ["lamb"])
        tt(lamp[:, 0, :], lamb[:, 0, :], lamb[:, 1, :], ALU.mult, ["lamb"], [("lamp", 0)])
        tt(lamp[:, 1, :], lamb[:, 2, :], lamb[:, 3, :], ALU.mult, ["lamb"], [("lamp", 1)])
        red(lams, lamp, ALU.add, [("lamp", 0), ("lamp", 1)], ["lams"])
        act(lams, lams, AF.Exp, ["lams"], ["lams"])
        tt(neglam, lams[:, 1:2], lams[:, 0:1], ALU.subtract, ["lams"], ["neglam"])
        ts(neglam, neglam, -lam_init0, None, ALU.add, None, ["neglam"], ["neglam"])
        dma("sp", gsc, a_subln_g, [], ["gsc"])
        ts(gsc, gsc, 1.0 - lam_init0, None, ALU.mult, None, ["gsc"], ["gsc"])

        sub(3)
        S.barrier()
        state["phase"] = "p0b"
        wkv = sb("wkv", [128, 8, 2048], BF16)
        memf = sb("memf", [128, 2, 1024], F32)
        memb = sb("memb", [128, 2, 1024], BF16)
        memT = sb("memT", [128, 8, 256], BF16)
        dma("sp", wkv, Wb["mem_w_kv"].rearrange("(c p) n -> p c n", p=128), [], ["wkv"])
        dma("sp", memf, mem.rearrange("(t p) d -> p t d", p=128), [], ["memf"])
        cp(memb, memf, ["memf"], ["memb"], eng="act")
        for mt in range(2):
            ptv = bank_bf(mt).rearrange("p (c t) -> p c t", c=8)
            for c in range(8):
                tr(ptv[:, c, :], memb[:, mt, c * 128:(c + 1) * 128], ident, ["memb"], [("psb", mt)])
            cp(memT[:, :, mt * 128:(mt + 1) * 128], ptv, [("psb", mt)], [("memT", mt)])
        for j in range(8):
            pk = bank(2 + j % 2)[:, 0:256]
            for k in range(8):
                mm(pk, wkv[:, k, j * 128:(j + 1) * 128], memT[:, k, :], k == 0, k == 7, ["wkv", ("memT", 0), ("memT", 1)], [("pk", j % 2)])
            cp(memKT[:, j, :], pk, [("pk", j % 2)], [("memKT", j)], eng="act" if j % 2 else "dve")
        for mt in range(2):
            for hf in range(2):
                pv = bank(4 + (mt * 2 + hf) % 2)
                for k in range(8):
                    mm(pv, memT[:, k, mt * 128:(mt + 1) * 128], wkv[:, k, 1024 + hf * 512:1024 + (hf + 1) * 512], k == 0, k == 7,
                       ["wkv", ("memT", 0), ("memT", 1)], [("pv", (mt * 2 + hf) % 2)])
                cp(memV[:, mt, hf * 512:(hf + 1) * 512], pv, [("pv", (mt * 2 + hf) % 2)], [("memV", mt, hf)], eng="act" if hf else "dve")


    def load_ln(li, j):
        dma("sp", gbc, ln_g[li, j].partition_broadcast(128), [], ["gbc"])
        dma("sp", bbc, ln_b[li, j].partition_broadcast(128), [], ["bbc"])

    class Epi:
        def __init__(self, pt_bank, nbuf=2):
            self.nb = nbuf
            self.hsb = [sb("e_h%d" % i, [128, 1024], F32) for i in range(nbuf)]
            self.z = [sb("e_z%d" % i, [128, 1024], F32) for i in range(nbuf)]
            self.hb = [sb("e_hb%d" % i, [128, 1024], BF16) for i in range(nbuf)]
            self.hT = [sb("e_hT%d" % i, [128, 8, 128], BF16) for i in range(nbuf)]
            self.st = [sb("e_st%d" % i, [128, 2, 6], F32) for i in range(nbuf)]
            self.mv = [sb("e_mv%d" % i, [128, 2], F32) for i in range(nbuf)]
            self.rs = [sb("e_rs%d" % i, [128, 1], F32) for i in range(nbuf)]
            self.ptb = pt_bank
            self.n = 0

        def run(self, t, mix, mixR, h_src, h_dst, dbg_dst=None):
            b = self.n % self.nb
            self.n += 1
            hsb, z, hb, hT, stt_, mv, rs = self.hsb[b], self.z[b], self.hb[b], self.hT[b], self.st[b], self.mv[b], self.rs[b]
            k = "e%d" % b
            rows = slice(t * 128, (t + 1) * 128)
            dma("sp", hsb, h_src[rows, :], [("Hsrc", t)], [k + "h"])
            stt(z, hsb, ALPHA, mix, ALU.mult, ALU.add, [k + "h"] + mixR, [k + "z"])
            for c in range(2):
                S.add("dve", lambda e, c=c: e.bn_stats(out=stt_[:, c, :], in_=z[:, c * 512:(c + 1) * 512]), reads=[k + "z"], writes=[(k + "st", c)])
            S.add("dve", lambda e: e.bn_aggr(out=mv, in_=stt_.rearrange("p a b -> p (a b)")), reads=[(k + "st", 0), (k + "st", 1)], writes=[k + "mv"])
            act(rs, mv[:, 1:2], AF.Sqrt, [k + "mv"], [k + "rs"], bias=epsc)
            recip(rs, rs, [k + "rs"], [k + "rs"])
            ts(z, z, mv[:, 0:1], rs, ALU.subtract, ALU.mult, [k + "z", k + "mv", k + "rs"], [k + "z"])
            tt(z, z, gbc, ALU.mult, [k + "z", "gbc"], [k + "z"], eng="pool")
            tt(hsb, z, bbc, ALU.add, [k + "z", "bbc"], [k + "h"])
            dma(ST_ENG, h_dst[rows, :], hsb, [k + "h"], [("Hdst", t)], key=k + "hst")
            if dbg_dst is not None:
                dma(ST_ENG, dbg_dst[rows, :], hsb, [k + "h"], [("dbg", t)], key=k + "dbg")
            cp(hb, hsb, [k + "h"], [k + "hb"], eng="act")
            ptv = bank_bf(self.ptb).rearrange("p (c t) -> p c t", c=8)
            for c in range(8):
                tr(ptv[:, c, :], hb[:, c * 128:(c + 1) * 128], ident, [k + "hb"], ["e_pt"])
            cp(hT, ptv, ["e_pt"], [k + "hT"], eng="act")
            dma(ST_ENG, HT[:, :, rows], hT, [k + "hT"], [("HT", t)], key=k + "hTst")

    def t0():
        phase("t0")
        xs = [sb("xs%d" % i, [128, 1024], F32) for i in range(2)]
        xb = [sb("xb%d" % i, [128, 1024], BF16) for i in range(2)]
        xT = [sb("xT%d" % i, [128, 8, 128], BF16) for i in range(2)]
        for t in range(NTL):
            b = t % 2
            rows = slice(t * 128, (t + 1) * 128)
            dma("sp", xs[b], x[rows, :], [], [("xs", b)])
            cp(xb[b], xs[b], [("xs", b)], [("xb", b)], eng="act")
            ptv = bank_bf(b).rearrange("p (c t) -> p c t", c=8)
            for c in range(8):
                tr(ptv[:, c, :], xb[b][:, c * 128:(c + 1) * 128], ident, [("xb", b)], [("pt", b)])
            cp(xT[b], ptv, [("pt", b)], [("xT", b)])
            dma(ST_ENG, HT[:, :, rows], xT[b], [("xT", b)], [("HT", t)], key=("xTst", b))


    def rope(dst, src, nh, dh, rt, off, hf, tA, tB, R, W, key):
        cc = rt[:, off:off + 2 * hf].unsqueeze(1).to_broadcast([128, nh, 2 * hf])
        s1 = rt[:, off + 2 * hf:off + 3 * hf].unsqueeze(1).to_broadcast([128, nh, hf])
        s2 = rt[:, off + 3 * hf:off + 4 * hf].unsqueeze(1).to_broadcast([128, nh, hf])
        tt(tA[:, 0:nh, 0:2 * hf], src[:, :, 0:2 * hf], cc, ALU.mult, R, [key + "A"])
        tt(tB[:, 0:nh, 0:hf], src[:, :, hf:2 * hf], s1, ALU.mult, R, [key + "B0"])
        tt(tB[:, 0:nh, hf:2 * hf], src[:, :, 0:hf], s2, ALU.mult, R, [key + "B1"])
        tt(dst[:, :, 0:2 * hf], tA[:, 0:nh, 0:2 * hf], tB[:, 0:nh, 0:2 * hf], ALU.add, [key + "A", key + "B0", key + "B1"], W)
        if dh > 2 * hf:
            cp(dst[:, :, 2 * hf:dh], src[:, :, 2 * hf:dh], R, W, eng="act")

    def diff_attention(h_src, h_dst, dbg_dst):
        phase("a1")
        wA = sb("wA", [128, 8, 3072], BF16)
        hts = [sb("hts%d" % i, [128, 8, 128], BF16) for i in range(2)]
        rts = [sb("rts%d" % i, [128, 128], F32) for i in range(2)]
        tok = [sb("tok%d" % i, [128, 16, 64], BF16) for i in range(2)]
        vtok = [sb("vtok%d" % i, [128, 1024], BF16) for i in range(2)]
        tA = sb("tA", [128, 16, 32], F32)
        tB = sb("tB", [128, 16, 32], F32)
        oT = [sb("oT%d" % i, [128, 8, 128], BF16) for i in range(2)]
        dma("sp", wA, Wb["a_w_in"].rearrange("(c p) n -> p c n", p=128), [], ["wA"])
        n = 0
        for t in range(NTL):
            b = t % 2
            rows = slice(t * 128, (t + 1) * 128)
            dma("sp", hts[b], HT[:, :, rows], [], [("hts", b)])
            dma("sp", rts[b], ROPE[rows, :], [], [("rts", b)])
            for which in range(3):
                pb = 2 * (n % 2)
                pa = bank(pb, 2)
                for hf in range(2):
                    for k in range(8):
                        mm(pa[:, hf * 512:(hf + 1) * 512], hts[b][:, k, :], wA[:, k, which * 1024 + hf * 512: which * 1024 + (hf + 1) * 512],
                           k == 0, k == 7, [("hts", b), "wA"], [("pa", pb, hf)])
                paR = [("pa", pb, 0), ("pa", pb, 1)]
                if which < 2:
                    tb_ = tok[n % 2]
                    rope(tb_, pa.rearrange("p (h d) -> p h d", h=16), 16, 64, rts[b], 0, 8, tA, tB, paR + [("rts", b)], [("tok", n % 2)], "r")
                    ptb = 4 + n % 2
                    ptv = bank_bf(ptb).rearrange("p (c t) -> p c t", c=8)
                    tf = tb_.rearrange("p h d -> p (h d)")
                    for c in range(8):
                        tr(ptv[:, c, :], tf[:, c * 128:(c + 1) * 128], ident, [("tok", n % 2)], [("pt", ptb)])
                    ob = oT[n % 2]
                    if which == 0:
                        act(ob, ptv, AF.Copy, [("pt", ptb)], [("oT", n % 2)], scale=0.125)
                    else:
                        cp(ob, ptv, [("pt", ptb)], [("oT", n % 2)])
                    dma(ST_ENG, (QT if which == 0 else KT)[:, :, rows], ob, [("oT", n % 2)], [("QK", which, t)], key=("oTst", n % 2))
                else:
                    cp(vtok[b], pa, paR, [("vtok", b)], eng="act")
                    dma(ST_ENG, V[rows, :], vtok[b], [("vtok", b)], [("V", t)], key=("vst", b))
                n += 1

        phase("a2")
        KTh = sb("KTh", [128, NT], BF16)
        QTh = [sb("QTh%d" % c, [128, NT], BF16) for c in range(2)]
        Vh = sb("Vh", [128, NTL, 128], BF16)
        memset(QTh[0][64:128, :], 0.0, [("QThz", 0)])
        memset(QTh[1][0:64, :], 0.0, [("QThz", 1)])
        NPT = 4
        pT = [sb("pT%d" % i, [128, 512], BF16) for i in range(NPT)]
        r0 = sb("r0", [128, 512], F32); r1 = sb("r1", [128, 512], F32)
        a0 = sb("a0", [128, 512], F32); a1 = sb("a1", [128, 512], F32)
        od = sb("od", [128, 512], F32); sq = sb("sq", [128, 512], F32); rstd = sb("rstd", [128, 512], F32)
        on = [sb("on%d" % i, [128, 512], BF16) for i in range(2)]
        lacc = [sb("lacc%d" % c, [128, 512], F32) for c in range(2)]
        cnt_u = [0]
        for h in range(8):
            dma("sp", KTh, KT[:, h, :], [], ["KTh"])
            dma("sp", QTh[0][0:64, :], QT[0:64, h, :], [], [("QTh", 0)])
            dma("sp", QTh[1][64:128, :], QT[64:128, h, :], [], [("QTh", 1)])
            dma("sp", Vh, V[:, h * 128:(h + 1) * 128].rearrange("(t p) e -> p t e", p=128), [], ["Vh"])
            for qb in range(NQB):
                nj = 4 * qb + 4
                units = [(j, c) for j in range(nj) for c in range(2)]
                base = cnt_u[0]

                def qk(i, qb=qb, base=base, units=units):
                    j, c = units[i]
                    g = base + i
                    c0 = 0 if j < 4 * qb else 128 * (j - 4 * qb)
                    sbk = g % 4
                    ps_ = bank(sbk)
                    mm(ps_[:, c0:512], KTh[:, j * 128:(j + 1) * 128], QTh[c][:, qb * 512 + c0:(qb + 1) * 512], True, True,
                       ["KTh", ("QTh", c), ("QThz", c)], [("ps", sbk)])
                    pt_ = pT[g % NPT]
                    pk = ("pT", g % NPT)
                    act(pt_[:, c0:512], ps_[:, c0:512], AF.Exp, [("ps", sbk)], [pk])
                    if j >= 4 * qb:
                        memset(pt_[64:128, c0:c0 + 64], 0.0, [pk])

                def pv(i, qb=qb, base=base, units=units, nj=nj):
                    j, c = units[i]
                    g = base + i
                    c0 = 0 if j < 4 * qb else 128 * (j - 4 * qb)
                    pt_ = pT[g % NPT]
                    pk = ("pT", g % NPT)
                    mm(bank(4 + c)[:, c0:512], Vh[:, j, :], pt_[:, c0:512], j == 0, j == nj - 1, ["Vh", pk], [("po", c)])
                    if j == 0:
                        cp(lacc[c], pt_, [pk], [("lacc", c)])
                    else:
                        tt(lacc[c][:, c0:512], pt_[:, c0:512], lacc[c][:, c0:512], ALU.add, [pk, ("lacc", c)], [("lacc", c)])

                qk(0)
                qk(1)
                for i in range(len(units)):
                    if i + 2 < len(units):
                        qk(i + 2)
                    pv(i)
                cnt_u[0] += len(units)
                for c in range(2):
                    mm(bank(6 + c), ones32, lacc[c], True, True, [("lacc", c)], [("pl", c)])
                recip(r0, bank(6), [("pl", 0)], ["r0"])
                recip(r1, bank(7), [("pl", 1)], ["r1"])
                tt(a0, bank(4), r0, ALU.mult, [("po", 0), "r0"], ["a0"])
                tt(a1, bank(5), r1, ALU.mult, [("po", 1), "r1"], ["a1"])
                stt(od, a1, neglam, a0, ALU.mult, ALU.add, ["a0", "a1"], ["od"])
                act(sq, od, AF.Square, ["od"], ["sq"])
                sbk = cnt_u[0] % 4
                cnt_u[0] += 1
                mm(bank(sbk), ones32, sq, True, True, ["sq"], [("ps", sbk)])
                act(rstd, bank(sbk), AF.Sqrt, [("ps", sbk)], ["rstd"], bias=epsc, scale=1.0 / 128.0)
                recip(rstd, rstd, ["rstd"], ["rstd"])
                ob = on[(h * NQB + qb) % 2]
                stt(ob, od, gsc, rstd, ALU.mult, ALU.mult, ["od", "rstd"], [("on", (h * NQB + qb) % 2)])
                dma(ST_ENG, OT[:, h, qb * 512:(qb + 1) * 512], ob, [("on", (h * NQB + qb) % 2)], [("OT", h, qb)], key=("onst", (h * NQB + qb) % 2))

        out_proj(Wb["a_w_out"], h_src, h_dst, 0, 0, dbg_dst)

    def out_proj(wdram, h_src, h_dst, li, lj, dbg_dst):
        phase("a3")
        wO = sb("wO", [128, 8, 1024], BF16)
        ots = [sb("ots%d" % i, [128, 8, 128], BF16) for i in range(2)]
        ep = Epi(6)
        dma("sp", wO, wdram.rearrange("(c p) n -> p c n", p=128), [], ["wO"])
        load_ln(li, lj)
        for t in range(NTL):
            b = t % 2
            rows = slice(t * 128, (t + 1) * 128)
            dma("sp", ots[b], OT[:, :, rows], [], [("ots", b)])
            pm = bank(2 * b, 2)
            for hf in range(2):
                for c in range(8):
                    mm(pm[:, hf * 512:(hf + 1) * 512], ots[b][:, c, :], wO[:, c, hf * 512:(hf + 1) * 512], c == 0, c == 7,
                       [("ots", b), "wO"], [("pm", b, hf)])
            ep.run(t, pm, [("pm", b, 0), ("pm", b, 1)], h_src, h_dst, dbg_dst)

    def cross_attention(li, h_src, h_dst, dbg_dst):
        phase("x%d" % li)
        wq = sb("wq", [128, 8, 1024], BF16)
        wo = sb("wo", [128, 8, 1024], BF16)
        hts = [sb("hts%d" % i, [128, 8, 128], BF16) for i in range(2)]
        qT = sb("qT", [128, 8, 128], BF16)
        nmx = sb("nmx", [128, 4], F32)
        lsum = sb("lsum", [128, 4], F32)
        P = sb("P", [128, 4, 256], BF16)
        Pn = sb("Pn", [128, 4, 256], BF16)
        PT = sb("PT", [128, 8, 128], BF16)
        oT = sb("oT", [128, 8, 128], BF16)
        ep = Epi(7)
        dma("sp", wq, Wb["xa_w_q"][li].rearrange("(c p) n -> p c n", p=128), [], ["wq"])
        dma("sp", wo, Wb["xa_w_out"][li].rearrange("(c p) n -> p c n", p=128), [], ["wo"])
        load_ln(li, 1)
        pA = bank(0, 2).rearrange("p (c t) -> p c t", c=8)
        psc = bank(2, 2).rearrange("p (h m) -> p h m", h=4)
        ptp = bank_bf(4).rearrange("p (c t) -> p c t", c=8)
        pmix = bank(5, 2)
        for t in range(NTL):
            b = t % 2
            rows = slice(t * 128, (t + 1) * 128)
            dma("sp", hts[b], HT[:, :, rows], [], [("hts", b)])
            for j in range(8):
                for k in range(8):
                    mm(pA[:, j, :], wq[:, k, j * 128:(j + 1) * 128], hts[b][:, k, :], k == 0, k == 7, ["wq", ("hts", b)], [("pA", j // 4)])
            act(qT, pA, AF.Copy, [("pA", 0), ("pA", 1)], ["qT"], scale=1.0 / 16.0)
            for hh in range(4):
                for dc in range(2):
                    mm(psc[:, hh, :], qT[:, 2 * hh + dc, :], memKT[:, 2 * hh + dc, :], dc == 0, dc == 1, ["qT"], [("psc", hh // 2)])
            red(nmx, psc, ALU.max, [("psc", 0), ("psc", 1)], ["nmx"], negate=True)
            for hh in range(4):
                act(P[:, hh, :], psc[:, hh, :], AF.Exp, [("psc", hh // 2), "nmx"], [("P", hh)], bias=nmx[:, hh:hh + 1], accum=lsum[:, hh:hh + 1])
            recip(lsum, lsum, [("P", hh) for hh in range(4)], ["lsum"])
            tt(Pn, P, lsum.unsqueeze(2).to_broadcast([128, 4, 256]), ALU.mult, [("P", hh) for hh in range(4)] + ["lsum"], ["Pn"])
            for hh in range(4):
                for mc in range(2):
                    tr(ptp[:, hh * 2 + mc, :], Pn[:, hh, mc * 128:(mc + 1) * 128], ident, ["Pn"], ["ptp"])
            cp(PT, ptp, ["ptp"], ["PT"])
            for hh in range(4):
                for dch in range(2):
                    for mc in range(2):
                        mm(pA[:, hh * 2 + dch, :], memV[:, mc, hh * 256 + dch * 128: hh * 256 + (dch + 1) * 128], PT[:, hh * 2 + mc, :],
                           mc == 0, mc == 1, ["PT"], [("pA", (hh * 2 + dch) // 4)])
            cp(oT, pA, [("pA", 0), ("pA", 1)], ["oT"], eng="act")
            for hf in range(2):
                for c in range(8):
                    mm(pmix[:, hf * 512:(hf + 1) * 512], oT[:, c, :], wo[:, c, hf * 512:(hf + 1) * 512], c == 0, c == 7, ["oT", "wo"], [("pmix", hf)])
            ep.run(t, pmix, [("pmix", 0), ("pmix", 1)], h_src, h_dst, dbg_dst)

    def moe(li, h_src, h_dst, dbg_dst):
        phase("m1_%d" % li)
        wr = sb("wr", [128, 8, 20], F32)
        whi = sb("whi", [128, 8, 20], BF16)
        wlo = sb("wlo", [128, 8, 20], BF16)
        rb = sb("rb", [128, 20], F32)
        LG = sb("LG", [128, NTL, 20], F32)
        hs = [sb("hs%d" % i, [128, 1024], F32) for i in range(2)]
        hhi = [sb("hhi%d" % i, [128, 1024], BF16) for i in range(2)]
        hlo = [sb("hlo%d" % i, [128, 1024], BF16) for i in range(2)]
        hiT = [sb("hiT%d" % i, [128, 8, 128], BF16) for i in range(2)]
        loT = [sb("loT%d" % i, [128, 8, 128], BF16) for i in range(2)]
        dma("sp", wr[:, :, 0:4], moe_w_group[li].rearrange("(c p) n -> p c n", p=128), [], [("wr", 0)], slow=True)
        dma("sp", wr[:, :, 4:20], moe_w_expert[li].rearrange("(c p) n -> p c n", p=128), [], [("wr", 1)], slow=True)
        dma("sp", rb[:, 0:4], moe_b_group[li].partition_broadcast(128), [], [("rb", 0)])
        dma("sp", rb[:, 4:20], moe_b_expert[li].partition_broadcast(128), [], [("rb", 1)])
        cp(whi, wr, [("wr", 0), ("wr", 1)], ["whi"])
        tt(wlo, wr, whi, ALU.subtract, [("wr", 0), ("wr", 1), "whi"], ["wlo"])
        for t in range(NTL):
            b = t % 2
            rows = slice(t * 128, (t + 1) * 128)
            dma("sp", hs[b], h_src[rows, :], [], [("hs", b)])
            cp(hhi[b], hs[b], [("hs", b)], [("hhi", b)], eng="act")
            tt(hlo[b], hs[b], hhi[b], ALU.subtract, [("hs", b), ("hhi", b)], [("hlo", b)])
            p1 = bank_bf(2 * b).rearrange("p (c t) -> p c t", c=8)
            p2 = bank_bf(2 * b + 1).rearrange("p (c t) -> p c t", c=8)
            for c in range(8):
                tr(p1[:, c, :], hhi[b][:, c * 128:(c + 1) * 128], ident, [("hhi", b)], [("p1", b)])
            for c in range(8):
                tr(p2[:, c, :], hlo[b][:, c * 128:(c + 1) * 128], ident, [("hlo", b)], [("p2", b)])
            cp(hiT[b], p1, [("p1", b)], [("hiT", b)], eng="act")
            cp(loT[b], p2, [("p2", b)], [("loT", b)])
            pl = bank(4 + b)[:, 0:20]
            n = 0
            for (aT, w_, ka, kw_) in ((hiT[b], whi, ("hiT", b), "whi"), (hiT[b], wlo, ("hiT", b), "wlo"), (loT[b], whi, ("loT", b), "whi")):
                for k in range(8):
                    mm(pl, aT[:, k, :], w_[:, k, :], n == 0, n == 23, [ka, kw_], [("pl", b)])
                    n += 1
            tt(LG[:, t, :], pl, rb, ALU.add, [("pl", b), ("rb", 0), ("rb", 1)], [("LG", t)])
        T = NTL
        G = LG[:, :, 0:4]
        E = LG[:, :, 4:20].rearrange("p t (g j) -> p t g j", g=4)
        LGR = [("LG", t) for t in range(NTL)]
        gmax = sb("gmax", [128, T], F32); goh = sb("goh", [128, T, 4], F32); ge = sb("ge", [128, T, 4], F32)
        gsum = sb("gsum", [128, T], F32); tmp4 = sb("tmp4", [128, T, 4, 4], F32); esel = sb("esel", [128, T, 4], F32)
        m1 = sb("m1", [128, T], F32); oh1 = sb("oh1", [128, T, 4], F32); e2 = sb("e2", [128, T, 4], F32)
        m2 = sb("m2", [128, T], F32); oh2 = sb("oh2", [128, T, 4], F32); dd = sb("dd", [128, T], F32)
        w1 = sb("w1", [128, T], F32); w2 = sb("w2", [128, T], F32); cin = sb("cin", [128, T, 4], F32); cin2 = sb("cin2", [128, T, 4], F32)

        def bc3(a):
            return a.unsqueeze(2).to_broadcast([128, T, 4])

        red(gmax, G, ALU.max, LGR, ["gmax"])
        tt(goh, G, bc3(gmax), ALU.is_equal, LGR + ["gmax"], ["goh"])
        tt(ge, G, bc3(gmax), ALU.subtract, LGR + ["gmax"], ["ge"])
        act(ge, ge, AF.Exp, ["ge"], ["ge"])
        red(gsum, ge, ALU.add, ["ge"], ["gsum"])
        recip(gsum, gsum, ["gsum"], ["gsum"])
        tt(tmp4, E, goh.unsqueeze(3).to_broadcast([128, T, 4, 4]), ALU.mult, LGR + ["goh"], ["tmp4"])
        red(esel, tmp4.rearrange("p t g j -> p t j g"), ALU.add, ["tmp4"], ["esel"])
        red(m1, esel, ALU.max, ["esel"], ["m1"])
        tt(oh1, esel, bc3(m1), ALU.is_equal, ["esel", "m1"], ["oh1"])
        stt(e2, oh1, -1e30, esel, ALU.mult, ALU.add, ["oh1", "esel"], ["e2"])
        red(m2, e2, ALU.max, ["e2"], ["m2"])
        tt(oh2, e2, bc3(m2), ALU.is_equal, ["e2", "m2"], ["oh2"])
        tt(dd, m2, m1, ALU.subtract, ["m1", "m2"], ["dd"])
        act(dd, dd, AF.Exp, ["dd"], ["dd"])
        ts(w1, dd, 1.0, None, ALU.add, None, ["dd"], ["w1"])
        recip(w1, w1, ["w1"], ["w1"])
        tt(w2, dd, w1, ALU.mult, ["dd", "w1"], ["w2"])
        tt(w1, w1, gsum, ALU.mult, ["w1", "gsum"], ["w1"])
        tt(w2, w2, gsum, ALU.mult, ["w2", "gsum"], ["w2"])
        tt(cin, oh1, bc3(w1), ALU.mult, ["oh1", "w1"], ["cin"])
        tt(cin2, oh2, bc3(w2), ALU.mult, ["oh2", "w2"], ["cin2"])
        tt(cin, cin, cin2, ALU.add, ["cin", "cin2"], ["cin"])
        tt(comb.rearrange("p t (g j) -> p t g j", g=4), goh.unsqueeze(3).to_broadcast([128, T, 4, 4]),
           cin.unsqueeze(2).to_broadcast([128, T, 4, 4]), ALU.mult, ["goh", "cin"], ["comb"])

        phase("m2_%d" % li)
        TB = 1024
        hTb = sb("hTb", [128, 8, TB], BF16)
        yacc = sb("yacc", [128, 8, 1024], F32)
        wg = [sb("wg%d" % i, [128, 8, 512], BF16) for i in range(2)]
        wu = [sb("wu%d" % i, [128, 8, 512], BF16) for i in range(2)]
        wd = [sb("wd%d" % i, [128, 4, 1024], BF16) for i in range(2)]
        sg = [sb("sg%d" % i, [128, 512], F32) for i in range(2)]
        hid = [sb("hid%d" % i, [128, 4, 512], BF16) for i in range(2)]
        ep = Epi(6)
        load_ln(li, 2)
        nh = 0
        ny = 0
        for blk in range(NT // TB):
            dma("sp", hTb, HT[:, :, blk * TB:(blk + 1) * TB], [], ["hTb"])
            memset(yacc, 0.0, ["yacc"] + [("yacc", tl, dh) for tl in range(8) for dh in range(2)])
            for e in range(16):
                wbuf = e % 2
                dma("sp", wg[wbuf], Wb["moe_w_gate"][li, e].rearrange("(c p) n -> p c n", p=128), [], [("wg", wbuf)])
                dma("sp", wu[wbuf], Wb["moe_w_up"][li, e].rearrange("(c p) n -> p c n", p=128), [], [("wu", wbuf)])
                dma("sp", wd[wbuf], Wb["moe_w_down"][li, e].rearrange("(c p) n -> p c n", p=128), [], [("wd", wbuf)])
                for half in range(TB // 512):
                    hb_ = hid[nh % 2]
                    hk = ("hid", nh % 2)
                    for fc in range(4):
                        pg = bank(0 + (fc % 2))
                        pu = bank(2 + (fc % 2))
                        for k in range(8):
                            mm(pg, wg[wbuf][:, k, fc * 128:(fc + 1) * 128], hTb[:, k, half * 512:(half + 1) * 512], k == 0, k == 7,
                               [("wg", wbuf), "hTb"], [("pg", fc % 2)])
                        for k in range(8):
                            mm(pu, wu[wbuf][:, k, fc * 128:(fc + 1) * 128], hTb[:, k, half * 512:(half + 1) * 512], k == 0, k == 7,
                               [("wu", wbuf), "hTb"], [("pu", fc % 2)])
                        act(sg[fc % 2], pg, AF.Silu, [("pg", fc % 2)], [("sg", fc % 2)])
                        tt(hb_[:, fc, :], sg[fc % 2], pu, ALU.mult, [("sg", fc % 2), ("pu", fc % 2)], [hk + (fc,)])
                    hR = [hk + (fc,) for fc in range(4)]
                    for tt_ in range(4):
                        tl = half * 4 + tt_
                        tg = blk * 8 + tl
                        for dh in range(2):
                            py = bank(4 + ny % 2)
                            for fc in range(4):
                                mm(py, hb_[:, fc, tt_ * 128:(tt_ + 1) * 128], wd[wbuf][:, fc, dh * 512:(dh + 1) * 512], fc == 0, fc == 3,
                                   hR + [("wd", wbuf)], [("py", ny % 2)])
                            ya = yacc[:, tl, dh * 512:(dh + 1) * 512]
                            stt(ya, py, comb[:, tg, e:e + 1], ya, ALU.mult, ALU.add, [("py", ny % 2), "yacc", ("yacc", tl, dh)], [("yacc", tl, dh)])
                            ny += 1
                    nh += 1
            for tl in range(8):
                ep.run(blk * 8 + tl, yacc[:, tl, :], [("yacc", tl, 0), ("yacc", tl, 1)], h_src, h_dst, dbg_dst)

    def dsa(h_src, h_dst, dbg_dst):
        phase("d1")
        wI = sb("wI", [128, 8, 624], BF16)
        wUQ = sb("wUQ", [128, 2, 1024], BF16)
        wQI = sb("wQI", [128, 2, 1024], BF16)
        wUKT = sb("wUKT", [128, 8, 256], BF16)
        gq = sb("gq", [128, 256], F32)
        gkv = sb("gkv", [128, 256], F32)
        hts = [sb("hts%d" % i, [128, 8, 128], BF16) for i in range(2)]
        rts = [sb("rts%d" % i, [128, 128], F32) for i in range(2)]
        sqj = sb("sqj", [128, 256], F32)
        ssq = sb("ssq", [128, 2], F32)
        cqn = sb("cqn", [128, 256], BF16)
        ckn = sb("ckn", [128, 256], BF16)
        cqT = sb("cqT", [128, 2, 128], BF16)
        kvTs = sb("kvTs", [128, 3, 128], BF16)
        krp = sb("krp", [128, 1, 128], BF16)
        kix = sb("kix", [128, 2, 64], BF16)
        kiT = sb("kiT", [128, 128], BF16)
        wix = sb("wix", [128, 16], F32)
        tA = sb("tA", [128, 16, 32], F32)
        tB = sb("tB", [128, 16, 32], F32)
        qtk = sb("qtk", [128, 8, 128], BF16)
        qiT = sb("qiT", [128, 8, 8, 16], BF16)
        qT = sb("qT", [128, 8, 128], BF16)
        qfT = sb("qfT", [128, 8, 3, 128], BF16)
        qiks = [sb("qik%d" % i, [128, 16, 64], BF16) for i in range(2)]
        dma("sp", wI, Wb["b_w_in"].rearrange("(c p) n -> p c n", p=128), [], ["wI"])
        dma("sp", wUQ, Wb["b_w_uq"].rearrange("(c p) n -> p c n", p=128), [], ["wUQ"])
        dma("sp", wQI, Wb["b_w_qidx"].rearrange("(c p) n -> p c n", p=128), [], ["wQI"])
        wukn = sb("wukn", [128, 8, 2, 128], BF16)
        memset(wukn, 0.0, ["wukn"])
        for rc in range(2):
            dma("sp", wukn[:, :, rc, 32:128], Wb["b_w_uk"][:, rc * 128:(rc + 1) * 128, :].rearrange("h p n -> p h n"), [], ["wukn", ("wukn", rc)], key=("wukn", rc))
        for hh in range(8):
            ptw = bank_bf(hh % 2).rearrange("p (c t) -> p c t", c=8)
            for rc in range(2):
                tr(ptw[:, rc, :], wukn[:, hh, rc, :], ident, ["wukn", ("wukn", 0), ("wukn", 1)], ["p%d" % (hh % 2)])
            cp(wUKT[:, hh, :].rearrange("p (c t) -> p c t", c=2), ptw[:, 0:2, :], ["p%d" % (hh % 2)], [("wUKT", hh)])
        wUKR = [("wUKT", hh) for hh in range(8)]
        dma("sp", gq, b_q_norm_g.partition_broadcast(128), [], ["gq"])
        dma("sp", gkv, b_kv_norm_g.partition_broadcast(128), [], ["gkv"])
        memset(kvTs, 0.0, ["kvTs"])
        memset(krp, 0.0, ["krp"])
        memset(qfT, 0.0, ["qfT"])
        sub(11)
        for t in range(NTL):
            b = t % 2
            rows = slice(t * 128, (t + 1) * 128)
            dma("sp", hts[b], HT[:, :, rows], [], [("hts", b)])
            dma("sp", rts[b], ROPE[rows, :], [], [("rts", b)])
            p0 = bank(0)
            p1 = bank(1)[:, 0:112]
            for k in range(8):
                mm(p0, hts[b][:, k, :], wI[:, k, 0:512], k == 0, k == 7, [("hts", b), "wI"], ["p0"])
            for k in range(8):
                mm(p1, hts[b][:, k, :], wI[:, k, 512:624], k == 0, k == 7, [("hts", b), "wI"], ["p1"])
            for i, (gg, dst, nm_) in enumerate(((gq, cqn, "cqn"), (gkv, ckn, "ckn"))):
                act(sqj, p0[:, i * 256:(i + 1) * 256], AF.Square, ["p0"], ["sqj"], accum=ssq[:, i:i + 1])
                act(ssq[:, i:i + 1], ssq[:, i:i + 1], AF.Sqrt, ["sqj"], [("ssq", i)], bias=epsc, scale=1.0 / 256.0)
                recip(ssq[:, i:i + 1], ssq[:, i:i + 1], [("ssq", i)], [("ssq", i)])
                stt(dst, p0[:, i * 256:(i + 1) * 256], ssq[:, i:i + 1], gg, ALU.mult, ALU.mult, ["p0", ("ssq", i), "gq", "gkv"], [nm_])
            dma(ST_ENG, CKV[rows, :], ckn, ["ckn"], [("CKV", t)], key="ckvst")
            sub(12)
            rope(krp[:, :, 0:32], p1[:, 0:32].rearrange("p (h d) -> p h d", h=1), 1, 32, rts[b], 32, 16, tA, tB, ["p1", ("rts", b)], ["krp"], "rk")
            rope(kix[:, 0:1, :], p1[:, 32:96].rearrange("p (h d) -> p h d", h=1), 1, 64, rts[b], 96, 8, tA, tB, ["p1", ("rts", b)], [("kix", 0)], "ri")
            cp(kix[:, 1:2, :], kix[:, 0:1, :], [("kix", 0)], [("kix", 1)])
            ts(wix, p1[:, 96:112], 1.0 / 32.0, None, ALU.mult, None, ["p1"], ["wix"])
            dma(ST_ENG, WIX[rows, :], wix, ["wix"], [("WIX", t)], key="wixst")
            sub(13)
            pt = bank_bf(2).rearrange("p (c t) -> p c t", c=8)
            for c in range(2):
                tr(pt[:, c, :], cqn[:, c * 128:(c + 1) * 128], ident, ["cqn"], ["pt2"])
            sub(1310)
            for c in range(2):
                tr(pt[:, 2 + c, :], ckn[:, c * 128:(c + 1) * 128], ident, ["ckn", ("CKV", t)], ["pt2"])
            sub(1311)
            tr(pt[:, 4, :], krp.rearrange("p h d -> p (h d)"), ident, ["krp"], ["pt2"])
            tr(pt[:, 5, :], kix.rearrange("p a d -> p (a d)"), ident, [("kix", 0), ("kix", 1)], ["pt2"])
            sub(131)
            cp(cqT, pt[:, 0:2, :], ["pt2"], ["cqT"])
            cp(kvTs[:, 0:2, :], pt[:, 2:4, :], ["pt2"], [("kvTs", 0)], eng="act")
            cp(kvTs[:, 2, :], pt[:, 4, :], ["pt2"], [("kvTs", 1)])
            cp(kiT, pt[:, 5, :], ["pt2"], ["kiT"], eng="act")
            sub(132)
            dma(ST_ENG, KVT[:, :, rows], kvTs, ["kvTs", ("kvTs", 0), ("kvTs", 1)], [("KVT", t)], key="kvtst")
            sub(133)
            dma(ST_ENG, KIT[:, rows], kiT, ["kiT"], [("KIT", t)], key="kitst")
            sub(14)
            pq = bank(3, 2)
            for hf in range(2):
                for c in range(2):
                    mm(pq[:, hf * 512:(hf + 1) * 512], cqT[:, c, :], wUQ[:, c, hf * 512:(hf + 1) * 512], c == 0, c == 1, ["cqT", "wUQ"], [("pq", hf)])
            rope(qtk, pq.rearrange("p (h d) -> p h d", h=8), 8, 128, rts[b], 32, 16, tA, tB, [("pq", 0), ("pq", 1), ("rts", b)], ["qtk"], "rq")
            sub(15)
            pqi = bank(5, 2)
            for hf in range(2):
                for c in range(2):
                    mm(pqi[:, hf * 512:(hf + 1) * 512], cqT[:, c, :], wQI[:, c, hf * 512:(hf + 1) * 512], c == 0, c == 1, ["cqT", "wQI"], [("pqi", hf)])
            qik = qiks[b]
            rope(qik, pqi.rearrange("p (h d) -> p h d", h=16), 16, 64, rts[b], 96, 8, tA, tB, [("pqi", 0), ("pqi", 1), ("rts", b)], [("qik", b)], "rqi")
            sub(16)
            pt7 = bank_bf(7).rearrange("p (c t) -> p c t", c=8)
            qf = qtk.rearrange("p h d -> p (h d)")
            for hh in range(8):
                tr(pt7[:, hh, :], qf[:, hh * 128:(hh + 1) * 128], ident, ["qtk"], ["pt7"])
            cp(qT, pt7, ["pt7"], ["qT"])
            qif = qik.rearrange("p h d -> p (h d)")
            for c in range(8):
                tr(pt7[:, c, :], qif[:, c * 128:(c + 1) * 128], ident, [("qik", b)], ["pt7"])
            cp(qiT.rearrange("p g j q -> p j g q"), pt7.rearrange("p j (g q) -> p j g q", g=8), ["pt7"], ["qiT"])
            dma(ST_ENG, QIT[:, t, :], qiT.rearrange("p g j q -> p (g j q)"), ["qiT"], [("QIT", t)], key="qitst")
            sub(17)
            sc = 1.0 / math.sqrt(128.0)
            pl_ = bank(0, 2).rearrange("p (c t) -> p c t", c=8)
            for half in range(2):
                for hh4 in range(4):
                    hh = half * 4 + hh4
                    for rc in range(2):
                        mm(pl_[:, hh4 * 2 + rc, :], wUKT[:, hh, rc * 128:(rc + 1) * 128], qT[:, hh, :], True, True,
                           wUKR + ["qT"], ["p0" if (hh4 * 2 + rc) < 4 else "p1"])
                for hh4 in range(4):
                    hh = half * 4 + hh4
                    act(qfT[:, hh, 0:2, :], pl_[:, hh4 * 2:hh4 * 2 + 2, :], AF.Copy, ["p0", "p1"], [("qfT", hh)], scale=sc)
            act(qfT[0:32, :, 2, :], qT[0:32, :, :], AF.Copy, ["qT"], [("qfTr")], scale=sc)
            dma(ST_ENG, QFT[:, :, :, rows], qfT, ["qfT", "qfTr"] + [("qfT", hh) for hh in range(8)], [("QFT", t)], key="qftst")

        phase("d2")
        KIs = sb("KIs", [128, NT], BF16)
        qis = [[sb("qis%d_%d" % (i, par), [128, 1024], BF16) for par in range(2)] for i in range(2)]
        for i in range(2):
            memset(qis[i][0][64:128, :], 0.0, [("qisz", i, 0)])
            memset(qis[i][1][0:64, :], 0.0, [("qisz", i, 1)])
        wxs = [sb("wxs%d" % i, [128, 16], F32) for i in range(2)]
        wsel = sb("wsel", [128, 8, 16], F32)
        wc = sb("wc", [128, 8, 2], F32)
        Wblk = [sb("Wblk%d" % i, [128, 16, 128], BF16) for i in range(2)]
        NRB = 4
        Rb = [sb("Rb%d" % i, [128, 512], BF16) for i in range(NRB)]
        sc_ = [sb("sc%d" % i, [128, NT], F32) for i in range(2)]
        junk = sb("junk", [128, NT], BF16)
        nmk = [sb("nmk%d" % i, [128, NT], BF16) for i in range(2)]
        lo = [sb("lo%d" % i, [128, 1], F32) for i in range(2)]
        hi = [sb("hi%d" % i, [128, 1], F32) for i in range(2)]
        stp = [sb("stp%d" % i, [128, 20], F32) for i in range(2)]
        mid = [sb("mid%d" % i, [128, 1], F32) for i in range(2)]
        cnt = [sb("cnt%d" % i, [128, 1], F32) for i in range(2)]
        geb = [sb("geb%d" % i, [128, 1], F32) for i in range(2)]
        NIT = 18
        hmask = cf[:, CF_HM:CF_HM + 16]
        dma("sp", KIs, KIT, [], ["KIs"])
        nlg = [0]

        def prep(t):
            b = t % 2
            rows = slice(t * 128, (t + 1) * 128)
            dma("sp", qis[b][0][0:64, :], QIT[0:64, t, :], [], [("qis", b, 0)])
            dma("sp", qis[b][1][64:128, :], QIT[64:128, t, :], [], [("qis", b, 1)])
            dma("sp", wxs[b], WIX[rows, :], [], [("wxs", b)])
            pw = bank(6)[:, 0:128].rearrange("p (g h) -> p g h", g=8)
            for g in range(8):
                mm(pw[:, g, :], cf[:, CF_SEL + g * 128:CF_SEL + (g + 1) * 128], wxs[b], True, True, [("wxs", b)], ["pw"])
            tt(wsel, pw, hmask.unsqueeze(1).to_broadcast([128, 8, 16]), ALU.mult, ["pw"], ["wsel"])
            red(wc, wsel.rearrange("p g (j r) -> p g r j", r=2), ALU.add, ["wsel"], ["wc"])
            for g in range(8):
                for par in range(2):
                    ts(Wblk[b][:, g * 2 + par, :], cb[:, CB_E + g * 128:CB_E + (g + 1) * 128], wc[:, g, par:par + 1], None, ALU.mult, None,
                       ["wc"], [("Wblk", b, g * 2 + par)], eng="pool")

        def main(t):
            b = t % 2
            nk = 128 * (t + 1)
            WR = [("Wblk", b, i) for i in range(16)]
            nkb = (nk + 511) // 512
            steps = [(kb, i) for kb in range(nkb) for i in range(16)]
            base = nlg[0]

            def geom(kb):
                kw = min(512, nk - kb * 512)
                return kw, slice(kb * 512, kb * 512 + kw)

            def lg(s):
                kb, i = steps[s]
                kw, ks = geom(kb)
                n = base + s
                g, par = i // 2, i % 2
                plg = bank(n % 4)
                mm(plg[:, 0:kw], qis[b][par][:, g * 128:(g + 1) * 128], KIs[:, ks], True, True, [("qis", b, par), ("qisz", b, par), "KIs"], [("plg", n % 4)])
                act(Rb[n % NRB][:, 0:kw], plg[:, 0:kw], AF.Relu, [("plg", n % 4)], [("Rb", n % NRB)])

            def scm(s):
                kb, i = steps[s]
                kw, ks = geom(kb)
                n = base + s
                pscore = bank(4 + kb % 2)
                mm(pscore[:, 0:kw], Wblk[b][:, i, :], Rb[n % NRB][:, 0:kw], i == 0, i == 15, WR + [("Rb", n % NRB)], [("pscore", kb % 2)])
                if i == 15:
                    cp(sc_[b][:, ks], pscore[:, 0:kw], [("pscore", kb % 2)], [("sc", b, kb)], eng="act")

            lg(0)
            lg(1)
            for s in range(len(steps)):
                if s + 2 < len(steps):
                    lg(s + 2)
                scm(s)
            nlg[0] += len(steps)

        def bisect(t):
            b = t % 2
            rows = slice(t * 128, (t + 1) * 128)
            nk = 128 * (t + 1)
            nkb = (nk + 511) // 512
            scR = [("sc", b, kb) for kb in range(nkb)]
            scK = ("scall", b)
            memset(sc_[b][0:64, t * 128 + 64:(t + 1) * 128], -1e30, [scK], R=scR)
            sv = sc_[b][:, 0:nk]
            if t >= 2:
                red(lo[b], sc_[b][:, 0:nk - 64], ALU.min, scR + [scK], [("lo", b)])
                red(hi[b], sv, ALU.max, scR + [scK], [("hi", b)])
                tt(mid[b], hi[b], lo[b], ALU.subtract, [("lo", b), ("hi", b)], [("mid", b)])
                for i in range(NIT):
                    ts(stp[b][:, i:i + 1], mid[b], 2.0 ** -(i + 1), None, ALU.mult, None, [("mid", b)], [("stp", b, i)], eng="pool")
                for i in range(NIT):
                    tt(mid[b], lo[b], stp[b][:, i:i + 1], ALU.add, [("lo", b), ("stp", b, i)], [("mid", b)])
                    ts(junk[:, 0:nk], sv, mid[b], 0.0, ALU.is_ge, ALU.add, scR + [scK, ("mid", b)], ["junk", ("cnt", b)], accum=cnt[b])
                    ts(geb[b], cnt[b], 255.5, stp[b][:, i:i + 1], ALU.is_ge, ALU.mult, [("cnt", b), ("stp", b, i)], [("geb", b)])
                    tt(lo[b], lo[b], geb[b], ALU.add, [("lo", b), ("geb", b)], [("lo", b)])
            else:
                memset(lo[b], -1e29, [("lo", b)], eng="dve")
            ts(nmk[b][:, 0:nk], sv, lo[b], NEG, ALU.is_lt, ALU.mult, scR + [scK, ("lo", b)], [("nmk", b)])
            dma(ST_ENG, NM[rows, 0:nk], nmk[b][:, 0:nk], [("nmk", b)], [("NM", t)], key=("nmst", b))

        prep(0)
        for t in range(NTL):
            main(t)
            if t + 1 < NTL:
                prep(t + 1)
            bisect(t)

        phase("d3")
        KVs = sb("KVs", [128, 3, NT], BF16)
        CKs = sb("CKs", [128, NTL, 256], BF16)
        wUV = sb("wUV", [128, 8, 2, 128], BF16)
        qfs = [sb("qfs%d" % i, [128, 8, 3, 512], BF16) for i in range(2)]
        NNM = 6
        nms = [sb("nms%d" % i, [128, 4, 128], BF16) for i in range(NNM)]
        NPT = 4
        pT = [sb("pT%d" % i, [128, 512], BF16) for i in range(NPT)]
        rl = sb("rl", [128, 512], F32)
        olat = sb("olat", [128, 2, 512], BF16)
        on = [sb("on%d" % i, [128, 512], BF16) for i in range(2)]
        lacc3 = sb("lacc3", [128, 512], F32)
        dma("sp", KVs, KVT, [], ["KVs"])
        dma("sp", CKs, CKV.rearrange("(t p) r -> p t r", p=128), [], ["CKs"])
        for hh in range(8):
            dma("sp", wUV[:, hh, :, :], Wb["b_w_uv"][hh].rearrange("(c p) v -> p c v", p=128), [], [("wUV", hh)])
        gcnt = [0]
        SB3 = (0, 1, 7)
        for qb in range(NQB):
            qbuf = qb % 2
            dma("sp", qfs[qbuf], QFT[:, :, :, qb * 512:(qb + 1) * 512], [], [("qfs", qbuf)])
            nj = 4 * qb + 4
            for hh in range(8):
                base = gcnt[0]

                def qk(j, qb=qb, hh=hh, base=base, qbuf=qbuf):
                    g = base + j
                    c0 = 0 if j < 4 * qb else 128 * (j - 4 * qb)
                    s0 = c0 // 128
                    nb_ = nms[g % NNM]
                    nk_ = ("nms", g % NNM)
                    dma("sp", nb_[:, s0:4, :], NM[qb * 512 + c0:(qb + 1) * 512, j * 128:(j + 1) * 128].rearrange("(s p) k -> p s k", p=128),
                        [], [nk_])
                    bk = SB3[g % 3]
                    ps_ = bank(bk)
                    pk_ = ("ps", bk)
                    mm(ps_[:, c0:512], KVs[:, 0, j * 128:(j + 1) * 128], qfs[qbuf][:, hh, 0, c0:512], True, False, ["KVs", ("qfs", qbuf)], [pk_])
                    mm(ps_[:, c0:512], KVs[:, 1, j * 128:(j + 1) * 128], qfs[qbuf][:, hh, 1, c0:512], False, False, ["KVs", ("qfs", qbuf)], [pk_])
                    mm(ps_[:, c0:512], KVs[:, 2, j * 128:(j + 1) * 128], qfs[qbuf][:, hh, 2, c0:512], False, False, ["KVs", ("qfs", qbuf)], [pk_])
                    for s in range(s0, 4):
                        mm(ps_[:, s * 128:(s + 1) * 128], nb_[:, s, :], ident, False, s == 3, [nk_], [pk_])
                    act(pT[g % NPT][:, c0:512], ps_[:, c0:512], AF.Exp, [pk_], [("pT", g % NPT)])

                def pv(j, qb=qb, hh=hh, base=base, nj=nj):
                    g = base + j
                    c0 = 0 if j < 4 * qb else 128 * (j - 4 * qb)
                    pt_ = pT[g % NPT]
                    pk = ("pT", g % NPT)
                    for rc in range(2):
                        mm(bank(2 + rc)[:, c0:512], CKs[:, j, rc * 128:(rc + 1) * 128], pt_[:, c0:512], j == 0, j == nj - 1, ["CKs", pk], [("po", rc)])
                    if j == 0:
                        cp(lacc3, pt_, [pk], ["lacc3"])
                    else:
                        tt(lacc3[:, c0:512], pt_[:, c0:512], lacc3[:, c0:512], ALU.add, [pk, "lacc3"], ["lacc3"])

                qk(0)
                if nj > 1:
                    qk(1)
                for j in range(nj):
                    if j + 2 < nj:
                        qk(j + 2)
                    pv(j)
                gcnt[0] += nj
                mm(bank(4), ones32, lacc3, True, True, ["lacc3"], ["pl"])
                recip(rl, bank(4), ["pl"], ["rl"])
                for rc in range(2):
                    tt(olat[:, rc, :], bank(2 + rc), rl, ALU.mult, [("po", rc), "rl"], [("olat", rc)])
                pvb = bank(5 + hh % 2)
                for rc in range(2):
                    mm(pvb, wUV[:, hh, rc, :], olat[:, rc, :], rc == 0, rc == 1, [("wUV", hh), ("olat", 0), ("olat", 1)], [("pv", hh % 2)])
                ob = on[hh % 2]
                cp(ob, pvb, [("pv", hh % 2)], [("on", hh % 2)], eng="act")
                dma(ST_ENG, OT[:, hh, qb * 512:(qb + 1) * 512], ob, [("on", hh % 2)], [("OT", hh, qb)], key=("onst", hh % 2))

        out_proj(Wb["b_w_out"], h_src, h_dst, 1, 0, dbg_dst)

    try:
        p0()
        t0()
        diff_attention(x, H[0], dbg.get("h0_0"))
        cross_attention(0, H[0], H[1], dbg.get("h0_1"))
        moe(0, H[1], H[0], dbg.get("h0_2"))
        dsa(H[0], H[1], dbg.get("h1_0"))
        cross_attention(1, H[1], H[0], dbg.get("h1_1"))
        moe(1, H[0], out, None)
    except StopBuild:
        pass
    S.barrier()
    S.emit()
    st.close()
    return nc, S


_CACHE = {}


def make_in_maps(inputs, NT, ncores):
    cf, cb = _consts()
    maps = []
    f = lambda a: np.ascontiguousarray(a, dtype=np.float32)
    for b in range(ncores):
        m = {
            "x": f(inputs["x"][b, :NT]), "mem": f(inputs["mem"][b]),
            "positions": np.ascontiguousarray(inputs["positions"][b, :NT].reshape(NT // 128, 128).astype(np.int32)),
            "a_w_in": f(inputs["a_w_in"][0]), "a_lambda": f(inputs["a_lambda"][0]),
            "a_subln_g": f(inputs["a_subln_g"][0].reshape(128, 1)), "a_w_out": f(inputs["a_w_out"][0]),
            "b_w_in": f(inputs["b_w_in"][0]), "b_q_norm_g": f(inputs["b_q_norm_g"][0]), "b_kv_norm_g": f(inputs["b_kv_norm_g"][0]),
            "b_w_uq": f(inputs["b_w_uq"][0]), "b_w_qidx": f(inputs["b_w_qidx"][0]), "b_w_uk": f(inputs["b_w_uk"][0]),
            "b_w_uv": f(inputs["b_w_uv"][0]), "b_w_out": f(inputs["b_w_out"][0]),
            "mem_w_kv": f(inputs["mem_w_kv"]), "xa_w_q": f(inputs["xa_w_q"]), "xa_w_out": f(inputs["xa_w_out"]),
            "moe_w_group": f(inputs["moe_w_group"]), "moe_b_group": f(inputs["moe_b_group"]),
            "moe_w_expert": f(inputs["moe_w_expert"]), "moe_b_expert": f(inputs["moe_b_expert"]),
            "moe_w_gate": f(inputs["moe_w_gate"]), "moe_w_up": f(inputs["moe_w_up"]), "moe_w_down": f(inputs["moe_w_down"]),
            "ln_g": f(inputs["ln_g"]), "ln_b": f(inputs["ln_b"]),
            "cstf": cf, "cstb": cb,
        }
        maps.append(m)
    return maps


def kernel(**inputs):
    NT = inputs["x"].shape[1]
    nb = inputs["x"].shape[0]
    if NT not in _CACHE:
        _CACHE[NT] = build(NT)[0]
    nc = _CACHE[NT]
    maps = make_in_maps(inputs, NT, nb)
    res = run_bass_kernel_spmd(nc, maps, core_ids=list(range(nb)))
    return np.stack([np.asarray(r["out"], dtype=np.float32) for r in res.results], axis=0)
```
